# Optimizing a Trainium2 kernel written in Bass

```python
import math
import jax
import jax.numpy as jnp
from jax import lax
import numpy as np

D_MODEL = 1024
BATCH = 8
SEQ = 8192
DEPTH = 2

CHUNK = 64
EPS = 1e-6
D_MIX = D_MODEL
HEAD_DIM = 64

A_WIDTH = 3 * D_MIX // 8
A_HEADS = A_WIDTH // HEAD_DIM
A_BLOCK = 128

B_WIDTH = 3 * D_MIX // 8
B_HEADS = B_WIDTH // HEAD_DIM
CONV_WIDTH = 4
LRU_C = 8.0

C_WIDTH = D_MIX - A_WIDTH - B_WIDTH
C_GROUP_DIM = 16
C_GROUPS = C_WIDTH // C_GROUP_DIM
C_STATE = 64

OFF_A_U = 0
OFF_A_V = OFF_A_U + A_WIDTH
OFF_B_X = OFF_A_V + A_WIDTH
OFF_B_G = OFF_B_X + B_WIDTH
OFF_C_X = OFF_B_G + B_WIDTH
IN_COLS = OFF_C_X + C_WIDTH

N_GROUPS = 4
EXPERTS_PER_GROUP = 4
N_EXPERTS = N_GROUPS * EXPERTS_PER_GROUP
TOP_K = 2
D_EXPERT = D_MODEL // 2
MOE_BLOCK = 256

kernel_name = "hybrid_gmlp_rglru_s5_hmoe"


def rms_norm(x, g):
    xf = x.astype(jnp.float32)
    y = xf * lax.rsqrt(jnp.mean(xf * xf, axis=-1, keepdims=True) + EPS)
    return (y * g.astype(jnp.float32)).astype(x.dtype)


def _linear_combine(left, right):
    a_l, b_l = left
    a_r, b_r = right
    return a_r * a_l, a_r * b_l + b_r


def linear_recurrence(a, b):
    _, h = lax.associative_scan(_linear_combine, (a, b), axis=1)
    return h


def causal_depthwise_conv(x, w, b):
    k_w = w.shape[0]
    seq = x.shape[1]
    xp = jnp.pad(x, ((0, 0), (k_w - 1, 0), (0, 0)))
    y = b + w[k_w - 1] * x
    for k in range(k_w - 1):
        y = y + w[k] * xp[:, k:k + seq]
    return y


def mixer_spatial_gating(u_raw, v_raw, v_norm_g, w_s, b_s):
    bsz, seq, _ = u_raw.shape
    u = jax.nn.gelu(u_raw)
    v = rms_norm(jax.nn.gelu(v_raw), v_norm_g)
    v = v.reshape(bsz, seq // A_BLOCK, A_BLOCK, A_HEADS, HEAD_DIM)
    chunk_id = jnp.arange(A_BLOCK) // CHUNK
    mask = chunk_id[None, :] <= chunk_id[:, None]
    w = jnp.where(mask[None], w_s, 0.0)
    sv = jnp.einsum("hij,bnjhd->bnihd", w, v) + b_s.T[:, :, None]
    return u * sv.reshape(bsz, seq, A_WIDTH)


def mixer_rglru(x_raw, g_raw, conv_w, conv_b, rg_w, rg_b, ig_w, ig_b, lam):
    bsz, seq, width = x_raw.shape
    xc = causal_depthwise_conv(x_raw, conv_w, conv_b)
    xh = xc.reshape(bsz, seq, B_HEADS, HEAD_DIM)
    r = jax.nn.sigmoid(jnp.einsum("bshi,hij->bshj", xh, rg_w).reshape(bsz, seq, width) + rg_b)
    i = jax.nn.sigmoid(jnp.einsum("bshi,hij->bshj", xh, ig_w).reshape(bsz, seq, width) + ig_b)
    log_a = -LRU_C * r.astype(jnp.float32) * jax.nn.softplus(-lam.astype(jnp.float32))
    a = jnp.exp(log_a)
    b = jnp.sqrt(-jnp.expm1(2.0 * log_a)) * (i * xc).astype(jnp.float32)
    h = linear_recurrence(a, b)
    return (h * jax.nn.gelu(g_raw.astype(jnp.float32))).astype(x_raw.dtype)


def mixer_s5(x_raw, a_re, a_im, log_dt, b_re, b_im, c_re, c_im, d, glu_w, glu_b):
    bsz, seq, width = x_raw.shape
    f32 = jnp.float32
    xf = x_raw.astype(f32)
    lam = lax.complex(a_re.astype(f32), a_im.astype(f32))
    dt = jnp.exp(log_dt.astype(f32))[:, None]
    lam_bar = jnp.exp(lam * dt)
    b_bar = ((lam_bar - 1.0) / lam)[:, :, None] * lax.complex(b_re.astype(f32), b_im.astype(f32))
    c_mat = lax.complex(c_re.astype(f32), c_im.astype(f32))
    xg = xf.reshape(bsz, seq, C_GROUPS, C_GROUP_DIM)
    bu = jnp.einsum("gpc,bsgc->bsgp", b_bar, xg.astype(jnp.complex64))
    state = linear_recurrence(jnp.broadcast_to(lam_bar, bu.shape), bu)
    y = jnp.einsum("gcp,bsgp->bsgc", c_mat, state).real.reshape(bsz, seq, width) + d.astype(f32) * xf
    y = jax.nn.gelu(y)
    y = y * jax.nn.sigmoid(y @ glu_w.astype(f32) + glu_b.astype(f32))
    return y.astype(x_raw.dtype)


def hybrid_mixer(h, norm_g, w_in, a_v_norm_g, a_spatial_w, a_spatial_b,
                 b_conv_w, b_conv_b, b_rg_w, b_rg_b, b_ig_w, b_ig_b, b_lambda,
                 c_a_re, c_a_im, c_log_dt, c_b_re, c_b_im, c_c_re, c_c_im, c_d, c_glu_w, c_glu_b,
                 mix_out_norm_g, w_out):
    z = rms_norm(h, norm_g) @ w_in
    y_a = mixer_spatial_gating(z[..., OFF_A_U:OFF_A_V], z[..., OFF_A_V:OFF_B_X],
                               a_v_norm_g, a_spatial_w, a_spatial_b)
    y_b = mixer_rglru(z[..., OFF_B_X:OFF_B_G], z[..., OFF_B_G:OFF_C_X],
                      b_conv_w, b_conv_b, b_rg_w, b_rg_b, b_ig_w, b_ig_b, b_lambda)
    y_c = mixer_s5(z[..., OFF_C_X:IN_COLS], c_a_re, c_a_im, c_log_dt, c_b_re, c_b_im,
                   c_c_re, c_c_im, c_d, c_glu_w, c_glu_b)
    y = jnp.concatenate([
        rms_norm(y_a, mix_out_norm_g[:A_WIDTH]),
        rms_norm(y_b, mix_out_norm_g[A_WIDTH:A_WIDTH + B_WIDTH]),
        rms_norm(y_c, mix_out_norm_g[A_WIDTH + B_WIDTH:]),
    ], axis=-1)
    return y @ w_out


def hierarchical_moe(hn, rg_w, rg_b, re_w, re_b, w_gate, w_up, w_down):
    bsz, seq, dm = hn.shape
    n_tok = bsz * seq
    xt = hn.reshape(n_tok, dm)
    f32 = jnp.float32
    g_logits = (xt @ rg_w).astype(f32) + rg_b.astype(f32)
    g_prob = jax.nn.softmax(g_logits, axis=-1)
    g_sel = jnp.argmax(g_logits, axis=-1).astype(jnp.int32)
    g_w = jnp.take_along_axis(g_prob, g_sel[:, None], axis=-1)[:, 0]
    e_all = jnp.einsum("td,gde->tge", xt, re_w).astype(f32) + re_b.astype(f32)
    e_logits = jnp.take_along_axis(e_all, g_sel[:, None, None], axis=1)[:, 0]
    e_top, e_idx = lax.top_k(e_logits, TOP_K)
    e_w = jax.nn.softmax(e_top, axis=-1)
    expert_id = g_sel[:, None] * EXPERTS_PER_GROUP + e_idx.astype(jnp.int32)
    gate = g_w[:, None] * e_w
    n_assign = n_tok * TOP_K
    flat_e = expert_id.reshape(n_assign)
    flat_tok = jnp.arange(n_assign, dtype=jnp.int32) // TOP_K
    flat_gate = gate.reshape(n_assign)
    order = jnp.argsort(flat_e)
    sorted_e = flat_e[order]
    counts = jnp.bincount(flat_e, length=N_EXPERTS)
    start = jnp.cumsum(counts) - counts
    padded = ((counts + MOE_BLOCK - 1) // MOE_BLOCK) * MOE_BLOCK
    pad_end = jnp.cumsum(padded)
    pad_start = pad_end - padded
    dest = pad_start[sorted_e] + (jnp.arange(n_assign, dtype=jnp.int32) - start[sorted_e])
    n_blocks = -(-n_assign // MOE_BLOCK) + N_EXPERTS
    n_rows = n_blocks * MOE_BLOCK
    row_tok = jnp.zeros((n_rows,), jnp.int32).at[dest].set(flat_tok[order])
    row_gate = jnp.zeros((n_rows,), f32).at[dest].set(flat_gate[order])
    block_e = jnp.minimum(
        jnp.searchsorted(pad_end, jnp.arange(n_blocks, dtype=pad_end.dtype) * MOE_BLOCK, side="right"),
        N_EXPERTS - 1).astype(jnp.int32)
    xs = jnp.take(xt, row_tok, axis=0).reshape(n_blocks, MOE_BLOCK, dm)

    def expert_block(args):
        xb, e = args
        return (jax.nn.silu(xb @ w_gate[e]) * (xb @ w_up[e])) @ w_down[e]

    ys = lax.map(expert_block, (xs, block_e)).reshape(n_rows, dm)
    ys = ys * row_gate[:, None].astype(ys.dtype)
    out = jnp.zeros((n_tok, dm), ys.dtype).at[row_tok].add(ys)
    return out.reshape(bsz, seq, dm)


def setup_inputs(seed: int = 0) -> dict:
    key = jax.random.key(seed)
    ks = iter(jax.random.split(key, 48))
    f32 = jnp.float32
    L = DEPTH

    def nrm(shape, scale):
        return scale * jax.random.normal(next(ks), shape, f32)

    x = jax.random.normal(next(ks), (BATCH, SEQ, D_MODEL), f32)
    norm_mix_g = 1.0 + nrm((L, D_MODEL), 0.02)
    w_in = nrm((L, D_MODEL, IN_COLS), D_MODEL ** -0.5)
    a_v_norm_g = 1.0 + nrm((L, A_WIDTH), 0.02)
    a_spatial_w = nrm((L, A_HEADS, A_BLOCK, A_BLOCK), A_BLOCK ** -0.5)
    a_spatial_b = 1.0 + nrm((L, A_HEADS, A_BLOCK), 0.02)
    b_conv_w = nrm((L, CONV_WIDTH, B_WIDTH), CONV_WIDTH ** -0.5)
    b_conv_b = nrm((L, B_WIDTH), 0.02)
    b_rg_w = nrm((L, B_HEADS, HEAD_DIM, HEAD_DIM), HEAD_DIM ** -0.5)
    b_rg_b = nrm((L, B_WIDTH), 0.02)
    b_ig_w = nrm((L, B_HEADS, HEAD_DIM, HEAD_DIM), HEAD_DIM ** -0.5)
    b_ig_b = nrm((L, B_WIDTH), 0.02)
    a_pow = jax.random.uniform(next(ks), (L, B_WIDTH), f32, 0.9, 0.999)
    a0 = a_pow ** (1.0 / LRU_C)
    b_lambda = jnp.log(a0) - jnp.log1p(-a0)
    n_idx = jnp.arange(C_STATE, dtype=f32)
    c_a_re = -0.5 * jnp.exp(nrm((L, C_GROUPS, C_STATE), 0.05))
    c_a_im = math.pi * n_idx + nrm((L, C_GROUPS, C_STATE), 0.01)
    c_log_dt = jax.random.uniform(next(ks), (L, C_GROUPS), f32, math.log(1e-3), math.log(1e-1))
    c_b_re = nrm((L, C_GROUPS, C_STATE, C_GROUP_DIM), (2 * C_GROUP_DIM) ** -0.5)
    c_b_im = nrm((L, C_GROUPS, C_STATE, C_GROUP_DIM), (2 * C_GROUP_DIM) ** -0.5)
    c_c_re = nrm((L, C_GROUPS, C_GROUP_DIM, C_STATE), 0.5)
    c_c_im = nrm((L, C_GROUPS, C_GROUP_DIM, C_STATE), 0.5)
    c_d = nrm((L, C_WIDTH), 1.0)
    c_glu_w = nrm((L, C_WIDTH, C_WIDTH), C_WIDTH ** -0.5)
    c_glu_b = nrm((L, C_WIDTH), 0.02)
    mix_out_norm_g = 1.0 + nrm((L, D_MIX), 0.02)
    w_out = nrm((L, D_MIX, D_MODEL), D_MIX ** -0.5)
    norm_ffn_g = 1.0 + nrm((L, D_MODEL), 0.02)
    router_group_w = nrm((L, D_MODEL, N_GROUPS), D_MODEL ** -0.5)
    router_group_b = nrm((L, N_GROUPS), 0.01)
    router_expert_w = nrm((L, N_GROUPS, D_MODEL, EXPERTS_PER_GROUP), D_MODEL ** -0.5)
    router_expert_b = nrm((L, N_GROUPS, EXPERTS_PER_GROUP), 0.01)
    expert_w_gate = nrm((L, N_EXPERTS, D_MODEL, D_EXPERT), D_MODEL ** -0.5)
    expert_w_up = nrm((L, N_EXPERTS, D_MODEL, D_EXPERT), D_MODEL ** -0.5)
    expert_w_down = nrm((L, N_EXPERTS, D_EXPERT, D_MODEL), D_EXPERT ** -0.5)
    final_norm_g = 1.0 + nrm((D_MODEL,), 0.02)
    return {
        "x": x, "norm_mix_g": norm_mix_g, "w_in": w_in,
        "a_v_norm_g": a_v_norm_g, "a_spatial_w": a_spatial_w, "a_spatial_b": a_spatial_b,
        "b_conv_w": b_conv_w, "b_conv_b": b_conv_b, "b_rg_w": b_rg_w, "b_rg_b": b_rg_b,
        "b_ig_w": b_ig_w, "b_ig_b": b_ig_b, "b_lambda": b_lambda,
        "c_a_re": c_a_re, "c_a_im": c_a_im, "c_log_dt": c_log_dt, "c_b_re": c_b_re, "c_b_im": c_b_im,
        "c_c_re": c_c_re, "c_c_im": c_c_im, "c_d": c_d, "c_glu_w": c_glu_w, "c_glu_b": c_glu_b,
        "mix_out_norm_g": mix_out_norm_g, "w_out": w_out, "norm_ffn_g": norm_ffn_g,
        "router_group_w": router_group_w, "router_group_b": router_group_b,
        "router_expert_w": router_expert_w, "router_expert_b": router_expert_b,
        "expert_w_gate": expert_w_gate, "expert_w_up": expert_w_up, "expert_w_down": expert_w_down,
        "final_norm_g": final_norm_g,
    }


def reference(x, norm_mix_g, w_in, a_v_norm_g, a_spatial_w, a_spatial_b,
              b_conv_w, b_conv_b, b_rg_w, b_rg_b, b_ig_w, b_ig_b, b_lambda,
              c_a_re, c_a_im, c_log_dt, c_b_re, c_b_im, c_c_re, c_c_im, c_d, c_glu_w, c_glu_b,
              mix_out_norm_g, w_out, norm_ffn_g,
              router_group_w, router_group_b, router_expert_w, router_expert_b,
              expert_w_gate, expert_w_up, expert_w_down, final_norm_g):
    h = x
    for l in range(DEPTH):
        mix = hybrid_mixer(h, norm_mix_g[l], w_in[l], a_v_norm_g[l], a_spatial_w[l], a_spatial_b[l],
                           b_conv_w[l], b_conv_b[l], b_rg_w[l], b_rg_b[l], b_ig_w[l], b_ig_b[l], b_lambda[l],
                           c_a_re[l], c_a_im[l], c_log_dt[l], c_b_re[l], c_b_im[l], c_c_re[l], c_c_im[l],
                           c_d[l], c_glu_w[l], c_glu_b[l], mix_out_norm_g[l], w_out[l])
        h = h + mix.astype(h.dtype)
        ffn = hierarchical_moe(rms_norm(h, norm_ffn_g[l]), router_group_w[l], router_group_b[l],
                               router_expert_w[l], router_expert_b[l],
                               expert_w_gate[l], expert_w_up[l], expert_w_down[l])
        h = h + ffn.astype(h.dtype)
    return rms_norm(h, final_norm_g).astype(x.dtype)
```

```python
import numpy as np
from contextlib import ExitStack
import concourse.bass as bass
import concourse.mybir as mybir
from concourse.bass_utils import run_bass_kernel_spmd

F32 = mybir.dt.float32
BF16 = mybir.dt.bfloat16
I32 = mybir.dt.int32
AF = mybir.ActivationFunctionType
ALU = mybir.AluOpType
AX = mybir.AxisListType

ENGS = ("pe", "act", "dve", "pool", "sp")
TWO_PI = float(2 * np.pi)


class Res:
    __slots__ = ("w", "r", "psum")

    def __init__(self):
        self.w = None
        self.r = {}
        self.psum = False


class TT_:
    def __init__(self, t):
        self.t = t
        self.r = Res()

    def __getitem__(self, k):
        return self.t[k]


class Prog:
    def __init__(self, nc, stack, n_dma_chan=48):
        self.nc = nc
        self.stack = stack
        self.q = {e: [] for e in ENGS}
        self.cnt = {e: 0 for e in ENGS}
        self.seen = {e: {} for e in ENGS}
        self.esem = {e: stack.enter_context(nc.semaphore("s_" + e)) for e in ENGS}
        self.chan = [stack.enter_context(nc.semaphore("s_dma%d" % i)) for i in range(n_dma_chan)]
        self.chan_cnt = [0] * n_dma_chan
        self.chan_next = 0
        half = n_dma_chan // 2
        self.ring = {"pool": list(range(half, n_dma_chan))}
        self.ring_pos = {"pool": 0, "hw": 0}
        self.ring["hw"] = list(range(0, half))
        self.semobj = {}
        for e in ENGS:
            self.semobj["E" + e] = self.esem[e]
        for i, s in enumerate(self.chan):
            self.semobj["C%d" % i] = s
        self.uid = 0
        self.pending = []
        self.sim_time = 0.0
        self.drain_tile = None

    def sb(self, shape, dtype, name=None, stack=None):
        self.uid += 1
        return TT_((stack or self.stack).enter_context(self.nc.sbuf_tensor(name or ("t%d" % self.uid), list(shape), dtype)))

    def ps(self, shape, dtype, name=None):
        self.uid += 1
        t = TT_(self.stack.enter_context(self.nc.psum_tensor(name or ("p%d" % self.uid), list(shape), dtype)))
        t.r.psum = True
        return t

    def _need(self, eng, reads, writes):
        deps = {}
        for r in reads:
            r = r.r if isinstance(r, TT_) else r
            if r.w is not None:
                k, v = r.w
                if deps.get(k, 0) < v:
                    deps[k] = v
            if r.psum:
                for k, v in r.r.items():
                    if k != "E" + eng and deps.get(k, 0) < v:
                        deps[k] = v
        for w in writes:
            w = w.r if isinstance(w, TT_) else w
            if w.w is not None:
                k, v = w.w
                if deps.get(k, 0) < v:
                    deps[k] = v
            for k, v in w.r.items():
                if deps.get(k, 0) < v:
                    deps[k] = v
        seen = self.seen[eng]
        for k, v in deps.items():
            if eng == "pe" and k == "Epe":
                continue
            if seen.get(k, 0) < v:
                seen[k] = v
                self.q[eng].append(("wait", k, v))

    def _commit(self, tok, reads, writes):
        k, v = tok
        for r in reads:
            r = r.r if isinstance(r, TT_) else r
            if r.r.get(k, 0) < v:
                r.r[k] = v
        for w in writes:
            w = w.r if isinstance(w, TT_) else w
            w.w = tok
            w.r = {}

    def op(self, eng, fn, reads=(), writes=(), cost=0.3, tset=None):
        self.pending.append(("op", eng, fn, self._norm(reads), self._norm(writes), cost, tset))

    def dma(self, eng, fn, reads=(), writes=(), cost=3.0):
        self.pending.append(("dma", eng, fn, self._norm(reads), self._norm(writes), cost, None))

    @staticmethod
    def _norm(lst):
        return [x if isinstance(x, Res) else x.r for x in lst]

    def _op_now(self, eng, fn, reads, writes):
        self._need(eng, reads, writes)
        self.cnt[eng] += 1
        tok = ("E" + eng, self.cnt[eng])
        self.q[eng].append(("op", fn))
        if eng == "dve" and self.drain_tile is not None and any(r.psum for r in reads):
            dt = self.drain_tile
            self.cnt[eng] += 1
            self.q[eng].append(("op", lambda e: e.memset(dt[:, 0:1], 0.0)))
            self._commit(tok, (), writes)
            self._commit(("E" + eng, self.cnt[eng]), reads, ())
            return
        self._commit(tok, reads, writes)

    def _dma_now(self, eng, fn, reads, writes):
        rk = "pool" if eng == "pool" else "hw"
        ring = self.ring[rk]
        c = ring[self.ring_pos[rk]]
        self.ring_pos[rk] = (self.ring_pos[rk] + 1) % len(ring)
        ck = "C%d" % c
        prev = self.chan_cnt[c] * 16
        if prev and self.seen[eng].get(ck, 0) < prev:
            self.seen[eng][ck] = prev
            self.q[eng].append(("wait", ck, prev))
        self._need(eng, reads, writes)
        self.chan_cnt[c] += 1
        tok = (ck, self.chan_cnt[c] * 16)
        self.q[eng].append(("dma", fn, ck))
        self._commit(tok, reads, writes)

    def flush(self, reorder=True, window=800):
        ops = self.pending
        self.pending = []
        n = len(ops)
        if n == 0:
            return
        if not reorder:
            order = range(n)
        else:
            preds = [None] * n
            lastw = {}
            readers = {}
            for i, o in enumerate(ops):
                ps = set()
                for r in o[3]:
                    j = lastw.get(id(r))
                    if j is not None:
                        ps.add(j)
                    if r.psum:
                        for j in readers.get(id(r), ()):
                            ps.add(j)
                for w in o[4]:
                    j = lastw.get(id(w))
                    if j is not None:
                        ps.add(j)
                    for j in readers.get(id(w), ()):
                        ps.add(j)
                for r in o[3]:
                    readers.setdefault(id(r), []).append(i)
                for w in o[4]:
                    lastw[id(w)] = i
                    readers[id(w)] = []
                ps.discard(i)
                preds[i] = ps
            succs = [[] for _ in range(n)]
            npred = [0] * n
            for i in range(n):
                npred[i] = len(preds[i])
                for j in preds[i]:
                    succs[j].append(i)
            rtime = [0.0] * n
            finish = [0.0] * n
            efree = {e: 0.0 for e in ENGS}
            ready = {e: [] for e in ENGS}
            for i in range(n):
                if npred[i] == 0:
                    ready[ops[i][1]].append(i)
            done = [False] * n
            lowest = 0
            order = []
            cur_set = None
            while len(order) < n:
                while lowest < n and done[lowest]:
                    lowest += 1
                lim = lowest + window
                best = None
                for e in ENGS:
                    ef = efree[e]
                    for i in ready[e]:
                        if i >= lim:
                            continue
                        st = rtime[i] if rtime[i] > ef else ef
                        if e == "act" and ops[i][6] is not None and ops[i][6] != cur_set:
                            st += 1.3
                        key = (st, i)
                        if best is None or key < best[0]:
                            best = (key, i, e)
                (st, _), i, e = best
                ready[e].remove(i)
                o = ops[i]
                if o[0] == "dma":
                    issue = 1.0 if e == "pool" else 0.06
                    efree[e] = st + issue
                    finish[i] = st + issue + o[5]
                else:
                    efree[e] = st + o[5]
                    finish[i] = st + o[5]
                    if e == "act" and o[6] is not None:
                        cur_set = o[6]
                done[i] = True
                order.append(i)
                for k in succs[i]:
                    npred[k] -= 1
                    t = finish[i] + (0.0 if ops[k][1] == e and o[0] == "op" else 0.15)
                    if t > rtime[k]:
                        rtime[k] = t
                    if npred[k] == 0:
                        ready[ops[k][1]].append(k)
            self.sim_time = max(finish)
        for i in order:
            o = ops[i]
            if o[0] == "op":
                self._op_now(o[1], o[2], o[3], o[4])
            else:
                self._dma_now(o[1], o[2], o[3], o[4])

    def barrier(self):
        for eng in ENGS:
            seen = self.seen[eng]
            for c in range(len(self.chan)):
                v = self.chan_cnt[c] * 16
                k = "C%d" % c
                if v and seen.get(k, 0) < v:
                    seen[k] = v
                    self.q[eng].append(("wait", k, v))
            for e in ENGS:
                k = "E" + e
                if e != eng and self.cnt[e] and seen.get(k, 0) < self.cnt[e]:
                    seen[k] = self.cnt[e]
                    self.q[eng].append(("wait", k, self.cnt[e]))

    def emit(self):
        nc = self.nc
        P = self

        def run(e, engobj):
            sem_e = P.esem[e]
            for item in P.q[e]:
                if item[0] == "wait":
                    engobj.wait_ge(P.semobj[item[1]], item[2])
                elif item[0] == "op":
                    item[1](engobj).then_inc(sem_e, 1)
                else:
                    item[1](engobj).then_inc(P.semobj[item[2]], 16)

        with nc.Block() as block:
            @block.tensor
            def _(e):
                run("pe", e)

            @block.scalar
            def _(e):
                run("act", e)

            @block.vector
            def _(e):
                run("dve", e)

            @block.gpsimd
            def _(e):
                run("pool", e)

            @block.sync
            def _(e):
                run("sp", e)
        self.q = {e: [] for e in ENGS}

    def phase_end(self, reorder=True):
        import os
        if os.environ.get("K_NOREORDER"):
            reorder = False
        self.flush(reorder)
        self.barrier()
        self.emit()


D = 1024
TT = 256
RD = 2
NSUB = TT // 128
EB = 512
NEXP = 16
EPS = 1e-6
BIG = 1.0e30

PARAMS = [
    ("norm_mix_g", [2, 1024]), ("w_in", [2, 1024, 1792]), ("a_v_norm_g", [2, 384]),
    ("a_spatial_w", [2, 6, 128, 128]), ("a_spatial_b", [2, 6, 128]), ("b_conv_w", [2, 4, 384]),
    ("b_conv_b", [2, 384]), ("b_rg_w", [2, 6, 64, 64]), ("b_rg_b", [2, 384]), ("b_ig_w", [2, 6, 64, 64]),
    ("b_ig_b", [2, 384]), ("b_lambda", [2, 384]), ("c_a_re", [2, 16, 64]), ("c_a_im", [2, 16, 64]),
    ("c_log_dt", [2, 16]), ("c_b_re", [2, 16, 64, 16]), ("c_b_im", [2, 16, 64, 16]),
    ("c_c_re", [2, 16, 16, 64]), ("c_c_im", [2, 16, 16, 64]), ("c_d", [2, 256]), ("c_glu_w", [2, 256, 256]),
    ("c_glu_b", [2, 256]), ("mix_out_norm_g", [2, 1024]), ("w_out", [2, 1024, 1024]), ("norm_ffn_g", [2, 1024]),
    ("router_group_w", [2, 1024, 4]), ("router_group_b", [2, 4]), ("router_expert_w", [2, 4, 1024, 4]),
    ("router_expert_b", [2, 4, 4]), ("expert_w_gate", [2, 16, 1024, 512]), ("expert_w_up", [2, 16, 1024, 512]),
    ("expert_w_down", [2, 16, 512, 1024]), ("final_norm_g", [1024]),
]


def build(S, n_layers=2, dbg=None):
    assert S % EB == 0
    NT = S // TT
    NS = S // 128
    NB = (2 * S) // EB + NEXP
    nc = bass.Bass("TRN2", target_bir_lowering=False)
    x_d = nc.dram_tensor("x", [S, D], F32, kind="ExternalInput").ap()
    W = {}
    for name, shp in PARAMS:
        W[name] = nc.dram_tensor(name, shp, F32, kind="ExternalInput").ap()
    out_d = nc.dram_tensor("out", [S, D], F32, kind="ExternalOutput").ap()
    hmid_d = nc.dram_tensor("hmid_scr", [S, D], F32, kind="Internal").ap()
    xf_d = nc.dram_tensor("xf_scr", [S, D], BF16, kind="Internal").ap()
    xs_d = nc.dram_tensor("xs_scr", [NB * EB, D], BF16, kind="Internal").ap()
    ys_d = nc.dram_tensor("ys_scr", [NB * EB, D], F32, kind="Internal").ap()
    r_hmid, r_xf, r_xs, r_ys = Res(), Res(), Res(), Res()
    dbg_d = {}
    REGS = {}
    if dbg:
        for k, shp in dbg.items():
            dbg_d[k] = nc.dram_tensor("dbg_" + k, shp, F32, kind="ExternalOutput").ap()

    nc_ctx = nc.allow_non_contiguous_dma(reason="small strided parameter loads")
    with ExitStack() as st0:
        st0.enter_context(nc_ctx)
        P = Prog(nc, st0)

        def fsz(ap):
            try:
                return int(ap.free_size())
            except Exception:
                n = 1
                for d in ap.shape[1:]:
                    n *= d
                return n

        def ecost(eng, ap, mult=1.0):
            n = fsz(ap) * mult
            if eng == "act":
                return 0.28 + n / 1200.0
            if eng == "dve":
                return 0.17 + n / 960.0
            return 0.35 + n / 480.0

        TSET = {AF.Gelu_apprx_tanh: 11, AF.Tanh: 11, AF.Exp: 6, AF.Ln: 6, AF.Sin: 9, AF.Silu: 18}

        def act(out, in_, func, reads, writes, **kw):
            P.op("act", lambda e: e.activation(out=out, in_=in_, func=func, **kw), reads, writes, cost=ecost("act", out),
                 tset=TSET.get(func))

        def tt(eng, out, in0, in1, op, reads, writes):
            P.op(eng, lambda e: e.tensor_tensor(out=out, in0=in0, in1=in1, op=op), reads, writes, cost=ecost(eng, out))

        def ts(eng, out, in0, s1, s2, op0, op1, reads, writes, **kw):
            if s2 is None:
                P.op(eng, lambda e: e.tensor_scalar(out=out, in0=in0, scalar1=s1, scalar2=None, op0=op0, **kw), reads, writes, cost=ecost(eng, out))
            else:
                P.op(eng, lambda e: e.tensor_scalar(out=out, in0=in0, scalar1=s1, scalar2=s2, op0=op0, op1=op1, **kw), reads, writes, cost=ecost(eng, out))

        def stt(out, in0, scalar, in1, op0, op1, reads, writes):
            P.op("dve", lambda e: e.scalar_tensor_tensor(out=out, in0=in0, scalar=scalar, in1=in1, op0=op0, op1=op1), reads, writes, cost=ecost("dve", out))

        def scan(out, d0, d1, init, reads, writes):
            P.op("dve", lambda e: e.tensor_tensor_scan(out=out, data0=d0, data1=d1, initial=init, op0=ALU.mult, op1=ALU.add), reads, writes,
                 cost=ecost("dve", out, 2.0))

        def red(out, in_, op, reads, writes):
            P.op("dve", lambda e: e.tensor_reduce(out=out, in_=in_, axis=AX.X, op=op), reads, writes, cost=ecost("dve", in_))

        def cp(eng, out, in_, reads, writes):
            if eng == "act":
                P.op("act", lambda e: e.activation(out=out, in_=in_, func=AF.Copy), reads, writes, cost=ecost("act", out))
            else:
                P.op(eng, lambda e: e.tensor_copy(out=out, in_=in_), reads, writes, cost=ecost(eng, out))

        def mm(out, lhsT, rhs, start, stop, reads, writes):
            n = max(64, fsz(rhs))
            c = n / 2400.0 * (4.0 if rhs.dtype == F32 else 1.0) + 0.03
            P.op("pe", lambda e: e.matmul(out, lhsT=lhsT, rhs=rhs, start=start, stop=stop), reads, writes, cost=c)

        def tr(out, in_, ident, reads, writes):
            P.op("pe", lambda e: e.transpose(out=out, in_=in_, identity=ident), reads, writes, cost=(0.35 if in_.dtype == F32 else 0.12))

        def dma(eng, out, in_, reads, writes):
            try:
                nb = int(out.nbytes())
            except Exception:
                nb = 65536
            P.dma(eng, lambda e: e.dma_start(out=out, in_=in_), reads, writes, cost=2.0 + nb / 250e3)

        def memset(eng, ap, val, writes):
            P.op(eng, lambda e: e.memset(ap, val), (), writes, cost=ecost(eng, ap))

        def recip(out, in_, reads, writes):
            P.op("dve", lambda e: e.reciprocal(out=out, in_=in_), reads, writes, cost=ecost("dve", out, 4.0))

        def rstd_from_ss(rs, ss, n, k=1):
            act(rs[:, 0:k], ss[:, 0:k], AF.Ln, [ss, eps_t], [rs], scale=1.0 / n, bias=eps_t[:, 0:1])
            act(rs[:, 0:k], rs[:, 0:k], AF.Exp, [rs], [rs], scale=-0.5)

        ident_b = P.sb([128, 128], BF16)
        ident_f = P.sb([128, 128], F32)
        ones_b = P.sb([128, 128], BF16)
        triu_b = P.sb([128, 128], BF16)
        iota_f = P.sb([128, 256], F32)
        iota16 = P.sb([128, 16], F32)
        pidx_i = P.sb([128, 1], I32)
        pidx_f = P.sb([128, 1], F32)
        eps_t = P.sb([128, 1], F32)
        onep_t = P.sb([128, 1], F32)
        halfpi_t = P.sb([128, 1], F32)
        rowtwo = P.sb([128, 2], F32)
        rowhalf = P.sb([128, 2], F32)
        rowq4 = P.sb([128, 4], F32)
        colq4 = P.sb([128, 4, 128], F32)
        tmpc = P.sb([128, 128], F32)
        tmpi = P.sb([128, 1], I32)
        ohs = P.sb([128, NS, 2, 16], BF16)
        rks = P.sb([128, NS, 2], F32)
        gts = P.sb([128, NS, 2], F32)
        base = P.sb([128, 16], F32)
        dest_i = P.sb([128, NS, 2], I32)
        blk_e = P.sb([128, NB], I32)
        psT = P.ps([128, 1024], BF16)
        psA = [P.ps([128, 512], F32) for _ in range(2)]
        psZ = [P.ps([128, 512], F32) for _ in range(2)]
        psM = [P.ps([128, 512], F32) for _ in range(3)]

        P.op("pool", lambda e: e.iota(iota_f[:], [[1, 256]], base=0, channel_multiplier=0, allow_small_or_imprecise_dtypes=True), (), [iota_f])
        P.op("pool", lambda e: e.iota(iota16[:], [[1, 16]], base=0, channel_multiplier=0, allow_small_or_imprecise_dtypes=True), (), [iota16])
        P.op("pool", lambda e: e.iota(pidx_i[:], [[1, 1]], base=0, channel_multiplier=1), (), [pidx_i])
        cp("dve", pidx_f[:], pidx_i[:], [pidx_i], [pidx_f])
        P.op("pool", lambda e: e.iota(tmpc[:], [[1, 128]], base=0, channel_multiplier=-1, allow_small_or_imprecise_dtypes=True), (), [tmpc])
        P.op("dve", lambda e: e.tensor_single_scalar(out=ident_f[:], in_=tmpc[:], scalar=0.0, op=ALU.is_equal), [tmpc], [ident_f])
        cp("dve", ident_b[:], ident_f[:], [ident_f], [ident_b])
        P.op("dve", lambda e: e.tensor_single_scalar(out=triu_b[:], in_=tmpc[:], scalar=0.0, op=ALU.is_gt), [tmpc], [triu_b])
        memset("pool", ones_b[:], 1.0, [ones_b])
        memset("pool", eps_t[:], EPS, [eps_t])
        memset("pool", onep_t[:], 1.0 + 1.2e-7, [onep_t])
        memset("pool", halfpi_t[:], float(np.pi / 2), [halfpi_t])
        P.op("dve", lambda e: e.tensor_single_scalar(out=tmpi[:], in_=pidx_i[:], scalar=4, op=ALU.arith_shift_right), [pidx_i], [tmpi])
        P.op("dve", lambda e: e.tensor_single_scalar(out=tmpi[:], in_=tmpi[:], scalar=1, op=ALU.bitwise_and), [tmpi], [tmpi])
        cp("dve", rowtwo[:, 1:2], tmpi[:], [tmpi], [rowtwo])
        ts("dve", rowtwo[:, 0:1], rowtwo[:, 1:2], -1.0, 1.0, ALU.mult, ALU.add, [rowtwo], [rowtwo])
        P.op("dve", lambda e: e.tensor_single_scalar(out=rowhalf[:, 1:2], in_=pidx_f[:], scalar=64.0, op=ALU.is_ge), [pidx_f], [rowhalf])
        ts("dve", rowhalf[:, 0:1], rowhalf[:, 1:2], -1.0, 1.0, ALU.mult, ALU.add, [rowhalf], [rowhalf])
        for q4 in range(4):
            ts("dve", rowq4[:, q4:q4 + 1], pidx_f[:], float(32 * q4), None, ALU.is_ge, None, [pidx_f], [rowq4])
            P.op("dve", lambda e, q4=q4: e.tensor_single_scalar(out=tmpc[:, 0:1], in_=pidx_f[:], scalar=float(32 * q4 + 32), op=ALU.is_lt), [pidx_f], [tmpc])
            tt("dve", rowq4[:, q4:q4 + 1], rowq4[:, q4:q4 + 1], tmpc[:, 0:1], ALU.mult, [rowq4, tmpc], [rowq4])
            memset("pool", colq4[:, q4, :], 0.0, [colq4])
            memset("pool", colq4[:, q4, 32 * q4:32 * q4 + 32], 1.0, [colq4])

        def load_layer_consts(l, C, stS):
            sb = P.sb

            def tmp(shape, dt):
                return P.sb(shape, dt, stack=stS)

            C["win"] = sb([128, 8, 1792], BF16)
            C["wout"] = sb([128, 8, 1024], BF16)
            C["gmix"] = sb([128, 1024], BF16)
            C["gffn"] = sb([128, 1024], F32)
            C["gv"] = sb([128, 384], BF16)
            C["goa"] = sb([128, 384], BF16)
            C["gobc"] = sb([128, 5], F32)
            C["wr"] = sb([128, 8, 20], F32)
            C["rb"] = sb([128, 20], F32)
            C["wmT"] = sb([128, 6, 128], BF16)
            C["bsT"] = sb([128, 6], F32)
            C["cw"] = sb([128, 3, 4], F32)
            C["coef"] = sb([128, 3], F32)
            C["coef2"] = sb([128, 3], F32)
            C["coefh"] = sb([128, 3], F32)
            C["rgbh"] = sb([128, 3], F32)
            C["igbh"] = sb([128, 3], F32)
            C["glubh"] = sb([128, 2], F32)
            C["gobch"] = sb([128, 2], F32)
            C["rr"] = sb([128, 8], F32)
            C["cos"] = sb([128, 8, TT], F32)
            C["sin"] = sb([128, 8, TT], F32)
            C["blhs"] = sb([128, 8, 2, 128], BF16)
            C["clhs"] = sb([128, 8, 2, 128], BF16)
            C["dd"] = sb([128, 2], F32)
            C["glub"] = sb([128, 2], F32)
            C["gluw"] = sb([128, 2, 256], BF16)
            C["cb"] = sb([128, 3], F32)
            C["rgb"] = sb([128, 3], F32)
            C["igb"] = sb([128, 3], F32)
            C["lam"] = sb([128, 3], F32)
            C["bdr"] = sb([128, 3, 128], BF16)
            C["bdi"] = sb([128, 3, 128], BF16)
            dma("pool", C["win"][:], W["w_in"][l].rearrange("(k p) n -> p k n", p=128), (), [C["win"]])
            dma("pool", C["wout"][:], W["w_out"][l].rearrange("(k p) n -> p k n", p=128), (), [C["wout"]])
            dma("pool", C["gmix"][:], W["norm_mix_g"][l].partition_broadcast(128), (), [C["gmix"]])
            dma("sp", C["gffn"][:], W["norm_ffn_g"][l].partition_broadcast(128), (), [C["gffn"]])
            dma("pool", C["gv"][:], W["a_v_norm_g"][l].partition_broadcast(128), (), [C["gv"]])
            dma("pool", C["goa"][:], W["mix_out_norm_g"][l][0:384].partition_broadcast(128), (), [C["goa"]])
            dma("sp", C["gobc"][:], W["mix_out_norm_g"][l][384:1024].rearrange("(c p) -> p c", p=128), (), [C["gobc"]])
            dma("sp", C["wr"][:, :, 0:4], W["router_group_w"][l].rearrange("(k p) n -> p k n", p=128), (), [C["wr"]])
            for g in range(4):
                dma("sp", C["wr"][:, :, 4 + 4 * g:8 + 4 * g], W["router_expert_w"][l, g].rearrange("(k p) n -> p k n", p=128), (), [C["wr"]])
            dma("sp", C["rb"][:, 0:4], W["router_group_b"][l].partition_broadcast(128), (), [C["rb"]])
            dma("sp", C["rb"][:, 4:20], W["router_expert_b"][l].rearrange("g e -> (g e)").partition_broadcast(128), (), [C["rb"]])
            wa_nat = tmp([128, 6, 128], F32)
            dma("sp", wa_nat[:], W["a_spatial_w"][l].rearrange("h i j -> i h j"), (), [wa_nat])
            for h in range(6):
                pz = psM[h % 3]
                tr(pz[:, 0:128], wa_nat[:, h, :], ident_f[:], [wa_nat, ident_f], [pz])
                cp("act" if h % 2 else "dve", C["wmT"][:, h, :], pz[:, 0:128], [pz], [C["wmT"]])
            memset("pool", C["wmT"][64:128, :, 0:64], 0.0, [C["wmT"]])
            dma("sp", C["bsT"][:], W["a_spatial_b"][l].rearrange("h i -> i h"), (), [C["bsT"]])
            for k in range(4):
                dma("sp", C["cw"][:, :, k], W["b_conv_w"][l, k].rearrange("(c p) -> p c", p=128), (), [C["cw"]])
            for nm, key in (("cb", "b_conv_b"), ("rgb", "b_rg_b"), ("igb", "b_ig_b"), ("lam", "b_lambda")):
                dma("sp", C[nm][:], W[key][l].rearrange("(c p) -> p c", p=128), (), [C[nm]])
            for nm, key in (("bdr", "b_rg_w"), ("bdi", "b_ig_w")):
                memset("pool", C[nm][:], 0.0, [C[nm]])
                for c3 in range(3):
                    for two in range(2):
                        dma("pool", C[nm][64 * two:64 * two + 64, c3, 64 * two:64 * two + 64], W[key][l, 2 * c3 + two], (), [C[nm]])
            spt = tmp([128, 3], F32)
            act(spt[:], C["lam"][:], AF.Exp, [C["lam"]], [spt], scale=-1.0)
            act(spt[:], spt[:], AF.Ln, [spt], [spt], bias=1.0)
            ts("dve", C["coef"][:], spt[:], -8.0, None, ALU.mult, None, [spt], [C["coef"]])
            ts("dve", C["coef2"][:], spt[:], -16.0, None, ALU.mult, None, [spt], [C["coef2"]])
            ts("dve", C["coefh"][:], spt[:], -4.0, None, ALU.mult, None, [spt], [C["coefh"]])
            ts("dve", C["rgbh"][:], C["rgb"][:], 0.5, None, ALU.mult, None, [C["rgb"]], [C["rgbh"]])
            ts("dve", C["igbh"][:], C["igb"][:], 0.5, None, ALU.mult, None, [C["igb"]], [C["igbh"]])
            ts("dve", C["gobch"][:], C["gobc"][:, 3:5], 0.5, None, ALU.mult, None, [C["gobc"]], [C["gobch"]])
            are = tmp([128, 8], F32)
            aim = tmp([128, 8], F32)
            ldt = tmp([128, 8], F32)
            dma("sp", are[:], W["c_a_re"][l].rearrange("(q two) p -> (two p) q", two=2), (), [are])
            dma("sp", aim[:], W["c_a_im"][l].rearrange("(q two) p -> (two p) q", two=2), (), [aim])
            ldv = W["c_log_dt"][l].rearrange("(q two) -> two q", two=2)
            dma("sp", ldt[0:64, :], ldv[0].partition_broadcast(64), (), [ldt])
            dma("sp", ldt[64:128, :], ldv[1].partition_broadcast(64), (), [ldt])
            dtt = tmp([128, 8], F32)
            act(dtt[:], ldt[:], AF.Exp, [ldt], [dtt])
            th = tmp([128, 8], F32)
            tt("dve", th[:], aim[:], dtt[:], ALU.mult, [aim, dtt], [th])
            tt("dve", C["rr"][:], are[:], dtt[:], ALU.mult, [are, dtt], [C["rr"]])
            act(C["rr"][:], C["rr"][:], AF.Exp, [C["rr"]], [C["rr"]])
            ang = tmp([128, 8, TT], F32)
            angi = tmp([128, 8, TT], I32)
            thn = tmp([128, 8], F32)
            ts("dve", thn[:], th[:], 1.0 / TWO_PI, None, ALU.mult, None, [th], [thn])
            ts("dve", ang[:, 0, :], iota_f[:, 0:TT], 1.0, None, ALU.add, None, [iota_f], [ang])
            for q in range(1, 8):
                cp("pool", ang[:, q, :], ang[:, 0, :], [ang], [ang])
            tt("dve", ang[:], ang[:], thn[:].unsqueeze(2).to_broadcast([128, 8, TT]), ALU.mult, [ang, thn], [ang])
            cp("dve", angi[:], ang[:], [ang], [angi])
            cp("dve", C["cos"][:], angi[:], [angi], [C["cos"]])
            tt("dve", ang[:], ang[:], C["cos"][:], ALU.subtract, [ang, C["cos"]], [ang])
            act(C["sin"][:], ang[:], AF.Sin, [ang], [C["sin"]], scale=TWO_PI * (1 - 1e-6))
            act(ang[:], ang[:], AF.Abs, [ang], [ang])
            act(C["cos"][:], ang[:], AF.Sin, [ang], [C["cos"]], scale=-TWO_PI * (1 - 1e-6), bias=halfpi_t[:, 0:1])
            nr = tmp([128, 8], F32)
            ni = tmp([128, 8], F32)
            den = tmp([128, 8], F32)
            fr = tmp([128, 8], F32)
            fi = tmp([128, 8], F32)
            t8 = tmp([128, 8], F32)
            tt("dve", nr[:], C["rr"][:], C["cos"][:, :, 0], ALU.mult, [C["rr"], C["cos"]], [nr])
            ts("dve", nr[:], nr[:], -1.0, None, ALU.add, None, [nr], [nr])
            tt("dve", ni[:], C["rr"][:], C["sin"][:, :, 0], ALU.mult, [C["rr"], C["sin"]], [ni])
            tt("dve", den[:], are[:], are[:], ALU.mult, [are], [den])
            tt("dve", t8[:], aim[:], aim[:], ALU.mult, [aim], [t8])
            tt("dve", den[:], den[:], t8[:], ALU.add, [den, t8], [den])
            recip(den[:], den[:], [den], [den])
            tt("dve", fr[:], nr[:], are[:], ALU.mult, [nr, are], [fr])
            tt("dve", t8[:], ni[:], aim[:], ALU.mult, [ni, aim], [t8])
            tt("dve", fr[:], fr[:], t8[:], ALU.add, [fr, t8], [fr])
            tt("dve", fr[:], fr[:], den[:], ALU.mult, [fr, den], [fr])
            tt("dve", fi[:], ni[:], are[:], ALU.mult, [ni, are], [fi])
            tt("dve", t8[:], nr[:], aim[:], ALU.mult, [nr, aim], [t8])
            tt("dve", fi[:], fi[:], t8[:], ALU.subtract, [fi, t8], [fi])
            tt("dve", fi[:], fi[:], den[:], ALU.mult, [fi, den], [fi])
            bre = tmp([128, 8, 16], F32)
            bim = tmp([128, 8, 16], F32)
            dma("sp", bre[:], W["c_b_re"][l].rearrange("(q two) p c -> (two p) q c", two=2), (), [bre])
            dma("sp", bim[:], W["c_b_im"][l].rearrange("(q two) p c -> (two p) q c", two=2), (), [bim])
            bbr = tmp([128, 8, 16], F32)
            bbi = tmp([128, 8, 16], F32)
            t16 = tmp([128, 8, 16], F32)
            frb = fr[:].unsqueeze(2).to_broadcast([128, 8, 16])
            fib = fi[:].unsqueeze(2).to_broadcast([128, 8, 16])
            tt("dve", bbr[:], bre[:], frb, ALU.mult, [bre, fr], [bbr])
            tt("dve", t16[:], bim[:], fib, ALU.mult, [bim, fi], [t16])
            tt("dve", bbr[:], bbr[:], t16[:], ALU.subtract, [bbr, t16], [bbr])
            tt("dve", bbi[:], bim[:], frb, ALU.mult, [bim, fr], [bbi])
            tt("dve", t16[:], bre[:], fib, ALU.mult, [bre, fi], [t16])
            tt("dve", bbi[:], bbi[:], t16[:], ALU.add, [bbi, t16], [bbi])
            bpad = tmp([128, 8, 2, 16], F32)
            for ri, src in enumerate((bbr, bbi)):
                for two in range(2):
                    ts("dve", bpad[:, :, two, :], src[:], rowhalf[:, two:two + 1], None, ALU.mult, None, [src, rowhalf], [bpad])
                for hh in range(2):
                    pz = psM[(ri * 2 + hh) % 3]
                    tr(pz[:, 0:128], bpad[:, 4 * hh:4 * hh + 4, :, :].rearrange("p a b c -> p (a b c)"), ident_f[:], [bpad, ident_f], [pz])
                    for q4 in range(4):
                        ts("dve", C["blhs"][:, 4 * hh + q4, ri, :], pz[:, 0:128], rowq4[:, q4:q4 + 1], None, ALU.mult, None, [pz, rowq4], [C["blhs"]])
            cn = tmp([128, 2, 64], F32)
            cpad = tmp([128, 2, 2, 64], F32)
            for ri, key in enumerate(("c_c_re", "c_c_im")):
                for hh in range(2):
                    dma("sp", cn[:, hh, :], W[key][l, 8 * hh:8 * hh + 8].rearrange("g c p -> (g c) p"), (), [cn])
                for two in range(2):
                    ts("dve", cpad[:, :, two, :], cn[:], rowtwo[:, two:two + 1], None, ALU.mult, None, [cn, rowtwo], [cpad])
                for hh in range(2):
                    pz = psM[(ri * 2 + hh) % 3]
                    tr(pz[:, 0:128], cpad[:, hh, :, :].rearrange("p a b -> p (a b)"), ident_f[:], [cpad, ident_f], [pz])
                    for q4 in range(4):
                        if ri == 0:
                            tt("dve", C["clhs"][:, 4 * hh + q4, 0, :], pz[:, 0:128], colq4[:, q4, :], ALU.mult, [pz, colq4], [C["clhs"]])
                        else:
                            stt(C["clhs"][:, 4 * hh + q4, 1, :], pz[:, 0:128], -1.0, colq4[:, q4, :], ALU.mult, ALU.mult, [pz, colq4], [C["clhs"]])
            dma("sp", C["dd"][:], W["c_d"][l].rearrange("(c p) -> p c", p=128), (), [C["dd"]])
            dma("sp", C["glub"][:], W["c_glu_b"][l].rearrange("(c p) -> p c", p=128), (), [C["glub"]])
            dma("pool", C["gluw"][:], W["c_glu_w"][l].rearrange("(k p) n -> p k n", p=128), (), [C["gluw"]])
            ts("dve", C["glubh"][:], C["glub"][:], 0.5, None, ALU.mult, None, [C["glub"]], [C["glubh"]])

        def alloc_mixer_work(Wk):
            sb = P.sb
            Wk["h"] = [sb([128, NSUB, 1024], F32) for _ in range(2)]
            Wk["zero"] = sb([128, 1024], BF16)
            Wk["xn"] = [sb([128, 1024], BF16) for _ in range(2)]
            Wk["xnT"] = [sb([128, 8, TT], BF16) for _ in range(2)]
            Wk["yT"] = [sb([128, 8, TT], BF16) for _ in range(2)]
            for nm in ("ssp", "rsp", "ssa", "rsa", "ssa2", "rsa2", "sse", "rse"):
                Wk[nm] = [sb([128, 2], F32) for _ in range(2)]
            Wk["u"] = sb([128, 384], F32)
            Wk["vg"] = sb([128, 384], F32)
            Wk["v"] = sb([128, 384], BF16)
            Wk["yab"] = sb([128, 384], BF16)
            Wk["xbe"] = [sb([128, 3, TT + 3], F32) for _ in range(2)]
            Wk["xc"] = sb([128, TT], F32)
            Wk["xcb"] = sb([128, TT], BF16)
            Wk["rg"] = sb([128, TT], F32)
            Wk["ig"] = sb([128, TT], F32)
            Wk["aa"] = sb([128, TT], F32)
            Wk["bb"] = sb([128, TT], F32)
            Wk["hs"] = sb([128, TT], F32)
            Wk["yb"] = sb([128, 3, TT], F32)
            Wk["ybs"] = sb([128, 3, TT], BF16)
            Wk["hcar"] = sb([128, 3], F32)
            Wk["nrmB"] = sb([128, TT], F32)
            Wk["nrmC"] = sb([128, TT], F32)
            Wk["xcc"] = [sb([128, 2, TT], F32) for _ in range(2)]
            Wk["xccb"] = [sb([128, 2, TT], BF16) for _ in range(2)]
            for nm in ("ure", "uim", "mre", "mim", "gre", "gim", "t1", "t2"):
                Wk[nm] = [sb([128, TT], F32) for _ in range(RD)]
            Wk["cst"] = [sb([128, 2], F32) for _ in range(RD)]
            Wk["sre"] = [sb([128, TT], BF16) for _ in range(RD)]
            Wk["sim"] = [sb([128, TT], BF16) for _ in range(RD)]
            Wk["scar"] = sb([128, 8, 2], F32)
            Wk["yc"] = sb([128, 2, TT], F32)
            Wk["ycb"] = sb([128, 2, TT], BF16)
            Wk["ycs"] = sb([128, 2, TT], BF16)
            Wk["sg"] = sb([128, TT], F32)
            Wk["xf"] = [sb([128, 1024], BF16) for _ in range(2)]
            Wk["xf32"] = sb([128, 1024], F32)
            Wk["xfT"] = [sb([128, 4, 128], F32) for _ in range(2)]
            for nm, w_ in (("lg", 20), ("em", 16), ("em2", 16), ("pen", 4), ("gsel", 4), ("rank", 16)):
                Wk[nm] = [sb([128, NSUB, w_], F32) for _ in range(2)]
            Wk["sm"] = [sb([128, 9, NSUB], F32) for _ in range(2)]
            Wk["ohb"] = [sb([128, NSUB, 16], BF16) for _ in range(2)]

        def mixer_tile(l, ti, C, Wk, first_layer):
            hb = Wk["h"][ti % 2]
            t0 = ti * TT
            for s in range(NSUB):
                gs = ti * NSUB + s
                rows = slice(t0 + s * 128, t0 + (s + 1) * 128)
                dma("sp", hb[:, s, :], (x_d if first_layer else hmid_d)[rows, :], (), [hb])
            xnT = Wk["xnT"][ti % 2]
            ss, rs = Wk["ssp"][ti % 2], Wk["rsp"][ti % 2]
            for s in range(NSUB):
                jk = Wk["xn"][s % 2]
                act(jk[:], hb[:, s, :], AF.Square, [hb], [ss, jk], accum_out=ss[:, s:s + 1])
            rstd_from_ss(rs, ss, 1024, NSUB)
            for s in range(NSUB):
                xn = Wk["xn"][s % 2]
                stt(xn[:], hb[:, s, :], rs[:, s:s + 1], C["gmix"][:], ALU.mult, ALU.mult, [hb, rs, C["gmix"]], [xn])
                for k in range(8):
                    tr(psT[:, k * 128:(k + 1) * 128], xn[:, k * 128:(k + 1) * 128], ident_b[:], [xn, ident_b], [psT])
                cp("act", xnT[:, :, s * 128:(s + 1) * 128], psT[:].rearrange("p (k t) -> p k t", k=8), [psT], [xnT])
            import os as _os
            _cut = float(_os.environ.get("K_CUT", "9"))
            if _cut <= 1:
                return
            yT = Wk["yT"][ti % 2]
            for s in range(NSUB):
                ssa, rsa = Wk["ssa"][s % 2], Wk["rsa"][s % 2]
                for half in range(2):
                    for k in range(8):
                        mm(psA[half][:, 0:384], xnT[:, k, s * 128:(s + 1) * 128], C["win"][:, k, 384 * half:384 * half + 384],
                           k == 0, k == 7, [xnT, C["win"]], [psA[half]])
                act(Wk["u"][:], psA[0][:, 0:384], AF.Gelu_apprx_tanh, [psA[0]], [Wk["u"]])
                act(Wk["vg"][:], psA[1][:, 0:384], AF.Gelu_apprx_tanh, [psA[1]], [Wk["vg"]])
                jk = Wk["v"]
                act(jk[:], Wk["vg"][:], AF.Square, [Wk["vg"]], [ssa, jk], accum_out=ssa[:, 0:1])
                rstd_from_ss(rsa, ssa, 384)
                stt(Wk["v"][:], Wk["vg"][:], rsa[:, 0:1], C["gv"][:], ALU.mult, ALU.mult, [Wk["vg"], rsa, C["gv"]], [Wk["v"]])
                for hh in range(6):
                    mm(psA[1][:, 64 * hh:64 * hh + 64], C["wmT"][:, hh, :], Wk["v"][:, 64 * hh:64 * hh + 64], True, True,
                       [C["wmT"], Wk["v"]], [psA[1]])
                ya = Wk["vg"]
                tt("dve", ya[:].rearrange("p (h d) -> p h d", h=6), psA[1][:, 0:384].rearrange("p (h d) -> p h d", h=6),
                   C["bsT"][:].unsqueeze(2).to_broadcast([128, 6, 64]), ALU.add, [psA[1], C["bsT"]], [ya])
                tt("dve", ya[:], ya[:], Wk["u"][:], ALU.mult, [ya, Wk["u"]], [ya])
                if dbg and "y_a" in dbg_d:
                    dma("sp", dbg_d["y_a"][t0 + s * 128:t0 + (s + 1) * 128, :], ya[:], [ya], ())
                ssa2, rsa2 = Wk["ssa2"][s % 2], Wk["rsa2"][s % 2]
                jk = Wk["yab"]
                act(jk[:], ya[:], AF.Square, [ya], [ssa2, jk], accum_out=ssa2[:, 0:1])
                rstd_from_ss(rsa2, ssa2, 384)
                stt(Wk["yab"][:], ya[:], rsa2[:, 0:1], C["goa"][:], ALU.mult, ALU.mult, [ya, rsa2, C["goa"]], [Wk["yab"]])
                for k in range(3):
                    tr(psT[:, k * 128:(k + 1) * 128], Wk["yab"][:, k * 128:(k + 1) * 128], ident_b[:], [Wk["yab"], ident_b], [psT])
                cp("act", yT[:, 0:3, s * 128:(s + 1) * 128], psT[:, 0:384].rearrange("p (k t) -> p k t", k=3), [psT], [yT])

            if _cut <= 2:
                return

            def zchunk(col0, pz, c0=0):
                for k in range(8):
                    mm(pz[:, c0:c0 + TT], C["win"][:, k, col0:col0 + 128], xnT[:, k, :], k == 0, k == 7, [C["win"], xnT], [pz])

            psB0, psB1 = psZ[0], psM[0]
            psC0, psC1, psC2 = psZ[1], psM[1], psM[2]
            xbe = Wk["xbe"][ti % 2]
            xbp = Wk["xbe"][(ti + 1) % 2]
            for c3 in range(3):
                zchunk(768 + 128 * c3, psB0)
                cp("act", xbe[:, c3, 3:3 + TT], psB0[:, 0:TT], [psB0], [xbe])
            if ti == 0:
                memset("pool", xbe[:, :, 0:3], 0.0, [xbe])
                memset("pool", Wk["hcar"][:], 0.0, [Wk["hcar"]])
                memset("pool", Wk["scar"][:], 0.0, [Wk["scar"]])
            else:
                cp("pool", xbe[:, :, 0:3], xbp[:, :, TT:TT + 3], [xbp], [xbe])
            for c3 in range(3):
                xc, xcb = Wk["xc"], Wk["xcb"]
                act(xc[:], xbe[:, c3, 3:3 + TT], AF.Identity, [xbe, C["cw"], C["cb"]], [xc], scale=C["cw"][:, c3, 3:4], bias=C["cb"][:, c3:c3 + 1])
                for k in range(3):
                    stt(xc[:], xbe[:, c3, k:k + TT], C["cw"][:, c3, k:k + 1], xc[:], ALU.mult, ALU.add, [xbe, C["cw"], xc], [xc])
                cp("act", xcb[:], xc[:], [xc], [xcb])
                mm(psB1[:, 0:TT], C["bdr"][:, c3, :], xcb[:], True, True, [C["bdr"], xcb], [psB1])
                mm(psB1[:, TT:2 * TT], C["bdi"][:, c3, :], xcb[:], True, True, [C["bdi"], xcb], [psB1])
                act(Wk["rg"][:], psB1[:, 0:TT], AF.Tanh, [psB1, C["rgbh"]], [Wk["rg"]], scale=0.5, bias=C["rgbh"][:, c3:c3 + 1])
                act(Wk["ig"][:], psB1[:, TT:2 * TT], AF.Tanh, [psB1, C["igbh"]], [Wk["ig"]], scale=0.5, bias=C["igbh"][:, c3:c3 + 1])
                act(Wk["aa"][:], Wk["rg"][:], AF.Exp, [Wk["rg"], C["coefh"]], [Wk["aa"]], scale=C["coefh"][:, c3:c3 + 1], bias=C["coefh"][:, c3:c3 + 1])
                act(Wk["bb"][:], Wk["rg"][:], AF.Exp, [Wk["rg"], C["coef"]], [Wk["bb"]], scale=C["coef"][:, c3:c3 + 1], bias=C["coef"][:, c3:c3 + 1])
                act(Wk["bb"][:], Wk["bb"][:], AF.Ln, [Wk["bb"], onep_t], [Wk["bb"]], scale=-1.0, bias=onep_t[:, 0:1])
                act(Wk["bb"][:], Wk["bb"][:], AF.Exp, [Wk["bb"]], [Wk["bb"]], scale=0.5)
                ts("pool", Wk["ig"][:], Wk["ig"][:], 1.0, None, ALU.add, None, [Wk["ig"]], [Wk["ig"]])
                tt("pool", Wk["ig"][:], Wk["ig"][:], xc[:], ALU.mult, [Wk["ig"], xc], [Wk["ig"]])
                stt(Wk["bb"][:], Wk["bb"][:], 0.5, Wk["ig"][:], ALU.mult, ALU.mult, [Wk["bb"], Wk["ig"]], [Wk["bb"]])
                scan(Wk["hs"][:], Wk["aa"][:], Wk["bb"][:], Wk["hcar"][:, c3:c3 + 1], [Wk["aa"], Wk["bb"], Wk["hcar"]], [Wk["hs"]])
                cp("act", Wk["hcar"][:, c3:c3 + 1], Wk["hs"][:, TT - 1:TT], [Wk["hs"]], [Wk["hcar"]])
                zchunk(1152 + 128 * c3, psB0)
                gg = Wk["rg"]
                act(gg[:], psB0[:, 0:TT], AF.Gelu_apprx_tanh, [psB0], [gg])
                tt("dve", Wk["yb"][:, c3, :], Wk["hs"][:], gg[:], ALU.mult, [Wk["hs"], gg], [Wk["yb"]])
                act(Wk["ybs"][:, c3, :], Wk["yb"][:, c3, :], AF.Square, [Wk["yb"]], [Wk["ybs"]])
            if dbg and "y_b" in dbg_d:
                for c3 in range(3):
                    dma("sp", dbg_d["y_b"][c3 * 128:(c3 + 1) * 128, t0:t0 + TT], Wk["yb"][:, c3, :], [Wk["yb"]], ())
            for c3 in range(3):
                mm(psB1[:, 0:TT], ones_b[:], Wk["ybs"][:, c3, :], c3 == 0, c3 == 2, [ones_b, Wk["ybs"]], [psB1])
            nrm = Wk["nrmB"]
            act(nrm[:], psB1[:, 0:TT], AF.Ln, [psB1, eps_t], [nrm], scale=1.0 / 384, bias=eps_t[:, 0:1])
            act(nrm[:], nrm[:], AF.Exp, [nrm], [nrm], scale=-0.5)
            for c3 in range(3):
                stt(yT[:, 3 + c3, :], Wk["yb"][:, c3, :], C["gobc"][:, c3:c3 + 1], nrm[:], ALU.mult, ALU.mult,
                    [Wk["yb"], C["gobc"], nrm], [yT])
            if _cut <= 3:
                return
            xcc, xccb = Wk["xcc"][ti % 2], Wk["xccb"][ti % 2]
            for hh in range(2):
                zchunk(1536 + 128 * hh, psC0)
                cp("act", xcc[:, hh, :], psC0[:, 0:TT], [psC0], [xcc])
                cp("dve", xccb[:, hh, :], psC0[:, 0:TT], [psC0], [xccb])
            for hh in range(2):
                for q4 in range(4):
                    q = 4 * hh + q4
                    j = (8 * ti + q) % RD
                    ure, uim, mre, mim, gre, gim, t1, t2 = (Wk[nm][j] for nm in ("ure", "uim", "mre", "mim", "gre", "gim", "t1", "t2"))
                    sre, sim = Wk["sre"][j], Wk["sim"][j]
                    cst = Wk["cst"][j]
                    mm(psC1[:, 0:TT], C["blhs"][:, q, 0, :], xccb[:, hh, :], True, True, [C["blhs"], xccb], [psC1])
                    mm(psC1[:, TT:2 * TT], C["blhs"][:, q, 1, :], xccb[:, hh, :], True, True, [C["blhs"], xccb], [psC1])
                    cq, sq_ = C["cos"][:, q, :], C["sin"][:, q, :]
                    L = TT - 1
                    cp("act", ure[:], psC1[:, 0:TT], [psC1], [ure])
                    cp("act", uim[:], psC1[:, TT:2 * TT], [psC1], [uim])
                    tt("pool", mre[:], ure[:], cq, ALU.mult, [ure, C["cos"]], [mre])
                    tt("pool", t1[:], uim[:], sq_, ALU.mult, [uim, C["sin"]], [t1])
                    tt("dve", mre[:], mre[:], t1[:], ALU.add, [mre, t1], [mre])
                    tt("pool", mim[:], uim[:], cq, ALU.mult, [uim, C["cos"]], [mim])
                    tt("pool", t2[:], ure[:], sq_, ALU.mult, [ure, C["sin"]], [t2])
                    tt("dve", mim[:], mim[:], t2[:], ALU.subtract, [mim, t2], [mim])
                    rb_ = C["rr"][:, q:q + 1].to_broadcast([128, TT])
                    scan(gre[:], rb_, mre[:], Wk["scar"][:, q, 0:1], [C["rr"], mre, Wk["scar"]], [gre])
                    scan(gim[:], rb_, mim[:], Wk["scar"][:, q, 1:2], [C["rr"], mim, Wk["scar"]], [gim])
                    tt("dve", cst[:, 0:1], gim[:, L:L + 1], C["sin"][:, q, L:L + 1], ALU.mult, [gim, C["sin"]], [cst])
                    stt(Wk["scar"][:, q, 0:1], gre[:, L:L + 1], C["cos"][:, q, L:L + 1], cst[:, 0:1], ALU.mult, ALU.subtract,
                        [gre, C["cos"], cst], [Wk["scar"]])
                    tt("dve", cst[:, 1:2], gim[:, L:L + 1], C["cos"][:, q, L:L + 1], ALU.mult, [gim, C["cos"]], [cst])
                    stt(Wk["scar"][:, q, 1:2], gre[:, L:L + 1], C["sin"][:, q, L:L + 1], cst[:, 1:2], ALU.mult, ALU.add,
                        [gre, C["sin"], cst], [Wk["scar"]])
                    tt("pool", t1[:], gre[:], cq, ALU.mult, [gre, C["cos"]], [t1])
                    tt("pool", t2[:], gim[:], sq_, ALU.mult, [gim, C["sin"]], [t2])
                    tt("dve", sre[:], t1[:], t2[:], ALU.subtract, [t1, t2], [sre])
                    tt("pool", t1[:], gre[:], sq_, ALU.mult, [gre, C["sin"]], [t1])
                    tt("pool", t2[:], gim[:], cq, ALU.mult, [gim, C["cos"]], [t2])
                    tt("dve", sim[:], t1[:], t2[:], ALU.add, [t1, t2], [sim])
                    mm(psC2[:, 0:TT], C["clhs"][:, q, 0, :], sre[:], q4 == 0, False, [C["clhs"], sre], [psC2])
                    mm(psC2[:, 0:TT], C["clhs"][:, q, 1, :], sim[:], False, q4 == 3, [C["clhs"], sim], [psC2])
                if _cut <= 3.7:
                    continue
                stt(Wk["yc"][:, hh, :], xcc[:, hh, :], C["dd"][:, hh:hh + 1], psC2[:, 0:TT], ALU.mult, ALU.add,
                    [xcc, C["dd"], psC2], [Wk["yc"]])
                act(Wk["yc"][:, hh, :], Wk["yc"][:, hh, :], AF.Gelu_apprx_tanh, [Wk["yc"]], [Wk["yc"]])
                cp("act", Wk["ycb"][:, hh, :], Wk["yc"][:, hh, :], [Wk["yc"]], [Wk["ycb"]])
            if _cut <= 3.8:
                return
            for ho in range(2):
                for hi in range(2):
                    mm(psC0[:, 0:TT], C["gluw"][:, hi, 128 * ho:128 * ho + 128], Wk["ycb"][:, hi, :], hi == 0, hi == 1, [C["gluw"], Wk["ycb"]], [psC0])
                act(Wk["sg"][:], psC0[:, 0:TT], AF.Tanh, [psC0, C["glubh"]], [Wk["sg"]], scale=0.5, bias=C["glubh"][:, ho:ho + 1])
                stt(Wk["yc"][:, ho, :], Wk["sg"][:], 1.0, Wk["yc"][:, ho, :], ALU.add, ALU.mult, [Wk["yc"], Wk["sg"]], [Wk["yc"]])
                act(Wk["ycs"][:, ho, :], Wk["yc"][:, ho, :], AF.Square, [Wk["yc"]], [Wk["ycs"]], scale=0.5)
            if dbg and "y_c" in dbg_d:
                for hh in range(2):
                    dma("sp", dbg_d["y_c"][hh * 128:(hh + 1) * 128, t0:t0 + TT], Wk["yc"][:, hh, :], [Wk["yc"]], ())
            for hh in range(2):
                mm(psC0[:, 0:TT], ones_b[:], Wk["ycs"][:, hh, :], hh == 0, hh == 1, [ones_b, Wk["ycs"]], [psC0])
            nrm = Wk["nrmC"]
            act(nrm[:], psC0[:, 0:TT], AF.Ln, [psC0, eps_t], [nrm], scale=1.0 / 256, bias=eps_t[:, 0:1])
            act(nrm[:], nrm[:], AF.Exp, [nrm], [nrm], scale=-0.5)
            for hh in range(2):
                stt(yT[:, 6 + hh, :], Wk["yc"][:, hh, :], C["gobch"][:, hh:hh + 1], nrm[:], ALU.mult, ALU.mult,
                    [Wk["yc"], C["gobch"], nrm], [yT])
            if _cut <= 4:
                return
            for s in range(NSUB):
                gs = ti * NSUB + s
                rows = slice(t0 + s * 128, t0 + (s + 1) * 128)
                for half in range(2):
                    for k in range(8):
                        mm(psA[half][:], yT[:, k, s * 128:(s + 1) * 128], C["wout"][:, k, 512 * half:512 * half + 512], k == 0, k == 7,
                           [yT, C["wout"]], [psA[half]])
                    tt("dve", hb[:, s, 512 * half:512 * half + 512], hb[:, s, 512 * half:512 * half + 512], psA[half][:], ALU.add,
                       [hb, psA[half]], [hb])
                dma("sp", hmid_d[rows, :], hb[:, s, :], [hb], ())
                sse, rse = Wk["sse"][gs % 2], Wk["rse"][gs % 2]
                jk = Wk["xf"][gs % 2]
                act(jk[:], hb[:, s, :], AF.Square, [hb], [sse, jk], accum_out=sse[:, 0:1])
                rstd_from_ss(rse, sse, 1024)
                xf = Wk["xf"][gs % 2]
                stt(Wk["xf32"][:], hb[:, s, :], rse[:, 0:1], C["gffn"][:], ALU.mult, ALU.mult, [hb, rse, C["gffn"]], [Wk["xf32"]])
                cp("act", xf[:], Wk["xf32"][:], [Wk["xf32"]], [xf])
                dma("sp", xf_d[rows, :], xf[:], [xf], ())
                route_logits(gs, s, C, Wk, psZ[0])
            route_tile(ti, C, Wk, psZ[0])

        def route_logits(gs, s_, C, Wk, pl):
            xf32 = Wk["xf32"]
            for half in range(2):
                xfT = Wk["xfT"][half]
                for k4 in range(4):
                    k = 4 * half + k4
                    tr(psA[half][:, k4 * 128:(k4 + 1) * 128], xf32[:, k * 128:(k + 1) * 128], ident_f[:], [xf32, ident_f], [psA[half]])
                cp("act", xfT[:], psA[half][:].rearrange("p (k t) -> p k t", k=4), [psA[half]], [xfT])
            for half in range(2):
                xfT = Wk["xfT"][half]
                for k4 in range(4):
                    k = 4 * half + k4
                    mm(pl[:, TT + 32 * s_:TT + 32 * s_ + 20], xfT[:, k4, :], C["wr"][:, k, :], k == 0, k == 7, [xfT, C["wr"]], [pl])

        def route_tile(ti, C, Wk, pl):
            J = NSUB
            g0 = ti * NSUB
            j = ti % 2
            lg, sm, em, em2, pen, gsel, rank, ohb = (Wk[nm][j] for nm in ("lg", "sm", "em", "em2", "pen", "gsel", "rank", "ohb"))
            plv = pl[:, TT:TT + 32 * J].rearrange("p (j c) -> p j c", j=J)
            tt("dve", lg[:], plv[:, :, 0:20], C["rb"][:].unsqueeze(1).to_broadcast([128, J, 20]), ALU.add, [pl, C["rb"]], [lg])
            red(sm[:, 0, :], lg[:, :, 0:4], ALU.max, [lg], [sm])
            tt("dve", em2[:, :, 0:4], lg[:, :, 0:4], sm[:, 0, :].unsqueeze(2).to_broadcast([128, J, 4]), ALU.subtract, [lg, sm], [em2])
            act(em2[:, :, 0:4], em2[:, :, 0:4], AF.Exp, [em2], [em2])
            red(sm[:, 1, :], em2[:, :, 0:4], ALU.add, [em2], [sm])
            recip(sm[:, 2, :], sm[:, 1, :], [sm], [sm])
            tt("dve", gsel[:], lg[:, :, 0:4], sm[:, 0, :].unsqueeze(2).to_broadcast([128, J, 4]), ALU.is_ge, [lg, sm], [gsel])
            ts("dve", pen[:], gsel[:], -1.0, BIG, ALU.add, ALU.mult, [gsel], [pen])
            for jj in range(J):
                tt("dve", em[:, jj, :].rearrange("p (g e) -> p g e", g=4), lg[:, jj, 4:20].rearrange("p (g e) -> p g e", g=4),
                   pen[:, jj, :].unsqueeze(2).to_broadcast([128, 4, 4]), ALU.add, [lg, pen], [em])
            red(sm[:, 3, :], em[:], ALU.max, [em], [sm])
            oh1 = ohs[:, g0:g0 + J, 0, :]
            oh2 = ohs[:, g0:g0 + J, 1, :]
            tt("dve", oh1, em[:], sm[:, 3, :].unsqueeze(2).to_broadcast([128, J, 16]), ALU.is_ge, [em, sm], [ohs])
            stt(em2[:], oh1, -BIG, em[:], ALU.mult, ALU.add, [ohs, em], [em2])
            red(sm[:, 4, :], em2[:], ALU.max, [em2], [sm])
            tt("dve", oh2, em2[:], sm[:, 4, :].unsqueeze(2).to_broadcast([128, J, 16]), ALU.is_ge, [em2, sm], [ohs])
            tt("dve", sm[:, 5, :], sm[:, 4, :], sm[:, 3, :], ALU.subtract, [sm], [sm])
            act(sm[:, 6, :], sm[:, 5, :], AF.Exp, [sm], [sm])
            ts("dve", sm[:, 7, :], sm[:, 6, :], 1.0, None, ALU.add, None, [sm], [sm])
            recip(sm[:, 7, :], sm[:, 7, :], [sm], [sm])
            tt("dve", sm[:, 8, :], sm[:, 6, :], sm[:, 7, :], ALU.mult, [sm], [sm])
            tt("dve", gts[:, g0:g0 + J, 0], sm[:, 7, :], sm[:, 2, :], ALU.mult, [sm], [gts])
            tt("dve", gts[:, g0:g0 + J, 1], sm[:, 8, :], sm[:, 2, :], ALU.mult, [sm], [gts])
            tt("dve", ohb[:], oh1, oh2, ALU.add, [ohs], [ohb])
            o1, o2 = TT + 32 * J, TT + 32 * J + 16 * J
            for jj in range(J):
                mm(pl[:, o1 + 16 * jj:o1 + 16 * jj + 16], triu_b[:], ohb[:, jj, :], True, True, [triu_b, ohb], [pl])
                mm(pl[:, o2 + 16 * jj:o2 + 16 * jj + 16], ones_b[:], ohb[:, jj, :], True, True, [ones_b, ohb], [pl])
            if ti == 0:
                memset("pool", base[:], 0.0, [base])
            prv = pl[:, o1:o1 + 16 * J].rearrange("p (j e) -> p j e", j=J)
            tt("dve", rank[:], prv, base[:].unsqueeze(1).to_broadcast([128, J, 16]), ALU.add, [pl, base], [rank])
            for jj in range(J):
                if jj >= 1:
                    for j0 in range(jj):
                        tt("dve", rank[:, jj, :], rank[:, jj, :], pl[:, o2 + 16 * j0:o2 + 16 * j0 + 16], ALU.add, [rank, pl], [rank])
            for jj in range(J):
                tt("dve", base[:], base[:], pl[:, o2 + 16 * jj:o2 + 16 * jj + 16], ALU.add, [base, pl], [base])
            for k in range(2):
                tt("dve", em2[:], ohs[:, g0:g0 + J, k, :], rank[:], ALU.mult, [ohs, rank], [em2])
                red(rks[:, g0:g0 + J, k], em2[:], ALU.add, [em2], [rks])

        def combine_phase(final):
            sb = P.sb
            hbs = [sb([128, 1024], F32) for _ in range(6)]
            yks = [sb([128, 1024], F32) for _ in range(8)]
            if final:
                gfin = sb([128, 1024], F32)
                dma("sp", gfin[:], W["final_norm_g"].partition_broadcast(128), (), [gfin])
                obs = [sb([128, 1024], F32) for _ in range(2)]
                junk = sb([128, 1024], BF16)
                sss = [sb([128, 1], F32) for _ in range(2)]
                rss = [sb([128, 1], F32) for _ in range(2)]
            for gs in range(NS):
                hb = hbs[gs % 6]
                rows = slice(gs * 128, (gs + 1) * 128)
                dma("sp", hb[:], hmid_d[rows, :], (), [hb])
                for k in range(2):
                    yk = yks[(2 * gs + k) % 8]
                    P.dma("pool", lambda e, k=k, yk=yk, gs=gs: e.indirect_dma_start(out=yk[:], out_offset=None, in_=ys_d,
                                                                             in_offset=bass.IndirectOffsetOnAxis(dest_i[:, gs, k:k + 1], 0)),
                          [dest_i], [yk], cost=6.0)
                    stt(hb[:], yk[:], gts[:, gs, k:k + 1], hb[:], ALU.mult, ALU.add, [yk, gts, hb], [hb])
                if not final:
                    dma("sp", hmid_d[rows, :], hb[:], [hb], ())
                else:
                    ss, rs, ob = sss[gs % 2], rss[gs % 2], obs[gs % 2]
                    act(junk[:], hb[:], AF.Square, [hb], [ss, junk], accum_out=ss[:, 0:1])
                    rstd_from_ss(rs, ss, 1024)
                    stt(ob[:], hb[:], rs[:, 0:1], gfin[:], ALU.mult, ALU.mult, [hb, rs, gfin], [ob])
                    dma("sp", out_d[rows, :], ob[:], [ob], ())

        class TTv:
            def __init__(self, base_t, sl):
                self.b = base_t
                self.sl = sl
                self.r = base_t.r

            def __getitem__(self, k):
                return self.b.t[:, self.sl]

        def dispatch_phase(Wd):
            sb = P.sb
            padded = sb([128, 16], F32)
            pend = sb([128, 16], F32)
            pstart = sb([128, 16], F32)
            ti_ = sb([128, 16], I32)
            ts("dve", padded[:], base[:], float(EB - 1), 1.0 / EB, ALU.add, ALU.mult, [base], [padded])
            ts("dve", padded[:], padded[:], -0.5 + 1.0 / (4 * EB), None, ALU.add, None, [padded], [padded])
            cp("dve", ti_[:], padded[:], [padded], [ti_])
            cp("dve", padded[:], ti_[:], [ti_], [padded])
            ts("dve", padded[:], padded[:], float(EB), None, ALU.mult, None, [padded], [padded])
            onesf = sb([128, 16], F32)
            memset("pool", onesf[:], 1.0, [onesf])
            P.op("dve", lambda e: e.tensor_tensor_scan(out=pend[:], data0=onesf[:], data1=padded[:], initial=0.0, op0=ALU.mult, op1=ALU.add),
                 [onesf, padded], [pend])
            tt("dve", pstart[:], pend[:], padded[:], ALU.subtract, [pend, padded], [pstart])
            big = sb([128, NS * 2, 16], F32)
            dsum = sb([128, NS * 2], F32)
            tt("dve", big[:], ohs[:].rearrange("p s k e -> p (s k) e"), pstart[:].unsqueeze(1).to_broadcast([128, NS * 2, 16]), ALU.mult,
               [ohs, pstart], [big])
            P.op("dve", lambda e: e.tensor_reduce(out=dsum[:], in_=big[:], axis=AX.X, op=ALU.add), [big], [dsum])
            tt("dve", dsum[:], dsum[:], rks[:].rearrange("p s k -> p (s k)"), ALU.add, [dsum, rks], [dsum])
            cp("dve", dest_i[:].rearrange("p s k -> p (s k)"), dsum[:], [dsum], [dest_i])
            bb_ = sb([128, NB, 16], F32)
            bsum = sb([128, NB], F32)
            thr = sb([128, NB], F32)
            ts("dve", thr[:], iota_f[:, 0:NB], float(EB), None, ALU.mult, None, [iota_f], [thr])
            tt("dve", bb_[:], pend[:].unsqueeze(1).to_broadcast([128, NB, 16]), thr[:].unsqueeze(2).to_broadcast([128, NB, 16]), ALU.is_le,
               [pend, thr], [bb_])
            P.op("dve", lambda e: e.tensor_reduce(out=bsum[:], in_=bb_[:], axis=AX.X, op=ALU.add), [bb_], [bsum])
            ts("dve", bsum[:], bsum[:], 15.0, None, ALU.min, None, [bsum], [bsum])
            cp("dve", blk_e[:], bsum[:], [bsum], [blk_e])
            xt = [sb([128, 1024], BF16) for _ in range(8)]
            for gs in range(NS):
                xb_ = xt[gs % 8]
                dma("sp", xb_[:], xf_d[gs * 128:(gs + 1) * 128, :], [r_xf], [xb_])
                for k in range(2):
                    P.dma("pool", lambda e, gs=gs, k=k, xb_=xb_: e.indirect_dma_start(
                        out=xs_d, out_offset=bass.IndirectOffsetOnAxis(dest_i[:, gs, k:k + 1], 0), in_=xb_[:], in_offset=None),
                        [xb_, dest_i], ())

        def expert_phase(l):
            sb = P.sb
            wg = [sb([128, 8, 512], BF16) for _ in range(2)]
            wu = [sb([128, 8, 512], BF16) for _ in range(2)]
            wd = [sb([128, 4, 1024], BF16) for _ in range(2)]
            xsb = [sb([128, 4, 1024], BF16) for _ in range(2)]
            xsTs = [sb([128, 8, EB], BF16) for _ in range(2)]
            sgts = [sb([128, EB], F32) for _ in range(2)]
            hTs = [sb([128, 4, EB], BF16) for _ in range(2)]
            yo = [sb([128, 1024], F32) for _ in range(4)]

            def wload(e, b, dst, src, pat):
                if "r" not in REGS:
                    REGS["r"] = e.alloc_register("ereg")
                rg = REGS["r"]
                id0 = nc.next_id()
                e.reg_load(rg, blk_e[0:1, b:b + 1])
                v = e.snap(rg, min_val=0, max_val=NEXP - 1)
                ins = e.dma_start(out=dst[:], in_=src[l][bass.ds(v, 1), :, :].rearrange(pat, p=128))
                id1 = nc.next_id()
                for i in range(id0, id1 + 1):
                    for nm in ("Pool_tmp_%d" % i, "Pool_Pool_ereg_snap_%d" % i):
                        try:
                            e.free_register(bass.RegisterHandle(nm, rg.engine))
                        except ValueError:
                            pass
                return ins

            for b in range(NB):
                i2 = b % 2
                xsT, hT = xsTs[i2], hTs[i2]
                P.dma("pool", lambda e, b=b, i2=i2: wload(e, b, wg[i2], W["expert_w_gate"], "o (k p) n -> p (o k) n"), [blk_e], [wg[i2]])
                P.dma("pool", lambda e, b=b, i2=i2: wload(e, b, wu[i2], W["expert_w_up"], "o (k p) n -> p (o k) n"), [blk_e], [wu[i2]])
                P.dma("pool", lambda e, b=b, i2=i2: wload(e, b, wd[i2], W["expert_w_down"], "o (k p) n -> p (o k) n"), [blk_e], [wd[i2]])
                dma("sp", xsb[i2][:], xs_d[b * EB:(b + 1) * EB, :].rearrange("(s p) d -> p s d", p=128), [r_xs], [xsb[i2]])
                for s in range(4):
                    if s % 2 == 0:
                        pt_ap, pt_res = psT[:], psT
                    else:
                        pt_ap, pt_res = psM[2][:].bitcast(BF16), psM[2]
                    for k in range(8):
                        tr(pt_ap[:, k * 128:(k + 1) * 128], xsb[i2][:, s, k * 128:(k + 1) * 128], ident_b[:], [xsb[i2], ident_b], [pt_res])
                    cp("act" if s % 2 else "dve", xsT[:, :, s * 128:(s + 1) * 128], pt_ap.rearrange("p (k t) -> p k t", k=8), [pt_res], [xsT])
                for f in range(4):
                    pg, pu = (psZ[0], psZ[1]) if f % 2 == 0 else (psM[0], psM[1])
                    sgt = sgts[f % 2]
                    for k in range(8):
                        mm(pg[:], wg[i2][:, k, 128 * f:128 * f + 128], xsT[:, k, :], k == 0, k == 7, [wg[i2], xsT], [pg])
                    for k in range(8):
                        mm(pu[:], wu[i2][:, k, 128 * f:128 * f + 128], xsT[:, k, :], k == 0, k == 7, [wu[i2], xsT], [pu])
                    act(sgt[:], pg[:], AF.Silu, [pg], [sgt])
                    tt("dve", hT[:, f, :], sgt[:], pu[:], ALU.mult, [sgt, pu], [hT])
                for s in range(4):
                    yb_ = yo[s % 4]
                    for half in range(2):
                        py = psA[half]
                        for f in range(4):
                            mm(py[:], hT[:, f, s * 128:(s + 1) * 128], wd[i2][:, f, 512 * half:512 * half + 512], f == 0, f == 3, [hT, wd[i2]], [py])
                        cp("act" if half else "dve", yb_[:, 512 * half:512 * half + 512], py[:], [py], [yb_])
                    dma("sp", ys_d[b * EB + s * 128:b * EB + (s + 1) * 128, :], yb_[:], [yb_], ())

        P.phase_end()
        for l in range(n_layers):
            with ExitStack() as stL:
                P.stack = stL
                C, Wk = {}, {}
                with ExitStack() as stS:
                    load_layer_consts(l, C, stS)
                    P.phase_end()
                alloc_mixer_work(Wk)
                memset("pool", Wk["zero"][:], 0.0, [Wk["zero"]])
                zrows = list(range(0, NB * EB, 128))
                zper = -(-len(zrows) // NT)
                for ti in range(NT):
                    mixer_tile(l, ti, C, Wk, first_layer=(l == 0))
                    for r0 in zrows[ti * zper:(ti + 1) * zper]:
                        dma("sp", xs_d[r0:r0 + 128, :], Wk["zero"][:], [Wk["zero"]], ())
                P.phase_end()
            import os as _os
            if _os.environ.get("K_STOP") == "mixer":
                break
            with ExitStack() as stD:
                P.stack = stD
                dispatch_phase(None)
                P.phase_end()
            with ExitStack() as stE:
                P.stack = stE
                expert_phase(l)
                P.phase_end()
            with ExitStack() as stC:
                P.stack = stC
                combine_phase(final=(l == n_layers - 1))
                P.phase_end()
        P.stack = st0
    return nc


_CACHE = {}


def kernel(**inputs):
    x = np.ascontiguousarray(inputs["x"], dtype=np.float32)
    B, S, _ = x.shape
    if S not in _CACHE:
        _CACHE[S] = build(S)
    nc = _CACHE[S]
    shared = {name: np.ascontiguousarray(inputs[name], dtype=np.float32) for name, _ in PARAMS}
    in_maps = []
    for b in range(B):
        m = dict(shared)
        m["x"] = x[b]
        in_maps.append(m)
    res = run_bass_kernel_spmd(nc, in_maps, core_ids=list(range(B)))
    return np.stack([np.asarray(r["out"], dtype=np.float32) for r in res.results], axis=0)
```

```python
import numpy as np
from contextlib import ExitStack
import concourse.bass as bass
import concourse.mybir as mybir
from concourse.bass_utils import run_bass_kernel_spmd

F32 = mybir.dt.float32
BF16 = mybir.dt.bfloat16
I32 = mybir.dt.int32
AF = mybir.ActivationFunctionType
ALU = mybir.AluOpType
AX = mybir.AxisListType

ENGS = ("pe", "act", "dve", "pool", "sp")
TWO_PI = float(2 * np.pi)


class Res:
    __slots__ = ("w", "r", "psum")

    def __init__(self):
        self.w = None
        self.r = {}
        self.psum = False


class TT_:
    def __init__(self, t):
        self.t = t
        self.r = Res()

    def __getitem__(self, k):
        return self.t[k]


class Prog:
    def __init__(self, nc, stack, n_dma_chan=48):
        self.nc = nc
        self.stack = stack
        self.q = {e: [] for e in ENGS}
        self.cnt = {e: 0 for e in ENGS}
        self.seen = {e: {} for e in ENGS}
        self.esem = {e: stack.enter_context(nc.semaphore("s_" + e)) for e in ENGS}
        self.chan = [stack.enter_context(nc.semaphore("s_dma%d" % i)) for i in range(n_dma_chan)]
        self.chan_cnt = [0] * n_dma_chan
        self.chan_next = 0
        half = n_dma_chan // 2
        self.ring = {"pool": list(range(half, n_dma_chan))}
        self.ring_pos = {"pool": 0, "hw": 0}
        self.ring["hw"] = list(range(0, half))
        self.semobj = {}
        for e in ENGS:
            self.semobj["E" + e] = self.esem[e]
        for i, s in enumerate(self.chan):
            self.semobj["C%d" % i] = s
        self.uid = 0
        self.pending = []
        self.sim_time = 0.0
        self.drain_tile = None

    def sb(self, shape, dtype, name=None, stack=None):
        self.uid += 1
        return TT_((stack or self.stack).enter_context(self.nc.sbuf_tensor(name or ("t%d" % self.uid), list(shape), dtype)))

    def ps(self, shape, dtype, name=None):
        self.uid += 1
        t = TT_(self.stack.enter_context(self.nc.psum_tensor(name or ("p%d" % self.uid), list(shape), dtype)))
        t.r.psum = True
        return t

    def _need(self, eng, reads, writes):
        deps = {}
        for r in reads:
            r = r.r if isinstance(r, TT_) else r
            if r.w is not None:
                k, v = r.w
                if deps.get(k, 0) < v:
                    deps[k] = v
            if r.psum:
                for k, v in r.r.items():
                    if k != "E" + eng and deps.get(k, 0) < v:
                        deps[k] = v
        for w in writes:
            w = w.r if isinstance(w, TT_) else w
            if w.w is not None:
                k, v = w.w
                if deps.get(k, 0) < v:
                    deps[k] = v
            for k, v in w.r.items():
                if deps.get(k, 0) < v:
                    deps[k] = v
        seen = self.seen[eng]
        for k, v in deps.items():
            if eng == "pe" and k == "Epe":
                continue
            if seen.get(k, 0) < v:
                seen[k] = v
                self.q[eng].append(("wait", k, v))

    def _commit(self, tok, reads, writes):
        k, v = tok
        for r in reads:
            r = r.r if isinstance(r, TT_) else r
            if r.r.get(k, 0) < v:
                r.r[k] = v
        for w in writes:
            w = w.r if isinstance(w, TT_) else w
            w.w = tok
            w.r = {}

    def op(self, eng, fn, reads=(), writes=(), cost=0.3, tset=None):
        self.pending.append(("op", eng, fn, self._norm(reads), self._norm(writes), cost, tset))

    def dma(self, eng, fn, reads=(), writes=(), cost=3.0):
        self.pending.append(("dma", eng, fn, self._norm(reads), self._norm(writes), cost, None))

    @staticmethod
    def _norm(lst):
        return [x if isinstance(x, Res) else x.r for x in lst]

    def _op_now(self, eng, fn, reads, writes):
        self._need(eng, reads, writes)
        self.cnt[eng] += 1
        tok = ("E" + eng, self.cnt[eng])
        self.q[eng].append(("op", fn))
        if eng == "dve" and self.drain_tile is not None and any(r.psum for r in reads):
            dt = self.drain_tile
            self.cnt[eng] += 1
            self.q[eng].append(("op", lambda e: e.memset(dt[:, 0:1], 0.0)))
            self._commit(tok, (), writes)
            self._commit(("E" + eng, self.cnt[eng]), reads, ())
            return
        self._commit(tok, reads, writes)

    def _dma_now(self, eng, fn, reads, writes):
        rk = "pool" if eng == "pool" else "hw"
        ring = self.ring[rk]
        c = ring[self.ring_pos[rk]]
        self.ring_pos[rk] = (self.ring_pos[rk] + 1) % len(ring)
        ck = "C%d" % c
        prev = self.chan_cnt[c] * 16
        if prev and self.seen[eng].get(ck, 0) < prev:
            self.seen[eng][ck] = prev
            self.q[eng].append(("wait", ck, prev))
        self._need(eng, reads, writes)
        self.chan_cnt[c] += 1
        tok = (ck, self.chan_cnt[c] * 16)
        self.q[eng].append(("dma", fn, ck))
        self._commit(tok, reads, writes)

    def flush(self, reorder=True, window=800):
        ops = self.pending
        self.pending = []
        n = len(ops)
        if n == 0:
            return
        if not reorder:
            order = range(n)
        else:
            preds = [None] * n
            lastw = {}
            readers = {}
            for i, o in enumerate(ops):
                ps = set()
                for r in o[3]:
                    j = lastw.get(id(r))
                    if j is not None:
                        ps.add(j)
                    if r.psum:
                        for j in readers.get(id(r), ()):
                            ps.add(j)
                for w in o[4]:
                    j = lastw.get(id(w))
                    if j is not None:
                        ps.add(j)
                    for j in readers.get(id(w), ()):
                        ps.add(j)
                for r in o[3]:
                    readers.setdefault(id(r), []).append(i)
                for w in o[4]:
                    lastw[id(w)] = i
                    readers[id(w)] = []
                ps.discard(i)
                preds[i] = ps
            succs = [[] for _ in range(n)]
            npred = [0] * n
            for i in range(n):
                npred[i] = len(preds[i])
                for j in preds[i]:
                    succs[j].append(i)
            rtime = [0.0] * n
            finish = [0.0] * n
            efree = {e: 0.0 for e in ENGS}
            ready = {e: [] for e in ENGS}
            for i in range(n):
                if npred[i] == 0:
                    ready[ops[i][1]].append(i)
            done = [False] * n
            lowest = 0
            order = []
            cur_set = None
            while len(order) < n:
                while lowest < n and done[lowest]:
                    lowest += 1
                lim = lowest + window
                best = None
                for e in ENGS:
                    ef = efree[e]
                    for i in ready[e]:
                        if i >= lim:
                            continue
                        st = rtime[i] if rtime[i] > ef else ef
                        if e == "act" and ops[i][6] is not None and ops[i][6] != cur_set:
                            st += 1.3
                        key = (st, i)
                        if best is None or key < best[0]:
                            best = (key, i, e)
                (st, _), i, e = best
                ready[e].remove(i)
                o = ops[i]
                if o[0] == "dma":
                    issue = 1.0 if e == "pool" else 0.06
                    efree[e] = st + issue
                    finish[i] = st + issue + o[5]
                else:
                    efree[e] = st + o[5]
                    finish[i] = st + o[5]
                    if e == "act" and o[6] is not None:
                        cur_set = o[6]
                done[i] = True
                order.append(i)
                for k in succs[i]:
                    npred[k] -= 1
                    t = finish[i] + (0.1 if ops[k][1] == e and o[0] == "op" else 0.45)
                    if t > rtime[k]:
                        rtime[k] = t
                    if npred[k] == 0:
                        ready[ops[k][1]].append(k)
            self.sim_time = max(finish)
        for i in order:
            o = ops[i]
            if o[0] == "op":
                self._op_now(o[1], o[2], o[3], o[4])
            else:
                self._dma_now(o[1], o[2], o[3], o[4])

    def barrier(self):
        for eng in ENGS:
            seen = self.seen[eng]
            for c in range(len(self.chan)):
                v = self.chan_cnt[c] * 16
                k = "C%d" % c
                if v and seen.get(k, 0) < v:
                    seen[k] = v
                    self.q[eng].append(("wait", k, v))
            for e in ENGS:
                k = "E" + e
                if e != eng and self.cnt[e] and seen.get(k, 0) < self.cnt[e]:
                    seen[k] = self.cnt[e]
                    self.q[eng].append(("wait", k, self.cnt[e]))

    def emit(self):
        nc = self.nc
        P = self

        def run(e, engobj):
            sem_e = P.esem[e]
            for item in P.q[e]:
                if item[0] == "wait":
                    engobj.wait_ge(P.semobj[item[1]], item[2])
                elif item[0] == "op":
                    item[1](engobj).then_inc(sem_e, 1)
                else:
                    item[1](engobj).then_inc(P.semobj[item[2]], 16)

        with nc.Block() as block:
            @block.tensor
            def _(e):
                run("pe", e)

            @block.scalar
            def _(e):
                run("act", e)

            @block.vector
            def _(e):
                run("dve", e)

            @block.gpsimd
            def _(e):
                run("pool", e)

            @block.sync
            def _(e):
                run("sp", e)
        self.q = {e: [] for e in ENGS}

    def phase_end(self, reorder=True):
        import os
        if os.environ.get("K_NOREORDER"):
            reorder = False
        self.flush(reorder)
        self.barrier()
        self.emit()


D = 1024
TT = 256
NSUB = TT // 128
EB = 512
NEXP = 16
EPS = 1e-6
BIG = 1.0e30

PARAMS = [
    ("norm_mix_g", [2, 1024]), ("w_in", [2, 1024, 1792]), ("a_v_norm_g", [2, 384]),
    ("a_spatial_w", [2, 6, 128, 128]), ("a_spatial_b", [2, 6, 128]), ("b_conv_w", [2, 4, 384]),
    ("b_conv_b", [2, 384]), ("b_rg_w", [2, 6, 64, 64]), ("b_rg_b", [2, 384]), ("b_ig_w", [2, 6, 64, 64]),
    ("b_ig_b", [2, 384]), ("b_lambda", [2, 384]), ("c_a_re", [2, 16, 64]), ("c_a_im", [2, 16, 64]),
    ("c_log_dt", [2, 16]), ("c_b_re", [2, 16, 64, 16]), ("c_b_im", [2, 16, 64, 16]),
    ("c_c_re", [2, 16, 16, 64]), ("c_c_im", [2, 16, 16, 64]), ("c_d", [2, 256]), ("c_glu_w", [2, 256, 256]),
    ("c_glu_b", [2, 256]), ("mix_out_norm_g", [2, 1024]), ("w_out", [2, 1024, 1024]), ("norm_ffn_g", [2, 1024]),
    ("router_group_w", [2, 1024, 4]), ("router_group_b", [2, 4]), ("router_expert_w", [2, 4, 1024, 4]),
    ("router_expert_b", [2, 4, 4]), ("expert_w_gate", [2, 16, 1024, 512]), ("expert_w_up", [2, 16, 1024, 512]),
    ("expert_w_down", [2, 16, 512, 1024]), ("final_norm_g", [1024]),
]


def build(S, n_layers=2, dbg=None):
    assert S % EB == 0
    NT = S // TT
    NS = S // 128
    NB = (2 * S) // EB + NEXP
    nc = bass.Bass("TRN2", target_bir_lowering=False)
    x_d = nc.dram_tensor("x", [S, D], F32, kind="ExternalInput").ap()
    W = {}
    for name, shp in PARAMS:
        W[name] = nc.dram_tensor(name, shp, F32, kind="ExternalInput").ap()
    out_d = nc.dram_tensor("out", [S, D], F32, kind="ExternalOutput").ap()
    hmid_d = nc.dram_tensor("hmid_scr", [S, D], F32, kind="Internal").ap()
    xf_d = nc.dram_tensor("xf_scr", [S, D], BF16, kind="Internal").ap()
    xs_d = nc.dram_tensor("xs_scr", [NB * EB, D], BF16, kind="Internal").ap()
    ys_d = nc.dram_tensor("ys_scr", [NB * EB, D], F32, kind="Internal").ap()
    r_hmid, r_xf, r_xs, r_ys = Res(), Res(), Res(), Res()
    dbg_d = {}
    REGS = {}
    if dbg:
        for k, shp in dbg.items():
            dbg_d[k] = nc.dram_tensor("dbg_" + k, shp, F32, kind="ExternalOutput").ap()

    nc_ctx = nc.allow_non_contiguous_dma(reason="small strided parameter loads")
    with ExitStack() as st0:
        st0.enter_context(nc_ctx)
        P = Prog(nc, st0)

        def fsz(ap):
            try:
                return int(ap.free_size())
            except Exception:
                n = 1
                for d in ap.shape[1:]:
                    n *= d
                return n

        def ecost(eng, ap, mult=1.0):
            n = fsz(ap) * mult
            if eng == "act":
                return 0.28 + n / 1200.0
            if eng == "dve":
                return 0.17 + n / 960.0
            return 0.35 + n / 480.0

        TSET = {AF.Gelu_apprx_tanh: 11, AF.Tanh: 11, AF.Exp: 6, AF.Ln: 6, AF.Sin: 9, AF.Silu: 18}

        def act(out, in_, func, reads, writes, **kw):
            P.op("act", lambda e: e.activation(out=out, in_=in_, func=func, **kw), reads, writes, cost=ecost("act", out),
                 tset=TSET.get(func))

        def tt(eng, out, in0, in1, op, reads, writes):
            P.op(eng, lambda e: e.tensor_tensor(out=out, in0=in0, in1=in1, op=op), reads, writes, cost=ecost(eng, out))

        def ts(eng, out, in0, s1, s2, op0, op1, reads, writes, **kw):
            if s2 is None:
                P.op(eng, lambda e: e.tensor_scalar(out=out, in0=in0, scalar1=s1, scalar2=None, op0=op0, **kw), reads, writes, cost=ecost(eng, out))
            else:
                P.op(eng, lambda e: e.tensor_scalar(out=out, in0=in0, scalar1=s1, scalar2=s2, op0=op0, op1=op1, **kw), reads, writes, cost=ecost(eng, out))

        def stt(out, in0, scalar, in1, op0, op1, reads, writes):
            P.op("dve", lambda e: e.scalar_tensor_tensor(out=out, in0=in0, scalar=scalar, in1=in1, op0=op0, op1=op1), reads, writes, cost=ecost("dve", out))

        def scan(out, d0, d1, init, reads, writes):
            P.op("dve", lambda e: e.tensor_tensor_scan(out=out, data0=d0, data1=d1, initial=init, op0=ALU.mult, op1=ALU.add), reads, writes,
                 cost=ecost("dve", out, 2.0))

        def red(out, in_, op, reads, writes):
            P.op("dve", lambda e: e.tensor_reduce(out=out, in_=in_, axis=AX.X, op=op), reads, writes, cost=ecost("dve", in_))

        def cp(eng, out, in_, reads, writes):
            if eng == "act":
                P.op("act", lambda e: e.activation(out=out, in_=in_, func=AF.Copy), reads, writes, cost=ecost("act", out))
            else:
                P.op(eng, lambda e: e.tensor_copy(out=out, in_=in_), reads, writes, cost=ecost(eng, out))

        def mm(out, lhsT, rhs, start, stop, reads, writes):
            n = max(64, fsz(rhs))
            c = n / 2400.0 * (4.0 if rhs.dtype == F32 else 1.0) + 0.03
            P.op("pe", lambda e: e.matmul(out, lhsT=lhsT, rhs=rhs, start=start, stop=stop), reads, writes, cost=c)

        def tr(out, in_, ident, reads, writes):
            P.op("pe", lambda e: e.transpose(out=out, in_=in_, identity=ident), reads, writes, cost=(0.35 if in_.dtype == F32 else 0.12))

        def dma(eng, out, in_, reads, writes):
            try:
                nb = int(out.nbytes())
            except Exception:
                nb = 65536
            P.dma(eng, lambda e: e.dma_start(out=out, in_=in_), reads, writes, cost=2.0 + nb / 250e3)

        def memset(eng, ap, val, writes):
            P.op(eng, lambda e: e.memset(ap, val), (), writes, cost=ecost(eng, ap))

        def recip(out, in_, reads, writes):
            P.op("dve", lambda e: e.reciprocal(out=out, in_=in_), reads, writes, cost=ecost("dve", out, 4.0))

        def rstd_from_ss(rs, ss, n, k=1):
            act(rs[:, 0:k], ss[:, 0:k], AF.Ln, [ss, eps_t], [rs], scale=1.0 / n, bias=eps_t[:, 0:1])
            act(rs[:, 0:k], rs[:, 0:k], AF.Exp, [rs], [rs], scale=-0.5)

        ident_b = P.sb([128, 128], BF16)
        ident_f = P.sb([128, 128], F32)
        ones_b = P.sb([128, 128], BF16)
        triu_b = P.sb([128, 128], BF16)
        iota_f = P.sb([128, 512], F32)
        iota16 = P.sb([128, 16], F32)
        pidx_i = P.sb([128, 1], I32)
        pidx_f = P.sb([128, 1], F32)
        eps_t = P.sb([128, 1], F32)
        onep_t = P.sb([128, 1], F32)
        halfpi_t = P.sb([128, 1], F32)
        rowtwo = P.sb([128, 2], F32)
        rowhalf = P.sb([128, 2], F32)
        rowq4 = P.sb([128, 4], F32)
        colq4 = P.sb([128, 4, 128], F32)
        tmpc = P.sb([128, 128], F32)
        tmpi = P.sb([128, 1], I32)
        ohs = P.sb([128, NS, 2, 16], BF16)
        rks = P.sb([128, NS, 2], F32)
        gts = P.sb([128, NS, 2], F32)
        base = P.sb([128, 16], F32)
        dest_i = P.sb([128, NS, 2], I32)
        blk_e = P.sb([128, NB], I32)
        psT = P.ps([128, 1024], BF16)
        psA = [P.ps([128, 512], F32) for _ in range(2)]
        psZ = [P.ps([128, 512], F32) for _ in range(2)]
        psM = [P.ps([128, 512], F32) for _ in range(3)]

        P.op("pool", lambda e: e.iota(iota_f[:], [[1, 512]], base=0, channel_multiplier=0, allow_small_or_imprecise_dtypes=True), (), [iota_f])
        P.op("pool", lambda e: e.iota(iota16[:], [[1, 16]], base=0, channel_multiplier=0, allow_small_or_imprecise_dtypes=True), (), [iota16])
        P.op("pool", lambda e: e.iota(pidx_i[:], [[1, 1]], base=0, channel_multiplier=1), (), [pidx_i])
        cp("dve", pidx_f[:], pidx_i[:], [pidx_i], [pidx_f])
        P.op("pool", lambda e: e.iota(tmpc[:], [[1, 128]], base=0, channel_multiplier=-1, allow_small_or_imprecise_dtypes=True), (), [tmpc])
        P.op("dve", lambda e: e.tensor_single_scalar(out=ident_f[:], in_=tmpc[:], scalar=0.0, op=ALU.is_equal), [tmpc], [ident_f])
        cp("dve", ident_b[:], ident_f[:], [ident_f], [ident_b])
        P.op("dve", lambda e: e.tensor_single_scalar(out=triu_b[:], in_=tmpc[:], scalar=0.0, op=ALU.is_gt), [tmpc], [triu_b])
        memset("pool", ones_b[:], 1.0, [ones_b])
        memset("pool", eps_t[:], EPS, [eps_t])
        memset("pool", onep_t[:], 1.0 + 1.2e-7, [onep_t])
        memset("pool", halfpi_t[:], float(np.pi / 2), [halfpi_t])
        P.op("dve", lambda e: e.tensor_single_scalar(out=tmpi[:], in_=pidx_i[:], scalar=4, op=ALU.arith_shift_right), [pidx_i], [tmpi])
        P.op("dve", lambda e: e.tensor_single_scalar(out=tmpi[:], in_=tmpi[:], scalar=1, op=ALU.bitwise_and), [tmpi], [tmpi])
        cp("dve", rowtwo[:, 1:2], tmpi[:], [tmpi], [rowtwo])
        ts("dve", rowtwo[:, 0:1], rowtwo[:, 1:2], -1.0, 1.0, ALU.mult, ALU.add, [rowtwo], [rowtwo])
        P.op("dve", lambda e: e.tensor_single_scalar(out=rowhalf[:, 1:2], in_=pidx_f[:], scalar=64.0, op=ALU.is_ge), [pidx_f], [rowhalf])
        ts("dve", rowhalf[:, 0:1], rowhalf[:, 1:2], -1.0, 1.0, ALU.mult, ALU.add, [rowhalf], [rowhalf])
        for q4 in range(4):
            ts("dve", rowq4[:, q4:q4 + 1], pidx_f[:], float(32 * q4), None, ALU.is_ge, None, [pidx_f], [rowq4])
            P.op("dve", lambda e, q4=q4: e.tensor_single_scalar(out=tmpc[:, 0:1], in_=pidx_f[:], scalar=float(32 * q4 + 32), op=ALU.is_lt), [pidx_f], [tmpc])
            tt("dve", rowq4[:, q4:q4 + 1], rowq4[:, q4:q4 + 1], tmpc[:, 0:1], ALU.mult, [rowq4, tmpc], [rowq4])
            memset("pool", colq4[:, q4, :], 0.0, [colq4])
            memset("pool", colq4[:, q4, 32 * q4:32 * q4 + 32], 1.0, [colq4])

        def load_layer_consts(l, C, stS):
            sb = P.sb

            def tmp(shape, dt):
                return P.sb(shape, dt, stack=stS)

            C["win"] = sb([128, 8, 1792], BF16)
            C["wout"] = sb([128, 8, 1024], BF16)
            C["gmix"] = sb([128, 1024], F32)
            C["gffn"] = sb([128, 1024], F32)
            C["gv"] = sb([128, 384], F32)
            C["goa"] = sb([128, 384], F32)
            C["gobc"] = sb([128, 5], F32)
            C["wr"] = sb([128, 8, 20], F32)
            C["rb"] = sb([128, 20], F32)
            C["wmT"] = sb([128, 6, 128], BF16)
            C["bsT"] = sb([128, 6], F32)
            C["cw"] = sb([128, 3, 4], F32)
            C["coef"] = sb([128, 3], F32)
            C["coef2"] = sb([128, 3], F32)
            C["coefh"] = sb([128, 3], F32)
            C["rgbh"] = sb([128, 3], F32)
            C["igbh"] = sb([128, 3], F32)
            C["glubh"] = sb([128, 2], F32)
            C["gobch"] = sb([128, 2], F32)
            C["rr"] = sb([128, 8], F32)
            C["cos"] = sb([128, 8, TT], F32)
            C["sin"] = sb([128, 8, TT], F32)
            C["blhs"] = sb([128, 8, 2, 128], BF16)
            C["clhs"] = sb([128, 8, 2, 128], BF16)
            C["dd"] = sb([128, 2], F32)
            C["glub"] = sb([128, 2], F32)
            C["gluw"] = sb([128, 2, 256], BF16)
            C["cb"] = sb([128, 3], F32)
            C["rgb"] = sb([128, 3], F32)
            C["igb"] = sb([128, 3], F32)
            C["lam"] = sb([128, 3], F32)
            C["bdr"] = sb([128, 3, 128], BF16)
            C["bdi"] = sb([128, 3, 128], BF16)
            dma("pool", C["win"][:], W["w_in"][l].rearrange("(k p) n -> p k n", p=128), (), [C["win"]])
            dma("pool", C["wout"][:], W["w_out"][l].rearrange("(k p) n -> p k n", p=128), (), [C["wout"]])
            dma("sp", C["gmix"][:], W["norm_mix_g"][l].partition_broadcast(128), (), [C["gmix"]])
            dma("sp", C["gffn"][:], W["norm_ffn_g"][l].partition_broadcast(128), (), [C["gffn"]])
            dma("sp", C["gv"][:], W["a_v_norm_g"][l].partition_broadcast(128), (), [C["gv"]])
            dma("sp", C["goa"][:], W["mix_out_norm_g"][l][0:384].partition_broadcast(128), (), [C["goa"]])
            dma("sp", C["gobc"][:], W["mix_out_norm_g"][l][384:1024].rearrange("(c p) -> p c", p=128), (), [C["gobc"]])
            dma("sp", C["wr"][:, :, 0:4], W["router_group_w"][l].rearrange("(k p) n -> p k n", p=128), (), [C["wr"]])
            for g in range(4):
                dma("sp", C["wr"][:, :, 4 + 4 * g:8 + 4 * g], W["router_expert_w"][l, g].rearrange("(k p) n -> p k n", p=128), (), [C["wr"]])
            dma("sp", C["rb"][:, 0:4], W["router_group_b"][l].partition_broadcast(128), (), [C["rb"]])
            dma("sp", C["rb"][:, 4:20], W["router_expert_b"][l].rearrange("g e -> (g e)").partition_broadcast(128), (), [C["rb"]])
            wa_nat = tmp([128, 6, 128], F32)
            dma("sp", wa_nat[:], W["a_spatial_w"][l].rearrange("h i j -> i h j"), (), [wa_nat])
            for h in range(6):
                pz = psM[h % 3]
                tr(pz[:, 0:128], wa_nat[:, h, :], ident_f[:], [wa_nat, ident_f], [pz])
                cp("act" if h % 2 else "dve", C["wmT"][:, h, :], pz[:, 0:128], [pz], [C["wmT"]])
            memset("pool", C["wmT"][64:128, :, 0:64], 0.0, [C["wmT"]])
            dma("sp", C["bsT"][:], W["a_spatial_b"][l].rearrange("h i -> i h"), (), [C["bsT"]])
            for k in range(4):
                dma("sp", C["cw"][:, :, k], W["b_conv_w"][l, k].rearrange("(c p) -> p c", p=128), (), [C["cw"]])
            for nm, key in (("cb", "b_conv_b"), ("rgb", "b_rg_b"), ("igb", "b_ig_b"), ("lam", "b_lambda")):
                dma("sp", C[nm][:], W[key][l].rearrange("(c p) -> p c", p=128), (), [C[nm]])
            for nm, key in (("bdr", "b_rg_w"), ("bdi", "b_ig_w")):
                memset("pool", C[nm][:], 0.0, [C[nm]])
                for c3 in range(3):
                    for two in range(2):
                        dma("pool", C[nm][64 * two:64 * two + 64, c3, 64 * two:64 * two + 64], W[key][l, 2 * c3 + two], (), [C[nm]])
            spt = tmp([128, 3], F32)
            act(spt[:], C["lam"][:], AF.Exp, [C["lam"]], [spt], scale=-1.0)
            act(spt[:], spt[:], AF.Ln, [spt], [spt], bias=1.0)
            ts("dve", C["coef"][:], spt[:], -8.0, None, ALU.mult, None, [spt], [C["coef"]])
            ts("dve", C["coef2"][:], spt[:], -16.0, None, ALU.mult, None, [spt], [C["coef2"]])
            ts("dve", C["coefh"][:], spt[:], -4.0, None, ALU.mult, None, [spt], [C["coefh"]])
            ts("dve", C["rgbh"][:], C["rgb"][:], 0.5, None, ALU.mult, None, [C["rgb"]], [C["rgbh"]])
            ts("dve", C["igbh"][:], C["igb"][:], 0.5, None, ALU.mult, None, [C["igb"]], [C["igbh"]])
            ts("dve", C["gobch"][:], C["gobc"][:, 3:5], 0.5, None, ALU.mult, None, [C["gobc"]], [C["gobch"]])
            are = tmp([128, 8], F32)
            aim = tmp([128, 8], F32)
            ldt = tmp([128, 8], F32)
            dma("sp", are[:], W["c_a_re"][l].rearrange("(q two) p -> (two p) q", two=2), (), [are])
            dma("sp", aim[:], W["c_a_im"][l].rearrange("(q two) p -> (two p) q", two=2), (), [aim])
            ldv = W["c_log_dt"][l].rearrange("(q two) -> two q", two=2)
            dma("sp", ldt[0:64, :], ldv[0].partition_broadcast(64), (), [ldt])
            dma("sp", ldt[64:128, :], ldv[1].partition_broadcast(64), (), [ldt])
            dtt = tmp([128, 8], F32)
            act(dtt[:], ldt[:], AF.Exp, [ldt], [dtt])
            th = tmp([128, 8], F32)
            tt("dve", th[:], aim[:], dtt[:], ALU.mult, [aim, dtt], [th])
            tt("dve", C["rr"][:], are[:], dtt[:], ALU.mult, [are, dtt], [C["rr"]])
            act(C["rr"][:], C["rr"][:], AF.Exp, [C["rr"]], [C["rr"]])
            ang = tmp([128, 8, TT], F32)
            angi = tmp([128, 8, TT], I32)
            thn = tmp([128, 8], F32)
            ts("dve", thn[:], th[:], 1.0 / TWO_PI, None, ALU.mult, None, [th], [thn])
            ts("dve", ang[:, 0, :], iota_f[:, 0:TT], 1.0, None, ALU.add, None, [iota_f], [ang])
            for q in range(1, 8):
                cp("pool", ang[:, q, :], ang[:, 0, :], [ang], [ang])
            tt("dve", ang[:], ang[:], thn[:].unsqueeze(2).to_broadcast([128, 8, TT]), ALU.mult, [ang, thn], [ang])
            cp("dve", angi[:], ang[:], [ang], [angi])
            cp("dve", C["cos"][:], angi[:], [angi], [C["cos"]])
            tt("dve", ang[:], ang[:], C["cos"][:], ALU.subtract, [ang, C["cos"]], [ang])
            act(C["sin"][:], ang[:], AF.Sin, [ang], [C["sin"]], scale=TWO_PI * (1 - 1e-6))
            act(ang[:], ang[:], AF.Abs, [ang], [ang])
            act(C["cos"][:], ang[:], AF.Sin, [ang], [C["cos"]], scale=-TWO_PI * (1 - 1e-6), bias=halfpi_t[:, 0:1])
            nr = tmp([128, 8], F32)
            ni = tmp([128, 8], F32)
            den = tmp([128, 8], F32)
            fr = tmp([128, 8], F32)
            fi = tmp([128, 8], F32)
            t8 = tmp([128, 8], F32)
            tt("dve", nr[:], C["rr"][:], C["cos"][:, :, 0], ALU.mult, [C["rr"], C["cos"]], [nr])
            ts("dve", nr[:], nr[:], -1.0, None, ALU.add, None, [nr], [nr])
            tt("dve", ni[:], C["rr"][:], C["sin"][:, :, 0], ALU.mult, [C["rr"], C["sin"]], [ni])
            tt("dve", den[:], are[:], are[:], ALU.mult, [are], [den])
            tt("dve", t8[:], aim[:], aim[:], ALU.mult, [aim], [t8])
            tt("dve", den[:], den[:], t8[:], ALU.add, [den, t8], [den])
            recip(den[:], den[:], [den], [den])
            tt("dve", fr[:], nr[:], are[:], ALU.mult, [nr, are], [fr])
            tt("dve", t8[:], ni[:], aim[:], ALU.mult, [ni, aim], [t8])
            tt("dve", fr[:], fr[:], t8[:], ALU.add, [fr, t8], [fr])
            tt("dve", fr[:], fr[:], den[:], ALU.mult, [fr, den], [fr])
            tt("dve", fi[:], ni[:], are[:], ALU.mult, [ni, are], [fi])
            tt("dve", t8[:], nr[:], aim[:], ALU.mult, [nr, aim], [t8])
            tt("dve", fi[:], fi[:], t8[:], ALU.subtract, [fi, t8], [fi])
            tt("dve", fi[:], fi[:], den[:], ALU.mult, [fi, den], [fi])
            bre = tmp([128, 8, 16], F32)
            bim = tmp([128, 8, 16], F32)
            dma("sp", bre[:], W["c_b_re"][l].rearrange("(q two) p c -> (two p) q c", two=2), (), [bre])
            dma("sp", bim[:], W["c_b_im"][l].rearrange("(q two) p c -> (two p) q c", two=2), (), [bim])
            bbr = tmp([128, 8, 16], F32)
            bbi = tmp([128, 8, 16], F32)
            t16 = tmp([128, 8, 16], F32)
            frb = fr[:].unsqueeze(2).to_broadcast([128, 8, 16])
            fib = fi[:].unsqueeze(2).to_broadcast([128, 8, 16])
            tt("dve", bbr[:], bre[:], frb, ALU.mult, [bre, fr], [bbr])
            tt("dve", t16[:], bim[:], fib, ALU.mult, [bim, fi], [t16])
            tt("dve", bbr[:], bbr[:], t16[:], ALU.subtract, [bbr, t16], [bbr])
            tt("dve", bbi[:], bim[:], frb, ALU.mult, [bim, fr], [bbi])
            tt("dve", t16[:], bre[:], fib, ALU.mult, [bre, fi], [t16])
            tt("dve", bbi[:], bbi[:], t16[:], ALU.add, [bbi, t16], [bbi])
            bpad = tmp([128, 8, 2, 16], F32)
            for ri, src in enumerate((bbr, bbi)):
                for two in range(2):
                    ts("dve", bpad[:, :, two, :], src[:], rowhalf[:, two:two + 1], None, ALU.mult, None, [src, rowhalf], [bpad])
                for hh in range(2):
                    pz = psM[(ri * 2 + hh) % 3]
                    tr(pz[:, 0:128], bpad[:, 4 * hh:4 * hh + 4, :, :].rearrange("p a b c -> p (a b c)"), ident_f[:], [bpad, ident_f], [pz])
                    for q4 in range(4):
                        ts("dve", C["blhs"][:, 4 * hh + q4, ri, :], pz[:, 0:128], rowq4[:, q4:q4 + 1], None, ALU.mult, None, [pz, rowq4], [C["blhs"]])
            cn = tmp([128, 2, 64], F32)
            cpad = tmp([128, 2, 2, 64], F32)
            for ri, key in enumerate(("c_c_re", "c_c_im")):
                for hh in range(2):
                    dma("sp", cn[:, hh, :], W[key][l, 8 * hh:8 * hh + 8].rearrange("g c p -> (g c) p"), (), [cn])
                for two in range(2):
                    ts("dve", cpad[:, :, two, :], cn[:], rowtwo[:, two:two + 1], None, ALU.mult, None, [cn, rowtwo], [cpad])
                for hh in range(2):
                    pz = psM[(ri * 2 + hh) % 3]
                    tr(pz[:, 0:128], cpad[:, hh, :, :].rearrange("p a b -> p (a b)"), ident_f[:], [cpad, ident_f], [pz])
                    for q4 in range(4):
                        if ri == 0:
                            tt("dve", C["clhs"][:, 4 * hh + q4, 0, :], pz[:, 0:128], colq4[:, q4, :], ALU.mult, [pz, colq4], [C["clhs"]])
                        else:
                            stt(C["clhs"][:, 4 * hh + q4, 1, :], pz[:, 0:128], -1.0, colq4[:, q4, :], ALU.mult, ALU.mult, [pz, colq4], [C["clhs"]])
            dma("sp", C["dd"][:], W["c_d"][l].rearrange("(c p) -> p c", p=128), (), [C["dd"]])
            dma("sp", C["glub"][:], W["c_glu_b"][l].rearrange("(c p) -> p c", p=128), (), [C["glub"]])
            dma("pool", C["gluw"][:], W["c_glu_w"][l].rearrange("(k p) n -> p k n", p=128), (), [C["gluw"]])
            ts("dve", C["glubh"][:], C["glub"][:], 0.5, None, ALU.mult, None, [C["glub"]], [C["glubh"]])

        def alloc_mixer_work(Wk):
            sb = P.sb
            Wk["h"] = [sb([128, NSUB, 1024], F32) for _ in range(2)]
            Wk["sqp"] = sb([128, 1024], BF16)
            Wk["sqa"] = sb([128, 384], BF16)
            Wk["sqe"] = sb([128, 1024], BF16)
            Wk["zero"] = sb([128, 1024], BF16)
            Wk["xn"] = [sb([128, 1024], BF16) for _ in range(2)]
            Wk["xnT"] = [sb([128, 8, TT], BF16) for _ in range(2)]
            Wk["yT"] = [sb([128, 8, TT], BF16) for _ in range(2)]
            for nm in ("ssp", "rsp", "ssa", "rsa", "ssa2", "rsa2", "sse", "rse"):
                Wk[nm] = [sb([128, 2], F32) for _ in range(2)]
            Wk["u"] = sb([128, 384], F32)
            Wk["vg"] = sb([128, 384], F32)
            Wk["v"] = sb([128, 384], BF16)
            Wk["yab"] = sb([128, 384], BF16)
            Wk["xbe"] = [sb([128, 3, TT + 3], F32) for _ in range(2)]
            Wk["xc"] = sb([128, TT], F32)
            Wk["xcb"] = sb([128, TT], BF16)
            Wk["rg"] = sb([128, TT], F32)
            Wk["ig"] = sb([128, TT], F32)
            Wk["aa"] = sb([128, TT], F32)
            Wk["bb"] = sb([128, TT], F32)
            Wk["hs"] = sb([128, TT], F32)
            Wk["yb"] = sb([128, 3, TT], F32)
            Wk["ybs"] = sb([128, 3, TT], BF16)
            Wk["hcar"] = sb([128, 3], F32)
            Wk["nrmB"] = sb([128, TT], F32)
            Wk["nrmC"] = sb([128, TT], F32)
            Wk["xcc"] = [sb([128, 2, TT], F32) for _ in range(2)]
            Wk["xccb"] = [sb([128, 2, TT], BF16) for _ in range(2)]
            for nm in ("ure", "uim", "mre", "mim", "gre", "gim"):
                Wk[nm] = [sb([128, TT], F32) for _ in range(2)]
            Wk["t1"] = [sb([128, TT], F32) for _ in range(2)]
            Wk["t2"] = [sb([128, TT], F32) for _ in range(2)]
            Wk["cst"] = [sb([128, 2], F32) for _ in range(2)]
            Wk["sre"] = [sb([128, TT], BF16) for _ in range(2)]
            Wk["sim"] = [sb([128, TT], BF16) for _ in range(2)]
            Wk["scar"] = sb([128, 8, 2], F32)
            Wk["yc"] = sb([128, 2, TT], F32)
            Wk["ycb"] = sb([128, 2, TT], BF16)
            Wk["ycs"] = sb([128, 2, TT], BF16)
            Wk["sg"] = sb([128, TT], F32)
            Wk["xf"] = [sb([128, 1024], BF16) for _ in range(2)]
            Wk["xf32"] = sb([128, 1024], F32)
            Wk["xfT"] = [sb([128, 4, 128], F32) for _ in range(2)]
            for nm, w_ in (("lg", 20), ("em", 16), ("em2", 16), ("pen", 4), ("gsel", 4), ("rank", 16)):
                Wk[nm] = [sb([128, NSUB, w_], F32) for _ in range(2)]
            Wk["sm"] = [sb([128, 9, NSUB], F32) for _ in range(2)]
            Wk["ohb"] = [sb([128, NSUB, 16], BF16) for _ in range(2)]

        def mixer_tile(l, ti, C, Wk, first_layer):
            hb = Wk["h"][ti % 2]
            t0 = ti * TT
            for s in range(NSUB):
                gs = ti * NSUB + s
                rows = slice(t0 + s * 128, t0 + (s + 1) * 128)
                dma("sp", hb[:, s, :], (x_d if first_layer else hmid_d)[rows, :], (), [hb])
            xnT = Wk["xnT"][ti % 2]
            ss, rs = Wk["ssp"][ti % 2], Wk["rsp"][ti % 2]
            for s in range(NSUB):
                jk = Wk["sqp"]
                act(jk[:], hb[:, s, :], AF.Square, [hb], [ss, jk], accum_out=ss[:, s:s + 1])
            rstd_from_ss(rs, ss, 1024, NSUB)
            for s in range(NSUB):
                xn = Wk["xn"][s % 2]
                stt(xn[:], hb[:, s, :], rs[:, s:s + 1], C["gmix"][:], ALU.mult, ALU.mult, [hb, rs, C["gmix"]], [xn])
                for k in range(8):
                    tr(psT[:, k * 128:(k + 1) * 128], xn[:, k * 128:(k + 1) * 128], ident_b[:], [xn, ident_b], [psT])
                cp("act", xnT[:, :, s * 128:(s + 1) * 128], psT[:].rearrange("p (k t) -> p k t", k=8), [psT], [xnT])
            import os as _os
            _cut = float(_os.environ.get("K_CUT", "9"))
            if _cut <= 1:
                return
            yT = Wk["yT"][ti % 2]
            for s in range(NSUB):
                ssa, rsa = Wk["ssa"][s % 2], Wk["rsa"][s % 2]
                for half in range(2):
                    for k in range(8):
                        mm(psA[half][:, 0:384], xnT[:, k, s * 128:(s + 1) * 128], C["win"][:, k, 384 * half:384 * half + 384],
                           k == 0, k == 7, [xnT, C["win"]], [psA[half]])
                act(Wk["u"][:], psA[0][:, 0:384], AF.Gelu_apprx_tanh, [psA[0]], [Wk["u"]])
                act(Wk["vg"][:], psA[1][:, 0:384], AF.Gelu_apprx_tanh, [psA[1]], [Wk["vg"]])
                jk = Wk["sqa"]
                act(jk[:], Wk["vg"][:], AF.Square, [Wk["vg"]], [ssa, jk], accum_out=ssa[:, 0:1])
                rstd_from_ss(rsa, ssa, 384)
                stt(Wk["v"][:], Wk["vg"][:], rsa[:, 0:1], C["gv"][:], ALU.mult, ALU.mult, [Wk["vg"], rsa, C["gv"]], [Wk["v"]])
                for hh in range(6):
                    mm(psA[1][:, 64 * hh:64 * hh + 64], C["wmT"][:, hh, :], Wk["v"][:, 64 * hh:64 * hh + 64], True, True,
                       [C["wmT"], Wk["v"]], [psA[1]])
                ya = Wk["vg"]
                tt("dve", ya[:].rearrange("p (h d) -> p h d", h=6), psA[1][:, 0:384].rearrange("p (h d) -> p h d", h=6),
                   C["bsT"][:].unsqueeze(2).to_broadcast([128, 6, 64]), ALU.add, [psA[1], C["bsT"]], [ya])
                tt("dve", ya[:], ya[:], Wk["u"][:], ALU.mult, [ya, Wk["u"]], [ya])
                if dbg and "y_a" in dbg_d:
                    dma("sp", dbg_d["y_a"][t0 + s * 128:t0 + (s + 1) * 128, :], ya[:], [ya], ())
                ssa2, rsa2 = Wk["ssa2"][s % 2], Wk["rsa2"][s % 2]
                jk = Wk["sqa"]
                act(jk[:], ya[:], AF.Square, [ya], [ssa2, jk], accum_out=ssa2[:, 0:1])
                rstd_from_ss(rsa2, ssa2, 384)
                stt(Wk["yab"][:], ya[:], rsa2[:, 0:1], C["goa"][:], ALU.mult, ALU.mult, [ya, rsa2, C["goa"]], [Wk["yab"]])
                for k in range(3):
                    tr(psT[:, k * 128:(k + 1) * 128], Wk["yab"][:, k * 128:(k + 1) * 128], ident_b[:], [Wk["yab"], ident_b], [psT])
                cp("act", yT[:, 0:3, s * 128:(s + 1) * 128], psT[:, 0:384].rearrange("p (k t) -> p k t", k=3), [psT], [yT])

            if _cut <= 2:
                return

            def zchunk(col0, pz, c0=0):
                for k in range(8):
                    mm(pz[:, c0:c0 + TT], C["win"][:, k, col0:col0 + 128], xnT[:, k, :], k == 0, k == 7, [C["win"], xnT], [pz])

            psB0, psB1 = psZ[0], psM[0]
            psC0, psC1, psC2 = psZ[1], psM[1], psM[2]
            xbe = Wk["xbe"][ti % 2]
            xbp = Wk["xbe"][(ti + 1) % 2]
            for c3 in range(3):
                zchunk(768 + 128 * c3, psB0)
                cp("act", xbe[:, c3, 3:3 + TT], psB0[:, 0:TT], [psB0], [xbe])
            if ti == 0:
                memset("pool", xbe[:, :, 0:3], 0.0, [xbe])
                memset("pool", Wk["hcar"][:], 0.0, [Wk["hcar"]])
                memset("pool", Wk["scar"][:], 0.0, [Wk["scar"]])
            else:
                cp("pool", xbe[:, :, 0:3], xbp[:, :, TT:TT + 3], [xbp], [xbe])
            for c3 in range(3):
                xc, xcb = Wk["xc"], Wk["xcb"]
                act(xc[:], xbe[:, c3, 3:3 + TT], AF.Identity, [xbe, C["cw"], C["cb"]], [xc], scale=C["cw"][:, c3, 3:4], bias=C["cb"][:, c3:c3 + 1])
                for k in range(3):
                    stt(xc[:], xbe[:, c3, k:k + TT], C["cw"][:, c3, k:k + 1], xc[:], ALU.mult, ALU.add, [xbe, C["cw"], xc], [xc])
                cp("act", xcb[:], xc[:], [xc], [xcb])
                mm(psB1[:, 0:TT], C["bdr"][:, c3, :], xcb[:], True, True, [C["bdr"], xcb], [psB1])
                mm(psB1[:, TT:2 * TT], C["bdi"][:, c3, :], xcb[:], True, True, [C["bdi"], xcb], [psB1])
                act(Wk["rg"][:], psB1[:, 0:TT], AF.Tanh, [psB1, C["rgbh"]], [Wk["rg"]], scale=0.5, bias=C["rgbh"][:, c3:c3 + 1])
                act(Wk["ig"][:], psB1[:, TT:2 * TT], AF.Tanh, [psB1, C["igbh"]], [Wk["ig"]], scale=0.5, bias=C["igbh"][:, c3:c3 + 1])
                act(Wk["aa"][:], Wk["rg"][:], AF.Exp, [Wk["rg"], C["coefh"]], [Wk["aa"]], scale=C["coefh"][:, c3:c3 + 1], bias=C["coefh"][:, c3:c3 + 1])
                act(Wk["bb"][:], Wk["rg"][:], AF.Exp, [Wk["rg"], C["coef"]], [Wk["bb"]], scale=C["coef"][:, c3:c3 + 1], bias=C["coef"][:, c3:c3 + 1])
                act(Wk["bb"][:], Wk["bb"][:], AF.Ln, [Wk["bb"], onep_t], [Wk["bb"]], scale=-1.0, bias=onep_t[:, 0:1])
                act(Wk["bb"][:], Wk["bb"][:], AF.Exp, [Wk["bb"]], [Wk["bb"]], scale=0.5)
                ts("pool", Wk["ig"][:], Wk["ig"][:], 1.0, None, ALU.add, None, [Wk["ig"]], [Wk["ig"]])
                tt("pool", Wk["ig"][:], Wk["ig"][:], xc[:], ALU.mult, [Wk["ig"], xc], [Wk["ig"]])
                stt(Wk["bb"][:], Wk["bb"][:], 0.5, Wk["ig"][:], ALU.mult, ALU.mult, [Wk["bb"], Wk["ig"]], [Wk["bb"]])
                scan(Wk["hs"][:], Wk["aa"][:], Wk["bb"][:], Wk["hcar"][:, c3:c3 + 1], [Wk["aa"], Wk["bb"], Wk["hcar"]], [Wk["hs"]])
                cp("act", Wk["hcar"][:, c3:c3 + 1], Wk["hs"][:, TT - 1:TT], [Wk["hs"]], [Wk["hcar"]])
                zchunk(1152 + 128 * c3, psB0)
                gg = Wk["rg"]
                act(gg[:], psB0[:, 0:TT], AF.Gelu_apprx_tanh, [psB0], [gg])
                tt("dve", Wk["yb"][:, c3, :], Wk["hs"][:], gg[:], ALU.mult, [Wk["hs"], gg], [Wk["yb"]])
                act(Wk["ybs"][:, c3, :], Wk["yb"][:, c3, :], AF.Square, [Wk["yb"]], [Wk["ybs"]])
            if dbg and "y_b" in dbg_d:
                for c3 in range(3):
                    dma("sp", dbg_d["y_b"][c3 * 128:(c3 + 1) * 128, t0:t0 + TT], Wk["yb"][:, c3, :], [Wk["yb"]], ())
            for c3 in range(3):
                mm(psB1[:, 0:TT], ones_b[:], Wk["ybs"][:, c3, :], c3 == 0, c3 == 2, [ones_b, Wk["ybs"]], [psB1])
            nrm = Wk["nrmB"]
            act(nrm[:], psB1[:, 0:TT], AF.Ln, [psB1, eps_t], [nrm], scale=1.0 / 384, bias=eps_t[:, 0:1])
            act(nrm[:], nrm[:], AF.Exp, [nrm], [nrm], scale=-0.5)
            for c3 in range(3):
                stt(yT[:, 3 + c3, :], Wk["yb"][:, c3, :], C["gobc"][:, c3:c3 + 1], nrm[:], ALU.mult, ALU.mult,
                    [Wk["yb"], C["gobc"], nrm], [yT])
            if _cut <= 3:
                return
            xcc, xccb = Wk["xcc"][ti % 2], Wk["xccb"][ti % 2]
            for hh in range(2):
                zchunk(1536 + 128 * hh, psC0)
                cp("act", xcc[:, hh, :], psC0[:, 0:TT], [psC0], [xcc])
                cp("dve", xccb[:, hh, :], psC0[:, 0:TT], [psC0], [xccb])
            for hh in range(2):
                for q4 in range(4):
                    q = 4 * hh + q4
                    j = q % 2
                    ure, uim, mre, mim, gre, gim, t1, t2 = (Wk[nm][j] for nm in ("ure", "uim", "mre", "mim", "gre", "gim", "t1", "t2"))
                    sre, sim = Wk["sre"][j], Wk["sim"][j]
                    cst = Wk["cst"][j]
                    mm(psC1[:, 0:TT], C["blhs"][:, q, 0, :], xccb[:, hh, :], True, True, [C["blhs"], xccb], [psC1])
                    mm(psC1[:, TT:2 * TT], C["blhs"][:, q, 1, :], xccb[:, hh, :], True, True, [C["blhs"], xccb], [psC1])
                    cq, sq_ = C["cos"][:, q, :], C["sin"][:, q, :]
                    L = TT - 1
                    cp("act", ure[:], psC1[:, 0:TT], [psC1], [ure])
                    cp("act", uim[:], psC1[:, TT:2 * TT], [psC1], [uim])
                    tt("pool", mre[:], ure[:], cq, ALU.mult, [ure, C["cos"]], [mre])
                    tt("pool", t1[:], uim[:], sq_, ALU.mult, [uim, C["sin"]], [t1])
                    tt("dve", mre[:], mre[:], t1[:], ALU.add, [mre, t1], [mre])
                    tt("pool", mim[:], uim[:], cq, ALU.mult, [uim, C["cos"]], [mim])
                    tt("pool", t2[:], ure[:], sq_, ALU.mult, [ure, C["sin"]], [t2])
                    tt("dve", mim[:], mim[:], t2[:], ALU.subtract, [mim, t2], [mim])
                    rb_ = C["rr"][:, q:q + 1].to_broadcast([128, TT])
                    scan(gre[:], rb_, mre[:], Wk["scar"][:, q, 0:1], [C["rr"], mre, Wk["scar"]], [gre])
                    scan(gim[:], rb_, mim[:], Wk["scar"][:, q, 1:2], [C["rr"], mim, Wk["scar"]], [gim])
                    tt("dve", cst[:, 0:1], gim[:, L:L + 1], C["sin"][:, q, L:L + 1], ALU.mult, [gim, C["sin"]], [cst])
                    stt(Wk["scar"][:, q, 0:1], gre[:, L:L + 1], C["cos"][:, q, L:L + 1], cst[:, 0:1], ALU.mult, ALU.subtract,
                        [gre, C["cos"], cst], [Wk["scar"]])
                    tt("dve", cst[:, 1:2], gim[:, L:L + 1], C["cos"][:, q, L:L + 1], ALU.mult, [gim, C["cos"]], [cst])
                    stt(Wk["scar"][:, q, 1:2], gre[:, L:L + 1], C["sin"][:, q, L:L + 1], cst[:, 1:2], ALU.mult, ALU.add,
                        [gre, C["sin"], cst], [Wk["scar"]])
                    tt("pool", t1[:], gre[:], cq, ALU.mult, [gre, C["cos"]], [t1])
                    tt("pool", t2[:], gim[:], sq_, ALU.mult, [gim, C["sin"]], [t2])
                    tt("dve", sre[:], t1[:], t2[:], ALU.subtract, [t1, t2], [sre])
                    tt("pool", t1[:], gre[:], sq_, ALU.mult, [gre, C["sin"]], [t1])
                    tt("pool", t2[:], gim[:], cq, ALU.mult, [gim, C["cos"]], [t2])
                    tt("dve", sim[:], t1[:], t2[:], ALU.add, [t1, t2], [sim])
                    mm(psC2[:, 0:TT], C["clhs"][:, q, 0, :], sre[:], q4 == 0, False, [C["clhs"], sre], [psC2])
                    mm(psC2[:, 0:TT], C["clhs"][:, q, 1, :], sim[:], False, q4 == 3, [C["clhs"], sim], [psC2])
                if _cut <= 3.7:
                    continue
                stt(Wk["yc"][:, hh, :], xcc[:, hh, :], C["dd"][:, hh:hh + 1], psC2[:, 0:TT], ALU.mult, ALU.add,
                    [xcc, C["dd"], psC2], [Wk["yc"]])
                act(Wk["yc"][:, hh, :], Wk["yc"][:, hh, :], AF.Gelu_apprx_tanh, [Wk["yc"]], [Wk["yc"]])
                cp("act", Wk["ycb"][:, hh, :], Wk["yc"][:, hh, :], [Wk["yc"]], [Wk["ycb"]])
            if _cut <= 3.8:
                return
            for ho in range(2):
                for hi in range(2):
                    mm(psC0[:, 0:TT], C["gluw"][:, hi, 128 * ho:128 * ho + 128], Wk["ycb"][:, hi, :], hi == 0, hi == 1, [C["gluw"], Wk["ycb"]], [psC0])
                act(Wk["sg"][:], psC0[:, 0:TT], AF.Tanh, [psC0, C["glubh"]], [Wk["sg"]], scale=0.5, bias=C["glubh"][:, ho:ho + 1])
                stt(Wk["yc"][:, ho, :], Wk["sg"][:], 1.0, Wk["yc"][:, ho, :], ALU.add, ALU.mult, [Wk["yc"], Wk["sg"]], [Wk["yc"]])
                act(Wk["ycs"][:, ho, :], Wk["yc"][:, ho, :], AF.Square, [Wk["yc"]], [Wk["ycs"]], scale=0.5)
            if dbg and "y_c" in dbg_d:
                for hh in range(2):
                    dma("sp", dbg_d["y_c"][hh * 128:(hh + 1) * 128, t0:t0 + TT], Wk["yc"][:, hh, :], [Wk["yc"]], ())
            for hh in range(2):
                mm(psC0[:, 0:TT], ones_b[:], Wk["ycs"][:, hh, :], hh == 0, hh == 1, [ones_b, Wk["ycs"]], [psC0])
            nrm = Wk["nrmC"]
            act(nrm[:], psC0[:, 0:TT], AF.Ln, [psC0, eps_t], [nrm], scale=1.0 / 256, bias=eps_t[:, 0:1])
            act(nrm[:], nrm[:], AF.Exp, [nrm], [nrm], scale=-0.5)
            for hh in range(2):
                stt(yT[:, 6 + hh, :], Wk["yc"][:, hh, :], C["gobch"][:, hh:hh + 1], nrm[:], ALU.mult, ALU.mult,
                    [Wk["yc"], C["gobch"], nrm], [yT])
            if _cut <= 4:
                return
            for s in range(NSUB):
                gs = ti * NSUB + s
                rows = slice(t0 + s * 128, t0 + (s + 1) * 128)
                for half in range(2):
                    for k in range(8):
                        mm(psA[half][:], yT[:, k, s * 128:(s + 1) * 128], C["wout"][:, k, 512 * half:512 * half + 512], k == 0, k == 7,
                           [yT, C["wout"]], [psA[half]])
                    tt("dve", hb[:, s, 512 * half:512 * half + 512], hb[:, s, 512 * half:512 * half + 512], psA[half][:], ALU.add,
                       [hb, psA[half]], [hb])
                dma("sp", hmid_d[rows, :], hb[:, s, :], [hb], ())
                sse, rse = Wk["sse"][gs % 2], Wk["rse"][gs % 2]
                jk = Wk["sqe"]
                act(jk[:], hb[:, s, :], AF.Square, [hb], [sse, jk], accum_out=sse[:, 0:1])
                rstd_from_ss(rse, sse, 1024)
                xf = Wk["xf"][gs % 2]
                stt(Wk["xf32"][:], hb[:, s, :], rse[:, 0:1], C["gffn"][:], ALU.mult, ALU.mult, [hb, rse, C["gffn"]], [Wk["xf32"]])
                cp("act", xf[:], Wk["xf32"][:], [Wk["xf32"]], [xf])
                dma("sp", xf_d[rows, :], xf[:], [xf], ())
                route_logits(gs, s, C, Wk, psZ[0])
            route_tile(ti, C, Wk, psZ[0])

        def route_logits(gs, s_, C, Wk, pl):
            xf32 = Wk["xf32"]
            for half in range(2):
                xfT = Wk["xfT"][half]
                for k4 in range(4):
                    k = 4 * half + k4
                    tr(psA[half][:, k4 * 128:(k4 + 1) * 128], xf32[:, k * 128:(k + 1) * 128], ident_f[:], [xf32, ident_f], [psA[half]])
                cp("act", xfT[:], psA[half][:].rearrange("p (k t) -> p k t", k=4), [psA[half]], [xfT])
            for half in range(2):
                xfT = Wk["xfT"][half]
                for k4 in range(4):
                    k = 4 * half + k4
                    mm(pl[:, TT + 32 * s_:TT + 32 * s_ + 20], xfT[:, k4, :], C["wr"][:, k, :], k == 0, k == 7, [xfT, C["wr"]], [pl])

        def route_tile(ti, C, Wk, pl):
            J = NSUB
            g0 = ti * NSUB
            j = ti % 2
            lg, sm, em, em2, pen, gsel, rank, ohb = (Wk[nm][j] for nm in ("lg", "sm", "em", "em2", "pen", "gsel", "rank", "ohb"))
            plv = pl[:, TT:TT + 32 * J].rearrange("p (j c) -> p j c", j=J)
            tt("dve", lg[:], plv[:, :, 0:20], C["rb"][:].unsqueeze(1).to_broadcast([128, J, 20]), ALU.add, [pl, C["rb"]], [lg])
            red(sm[:, 0, :], lg[:, :, 0:4], ALU.max, [lg], [sm])
            tt("dve", em2[:, :, 0:4], lg[:, :, 0:4], sm[:, 0, :].unsqueeze(2).to_broadcast([128, J, 4]), ALU.subtract, [lg, sm], [em2])
            act(em2[:, :, 0:4], em2[:, :, 0:4], AF.Exp, [em2], [em2])
            red(sm[:, 1, :], em2[:, :, 0:4], ALU.add, [em2], [sm])
            recip(sm[:, 2, :], sm[:, 1, :], [sm], [sm])
            tt("dve", gsel[:], lg[:, :, 0:4], sm[:, 0, :].unsqueeze(2).to_broadcast([128, J, 4]), ALU.is_ge, [lg, sm], [gsel])
            ts("dve", pen[:], gsel[:], -1.0, BIG, ALU.add, ALU.mult, [gsel], [pen])
            for jj in range(J):
                tt("dve", em[:, jj, :].rearrange("p (g e) -> p g e", g=4), lg[:, jj, 4:20].rearrange("p (g e) -> p g e", g=4),
                   pen[:, jj, :].unsqueeze(2).to_broadcast([128, 4, 4]), ALU.add, [lg, pen], [em])
            red(sm[:, 3, :], em[:], ALU.max, [em], [sm])
            oh1 = ohs[:, g0:g0 + J, 0, :]
            oh2 = ohs[:, g0:g0 + J, 1, :]
            tt("dve", oh1, em[:], sm[:, 3, :].unsqueeze(2).to_broadcast([128, J, 16]), ALU.is_ge, [em, sm], [ohs])
            stt(em2[:], oh1, -BIG, em[:], ALU.mult, ALU.add, [ohs, em], [em2])
            red(sm[:, 4, :], em2[:], ALU.max, [em2], [sm])
            tt("dve", oh2, em2[:], sm[:, 4, :].unsqueeze(2).to_broadcast([128, J, 16]), ALU.is_ge, [em2, sm], [ohs])
            tt("dve", sm[:, 5, :], sm[:, 4, :], sm[:, 3, :], ALU.subtract, [sm], [sm])
            act(sm[:, 6, :], sm[:, 5, :], AF.Exp, [sm], [sm])
            ts("dve", sm[:, 7, :], sm[:, 6, :], 1.0, None, ALU.add, None, [sm], [sm])
            recip(sm[:, 7, :], sm[:, 7, :], [sm], [sm])
            tt("dve", sm[:, 8, :], sm[:, 6, :], sm[:, 7, :], ALU.mult, [sm], [sm])
            tt("dve", gts[:, g0:g0 + J, 0], sm[:, 7, :], sm[:, 2, :], ALU.mult, [sm], [gts])
            tt("dve", gts[:, g0:g0 + J, 1], sm[:, 8, :], sm[:, 2, :], ALU.mult, [sm], [gts])
            tt("dve", ohb[:], oh1, oh2, ALU.add, [ohs], [ohb])
            o1, o2 = TT + 32 * J, TT + 32 * J + 16 * J
            for jj in range(J):
                mm(pl[:, o1 + 16 * jj:o1 + 16 * jj + 16], triu_b[:], ohb[:, jj, :], True, True, [triu_b, ohb], [pl])
                mm(pl[:, o2 + 16 * jj:o2 + 16 * jj + 16], ones_b[:], ohb[:, jj, :], True, True, [ones_b, ohb], [pl])
            if ti == 0:
                memset("pool", base[:], 0.0, [base])
            prv = pl[:, o1:o1 + 16 * J].rearrange("p (j e) -> p j e", j=J)
            tt("dve", rank[:], prv, base[:].unsqueeze(1).to_broadcast([128, J, 16]), ALU.add, [pl, base], [rank])
            for jj in range(J):
                if jj >= 1:
                    for j0 in range(jj):
                        tt("dve", rank[:, jj, :], rank[:, jj, :], pl[:, o2 + 16 * j0:o2 + 16 * j0 + 16], ALU.add, [rank, pl], [rank])
            for jj in range(J):
                tt("dve", base[:], base[:], pl[:, o2 + 16 * jj:o2 + 16 * jj + 16], ALU.add, [base, pl], [base])
            for k in range(2):
                tt("dve", em2[:], ohs[:, g0:g0 + J, k, :], rank[:], ALU.mult, [ohs, rank], [em2])
                red(rks[:, g0:g0 + J, k], em2[:], ALU.add, [em2], [rks])

        def combine_phase(final):
            sb = P.sb
            hbs = [sb([128, 1024], F32) for _ in range(6)]
            yks = [sb([128, 1024], F32) for _ in range(8)]
            if final:
                gfin = sb([128, 1024], F32)
                dma("sp", gfin[:], W["final_norm_g"].partition_broadcast(128), (), [gfin])
                obs = [sb([128, 1024], F32) for _ in range(2)]
                junk = sb([128, 1024], BF16)
                sss = [sb([128, 1], F32) for _ in range(2)]
                rss = [sb([128, 1], F32) for _ in range(2)]
            for gs in range(NS):
                hb = hbs[gs % 6]
                rows = slice(gs * 128, (gs + 1) * 128)
                dma("sp", hb[:], hmid_d[rows, :], (), [hb])
                for k in range(2):
                    yk = yks[(2 * gs + k) % 8]
                    P.dma("pool", lambda e, k=k, yk=yk, gs=gs: e.indirect_dma_start(out=yk[:], out_offset=None, in_=ys_d,
                                                                             in_offset=bass.IndirectOffsetOnAxis(dest_i[:, gs, k:k + 1], 0)),
                          [dest_i], [yk], cost=6.0)
                    stt(hb[:], yk[:], gts[:, gs, k:k + 1], hb[:], ALU.mult, ALU.add, [yk, gts, hb], [hb])
                if not final:
                    dma("sp", hmid_d[rows, :], hb[:], [hb], ())
                else:
                    ss, rs, ob = sss[gs % 2], rss[gs % 2], obs[gs % 2]
                    act(junk[:], hb[:], AF.Square, [hb], [ss, junk], accum_out=ss[:, 0:1])
                    rstd_from_ss(rs, ss, 1024)
                    stt(ob[:], hb[:], rs[:, 0:1], gfin[:], ALU.mult, ALU.mult, [hb, rs, gfin], [ob])
                    dma("sp", out_d[rows, :], ob[:], [ob], ())

        class TTv:
            def __init__(self, base_t, sl):
                self.b = base_t
                self.sl = sl
                self.r = base_t.r

            def __getitem__(self, k):
                return self.b.t[:, self.sl]

        def dispatch_phase(Wd):
            sb = P.sb
            padded = sb([128, 16], F32)
            pend = sb([128, 16], F32)
            pstart = sb([128, 16], F32)
            ti_ = sb([128, 16], I32)
            ts("dve", padded[:], base[:], float(EB - 1), 1.0 / EB, ALU.add, ALU.mult, [base], [padded])
            ts("dve", padded[:], padded[:], -0.5 + 1.0 / (4 * EB), None, ALU.add, None, [padded], [padded])
            cp("dve", ti_[:], padded[:], [padded], [ti_])
            cp("dve", padded[:], ti_[:], [ti_], [padded])
            ts("dve", padded[:], padded[:], float(EB), None, ALU.mult, None, [padded], [padded])
            onesf = sb([128, 16], F32)
            memset("pool", onesf[:], 1.0, [onesf])
            P.op("dve", lambda e: e.tensor_tensor_scan(out=pend[:], data0=onesf[:], data1=padded[:], initial=0.0, op0=ALU.mult, op1=ALU.add),
                 [onesf, padded], [pend])
            tt("dve", pstart[:], pend[:], padded[:], ALU.subtract, [pend, padded], [pstart])
            big = sb([128, NS * 2, 16], F32)
            dsum = sb([128, NS * 2], F32)
            tt("dve", big[:], ohs[:].rearrange("p s k e -> p (s k) e"), pstart[:].unsqueeze(1).to_broadcast([128, NS * 2, 16]), ALU.mult,
               [ohs, pstart], [big])
            P.op("dve", lambda e: e.tensor_reduce(out=dsum[:], in_=big[:], axis=AX.X, op=ALU.add), [big], [dsum])
            tt("dve", dsum[:], dsum[:], rks[:].rearrange("p s k -> p (s k)"), ALU.add, [dsum, rks], [dsum])
            cp("dve", dest_i[:].rearrange("p s k -> p (s k)"), dsum[:], [dsum], [dest_i])
            bb_ = sb([128, NB, 16], F32)
            bsum = sb([128, NB], F32)
            thr = sb([128, NB], F32)
            ts("dve", thr[:], iota_f[:, 0:NB], float(EB), None, ALU.mult, None, [iota_f], [thr])
            tt("dve", bb_[:], pend[:].unsqueeze(1).to_broadcast([128, NB, 16]), thr[:].unsqueeze(2).to_broadcast([128, NB, 16]), ALU.is_le,
               [pend, thr], [bb_])
            P.op("dve", lambda e: e.tensor_reduce(out=bsum[:], in_=bb_[:], axis=AX.X, op=ALU.add), [bb_], [bsum])
            ts("dve", bsum[:], bsum[:], 15.0, None, ALU.min, None, [bsum], [bsum])
            cp("dve", blk_e[:], bsum[:], [bsum], [blk_e])
            xt = [sb([128, 1024], BF16) for _ in range(8)]
            for gs in range(NS):
                xb_ = xt[gs % 8]
                dma("sp", xb_[:], xf_d[gs * 128:(gs + 1) * 128, :], [r_xf], [xb_])
                for k in range(2):
                    P.dma("pool", lambda e, gs=gs, k=k, xb_=xb_: e.indirect_dma_start(
                        out=xs_d, out_offset=bass.IndirectOffsetOnAxis(dest_i[:, gs, k:k + 1], 0), in_=xb_[:], in_offset=None),
                        [xb_, dest_i], ())

        def expert_phase(l):
            sb = P.sb
            wg = [sb([128, 8, 512], BF16) for _ in range(2)]
            wu = [sb([128, 8, 512], BF16) for _ in range(2)]
            wd = [sb([128, 4, 1024], BF16) for _ in range(2)]
            xsb = [sb([128, 4, 1024], BF16) for _ in range(2)]
            xsTs = [sb([128, 8, EB], BF16) for _ in range(2)]
            sgts = [sb([128, EB], F32) for _ in range(2)]
            hTs = [sb([128, 4, EB], BF16) for _ in range(2)]
            yo = [sb([128, 1024], F32) for _ in range(4)]

            def wload(e, b, dst, src, pat):
                if "r" not in REGS:
                    REGS["r"] = e.alloc_register("ereg")
                rg = REGS["r"]
                id0 = nc.next_id()
                e.reg_load(rg, blk_e[0:1, b:b + 1])
                v = e.snap(rg, min_val=0, max_val=NEXP - 1)
                ins = e.dma_start(out=dst[:], in_=src[l][bass.ds(v, 1), :, :].rearrange(pat, p=128))
                id1 = nc.next_id()
                for i in range(id0, id1 + 1):
                    for nm in ("Pool_tmp_%d" % i, "Pool_Pool_ereg_snap_%d" % i):
                        try:
                            e.free_register(bass.RegisterHandle(nm, rg.engine))
                        except ValueError:
                            pass
                return ins

            for b in range(NB):
                i2 = b % 2
                xsT, hT = xsTs[i2], hTs[i2]
                P.dma("pool", lambda e, b=b, i2=i2: wload(e, b, wg[i2], W["expert_w_gate"], "o (k p) n -> p (o k) n"), [blk_e], [wg[i2]])
                P.dma("pool", lambda e, b=b, i2=i2: wload(e, b, wu[i2], W["expert_w_up"], "o (k p) n -> p (o k) n"), [blk_e], [wu[i2]])
                P.dma("pool", lambda e, b=b, i2=i2: wload(e, b, wd[i2], W["expert_w_down"], "o (k p) n -> p (o k) n"), [blk_e], [wd[i2]])
                dma("sp", xsb[i2][:], xs_d[b * EB:(b + 1) * EB, :].rearrange("(s p) d -> p s d", p=128), [r_xs], [xsb[i2]])
                for s in range(4):
                    if s % 2 == 0:
                        pt_ap, pt_res = psT[:], psT
                    else:
                        pt_ap, pt_res = psM[2][:].bitcast(BF16), psM[2]
                    for k in range(8):
                        tr(pt_ap[:, k * 128:(k + 1) * 128], xsb[i2][:, s, k * 128:(k + 1) * 128], ident_b[:], [xsb[i2], ident_b], [pt_res])
                    cp("act" if s % 2 else "dve", xsT[:, :, s * 128:(s + 1) * 128], pt_ap.rearrange("p (k t) -> p k t", k=8), [pt_res], [xsT])
                for f in range(4):
                    pg, pu = (psZ[0], psZ[1]) if f % 2 == 0 else (psM[0], psM[1])
                    sgt = sgts[f % 2]
                    for k in range(8):
                        mm(pg[:], wg[i2][:, k, 128 * f:128 * f + 128], xsT[:, k, :], k == 0, k == 7, [wg[i2], xsT], [pg])
                    for k in range(8):
                        mm(pu[:], wu[i2][:, k, 128 * f:128 * f + 128], xsT[:, k, :], k == 0, k == 7, [wu[i2], xsT], [pu])
                    act(sgt[:], pg[:], AF.Silu, [pg], [sgt])
                    tt("dve", hT[:, f, :], sgt[:], pu[:], ALU.mult, [sgt, pu], [hT])
                for s in range(4):
                    yb_ = yo[s % 4]
                    for half in range(2):
                        py = psA[half]
                        for f in range(4):
                            mm(py[:], hT[:, f, s * 128:(s + 1) * 128], wd[i2][:, f, 512 * half:512 * half + 512], f == 0, f == 3, [hT, wd[i2]], [py])
                        cp("act" if half else "dve", yb_[:, 512 * half:512 * half + 512], py[:], [py], [yb_])
                    dma("sp", ys_d[b * EB + s * 128:b * EB + (s + 1) * 128, :], yb_[:], [yb_], ())

        P.phase_end()
        for l in range(n_layers):
            with ExitStack() as stL:
                P.stack = stL
                C, Wk = {}, {}
                with ExitStack() as stS:
                    load_layer_consts(l, C, stS)
                    P.phase_end()
                alloc_mixer_work(Wk)
                memset("pool", Wk["zero"][:], 0.0, [Wk["zero"]])
                zrows = list(range(0, NB * EB, 128))
                zper = -(-len(zrows) // NT)
                for ti in range(NT):
                    mixer_tile(l, ti, C, Wk, first_layer=(l == 0))
                    for r0 in zrows[ti * zper:(ti + 1) * zper]:
                        dma("sp", xs_d[r0:r0 + 128, :], Wk["zero"][:], [Wk["zero"]], ())
                P.phase_end()
            import os as _os
            if _os.environ.get("K_STOP") == "mixer":
                break
            with ExitStack() as stD:
                P.stack = stD
                dispatch_phase(None)
                P.phase_end()
            with ExitStack() as stE:
                P.stack = stE
                expert_phase(l)
                P.phase_end()
            with ExitStack() as stC:
                P.stack = stC
                combine_phase(final=(l == n_layers - 1))
                P.phase_end()
        P.stack = st0
    return nc


_CACHE = {}


def kernel(**inputs):
    x = np.ascontiguousarray(inputs["x"], dtype=np.float32)
    B, S, _ = x.shape
    if S not in _CACHE:
        _CACHE[S] = build(S)
    nc = _CACHE[S]
    shared = {name: np.ascontiguousarray(inputs[name], dtype=np.float32) for name, _ in PARAMS}
    in_maps = []
    for b in range(B):
        m = dict(shared)
        m["x"] = x[b]
        in_maps.append(m)
    res = run_bass_kernel_spmd(nc, in_maps, core_ids=list(range(B)))
    return np.stack([np.asarray(r["out"], dtype=np.float32) for r in res.results], axis=0)
```

```python
import numpy as np
from contextlib import ExitStack
import concourse.bass as bass
import concourse.mybir as mybir
from concourse.bass_utils import run_bass_kernel_spmd

F32 = mybir.dt.float32
BF16 = mybir.dt.bfloat16
I32 = mybir.dt.int32
AF = mybir.ActivationFunctionType
ALU = mybir.AluOpType
AX = mybir.AxisListType

ENGS = ("pe", "act", "dve", "pool", "sp")
TWO_PI = float(2 * np.pi)


class Res:
    __slots__ = ("w", "r", "psum")

    def __init__(self):
        self.w = None
        self.r = {}
        self.psum = False


class TT_:
    def __init__(self, t):
        self.t = t
        self.r = Res()

    def __getitem__(self, k):
        return self.t[k]


class Prog:
    def __init__(self, nc, stack, n_dma_chan=48):
        self.nc = nc
        self.stack = stack
        self.q = {e: [] for e in ENGS}
        self.cnt = {e: 0 for e in ENGS}
        self.seen = {e: {} for e in ENGS}
        self.esem = {e: stack.enter_context(nc.semaphore("s_" + e)) for e in ENGS}
        self.chan = [stack.enter_context(nc.semaphore("s_dma%d" % i)) for i in range(n_dma_chan)]
        self.chan_cnt = [0] * n_dma_chan
        self.chan_next = 0
        half = n_dma_chan // 2
        self.ring = {"pool": list(range(half, n_dma_chan))}
        self.ring_pos = {"pool": 0, "hw": 0}
        self.ring["hw"] = list(range(0, half))
        self.semobj = {}
        for e in ENGS:
            self.semobj["E" + e] = self.esem[e]
        for i, s in enumerate(self.chan):
            self.semobj["C%d" % i] = s
        self.uid = 0
        self.pending = []
        self.sim_time = 0.0
        self.drain_tile = None

    def sb(self, shape, dtype, name=None, stack=None):
        self.uid += 1
        return TT_((stack or self.stack).enter_context(self.nc.sbuf_tensor(name or ("t%d" % self.uid), list(shape), dtype)))

    def ps(self, shape, dtype, name=None):
        self.uid += 1
        t = TT_(self.stack.enter_context(self.nc.psum_tensor(name or ("p%d" % self.uid), list(shape), dtype)))
        t.r.psum = True
        return t

    def _need(self, eng, reads, writes):
        deps = {}
        for r in reads:
            r = r.r if isinstance(r, TT_) else r
            if r.w is not None:
                k, v = r.w
                if deps.get(k, 0) < v:
                    deps[k] = v
            if r.psum:
                for k, v in r.r.items():
                    if k != "E" + eng and deps.get(k, 0) < v:
                        deps[k] = v
        for w in writes:
            w = w.r if isinstance(w, TT_) else w
            if w.w is not None:
                k, v = w.w
                if deps.get(k, 0) < v:
                    deps[k] = v
            for k, v in w.r.items():
                if deps.get(k, 0) < v:
                    deps[k] = v
        seen = self.seen[eng]
        for k, v in deps.items():
            if eng == "pe" and k == "Epe":
                continue
            if seen.get(k, 0) < v:
                seen[k] = v
                self.q[eng].append(("wait", k, v))

    def _commit(self, tok, reads, writes):
        k, v = tok
        for r in reads:
            r = r.r if isinstance(r, TT_) else r
            if r.r.get(k, 0) < v:
                r.r[k] = v
        for w in writes:
            w = w.r if isinstance(w, TT_) else w
            w.w = tok
            w.r = {}

    def op(self, eng, fn, reads=(), writes=(), cost=0.3, tset=None):
        self.pending.append(("op", eng, fn, self._norm(reads), self._norm(writes), cost, tset))

    def dma(self, eng, fn, reads=(), writes=(), cost=3.0):
        self.pending.append(("dma", eng, fn, self._norm(reads), self._norm(writes), cost, None))

    @staticmethod
    def _norm(lst):
        return [x if isinstance(x, Res) else x.r for x in lst]

    def _op_now(self, eng, fn, reads, writes):
        self._need(eng, reads, writes)
        self.cnt[eng] += 1
        tok = ("E" + eng, self.cnt[eng])
        self.q[eng].append(("op", fn))
        if eng == "dve" and self.drain_tile is not None and any(r.psum for r in reads):
            dt = self.drain_tile
            self.cnt[eng] += 1
            self.q[eng].append(("op", lambda e: e.memset(dt[:, 0:1], 0.0)))
            self._commit(tok, (), writes)
            self._commit(("E" + eng, self.cnt[eng]), reads, ())
            return
        self._commit(tok, reads, writes)

    def _dma_now(self, eng, fn, reads, writes):
        rk = "pool" if eng == "pool" else "hw"
        ring = self.ring[rk]
        c = ring[self.ring_pos[rk]]
        self.ring_pos[rk] = (self.ring_pos[rk] + 1) % len(ring)
        ck = "C%d" % c
        prev = self.chan_cnt[c] * 16
        if prev and self.seen[eng].get(ck, 0) < prev:
            self.seen[eng][ck] = prev
            self.q[eng].append(("wait", ck, prev))
        self._need(eng, reads, writes)
        self.chan_cnt[c] += 1
        tok = (ck, self.chan_cnt[c] * 16)
        self.q[eng].append(("dma", fn, ck))
        self._commit(tok, reads, writes)

    def flush(self, reorder=True, window=800):
        ops = self.pending
        self.pending = []
        n = len(ops)
        if n == 0:
            return
        if not reorder:
            order = range(n)
        else:
            preds = [None] * n
            lastw = {}
            readers = {}
            for i, o in enumerate(ops):
                ps = set()
                for r in o[3]:
                    j = lastw.get(id(r))
                    if j is not None:
                        ps.add(j)
                    if r.psum:
                        for j in readers.get(id(r), ()):
                            ps.add(j)
                for w in o[4]:
                    j = lastw.get(id(w))
                    if j is not None:
                        ps.add(j)
                    for j in readers.get(id(w), ()):
                        ps.add(j)
                for r in o[3]:
                    readers.setdefault(id(r), []).append(i)
                for w in o[4]:
                    lastw[id(w)] = i
                    readers[id(w)] = []
                ps.discard(i)
                preds[i] = ps
            succs = [[] for _ in range(n)]
            npred = [0] * n
            for i in range(n):
                npred[i] = len(preds[i])
                for j in preds[i]:
                    succs[j].append(i)
            blevel = [0.0] * n
            for i in range(n - 1, -1, -1):
                m = 0.0
                for k in succs[i]:
                    if blevel[k] > m:
                        m = blevel[k]
                blevel[i] = m + (ops[i][5] if ops[i][0] == "op" else 0.5) + 0.3
            rtime = [0.0] * n
            finish = [0.0] * n
            efree = {e: 0.0 for e in ENGS}
            ready = {e: [] for e in ENGS}
            for i in range(n):
                if npred[i] == 0:
                    ready[ops[i][1]].append(i)
            done = [False] * n
            lowest = 0
            order = []
            cur_set = None
            while len(order) < n:
                while lowest < n and done[lowest]:
                    lowest += 1
                lim = lowest + window
                best = None
                for e in ENGS:
                    ef = efree[e]
                    for i in ready[e]:
                        if i >= lim:
                            continue
                        st = rtime[i] if rtime[i] > ef else ef
                        if e == "act" and ops[i][6] is not None and ops[i][6] != cur_set:
                            st += 1.3
                        key = (int(st * 4.0), -blevel[i], i)
                        if best is None or key < best[0]:
                            best = (key, i, e)
                (_, _, _), i, e = best
                st = max(rtime[i], efree[e]) + (1.3 if (e == "act" and ops[i][6] is not None and ops[i][6] != cur_set) else 0.0)
                ready[e].remove(i)
                o = ops[i]
                if o[0] == "dma":
                    issue = 1.0 if e == "pool" else 0.06
                    efree[e] = st + issue
                    finish[i] = st + issue + o[5]
                else:
                    efree[e] = st + o[5]
                    finish[i] = st + o[5]
                    if e == "act" and o[6] is not None:
                        cur_set = o[6]
                done[i] = True
                order.append(i)
                for k in succs[i]:
                    npred[k] -= 1
                    t = finish[i] + (0.1 if ops[k][1] == e and o[0] == "op" else 0.45)
                    if t > rtime[k]:
                        rtime[k] = t
                    if npred[k] == 0:
                        ready[ops[k][1]].append(k)
            self.sim_time = max(finish)
        for i in order:
            o = ops[i]
            if o[0] == "op":
                self._op_now(o[1], o[2], o[3], o[4])
            else:
                self._dma_now(o[1], o[2], o[3], o[4])

    def barrier(self):
        for eng in ENGS:
            seen = self.seen[eng]
            for c in range(len(self.chan)):
                v = self.chan_cnt[c] * 16
                k = "C%d" % c
                if v and seen.get(k, 0) < v:
                    seen[k] = v
                    self.q[eng].append(("wait", k, v))
            for e in ENGS:
                k = "E" + e
                if e != eng and self.cnt[e] and seen.get(k, 0) < self.cnt[e]:
                    seen[k] = self.cnt[e]
                    self.q[eng].append(("wait", k, self.cnt[e]))

    def emit(self):
        nc = self.nc
        P = self

        def run(e, engobj):
            sem_e = P.esem[e]
            for item in P.q[e]:
                if item[0] == "wait":
                    engobj.wait_ge(P.semobj[item[1]], item[2])
                elif item[0] == "op":
                    item[1](engobj).then_inc(sem_e, 1)
                else:
                    item[1](engobj).then_inc(P.semobj[item[2]], 16)

        with nc.Block() as block:
            @block.tensor
            def _(e):
                run("pe", e)

            @block.scalar
            def _(e):
                run("act", e)

            @block.vector
            def _(e):
                run("dve", e)

            @block.gpsimd
            def _(e):
                run("pool", e)

            @block.sync
            def _(e):
                run("sp", e)
        self.q = {e: [] for e in ENGS}

    def phase_end(self, reorder=True):
        import os
        if os.environ.get("K_NOREORDER"):
            reorder = False
        self.flush(reorder)
        self.barrier()
        self.emit()


D = 1024
TT = 256
NSUB = TT // 128
EB = 512
NEXP = 16
EPS = 1e-6
BIG = 1.0e30

PARAMS = [
    ("norm_mix_g", [2, 1024]), ("w_in", [2, 1024, 1792]), ("a_v_norm_g", [2, 384]),
    ("a_spatial_w", [2, 6, 128, 128]), ("a_spatial_b", [2, 6, 128]), ("b_conv_w", [2, 4, 384]),
    ("b_conv_b", [2, 384]), ("b_rg_w", [2, 6, 64, 64]), ("b_rg_b", [2, 384]), ("b_ig_w", [2, 6, 64, 64]),
    ("b_ig_b", [2, 384]), ("b_lambda", [2, 384]), ("c_a_re", [2, 16, 64]), ("c_a_im", [2, 16, 64]),
    ("c_log_dt", [2, 16]), ("c_b_re", [2, 16, 64, 16]), ("c_b_im", [2, 16, 64, 16]),
    ("c_c_re", [2, 16, 16, 64]), ("c_c_im", [2, 16, 16, 64]), ("c_d", [2, 256]), ("c_glu_w", [2, 256, 256]),
    ("c_glu_b", [2, 256]), ("mix_out_norm_g", [2, 1024]), ("w_out", [2, 1024, 1024]), ("norm_ffn_g", [2, 1024]),
    ("router_group_w", [2, 1024, 4]), ("router_group_b", [2, 4]), ("router_expert_w", [2, 4, 1024, 4]),
    ("router_expert_b", [2, 4, 4]), ("expert_w_gate", [2, 16, 1024, 512]), ("expert_w_up", [2, 16, 1024, 512]),
    ("expert_w_down", [2, 16, 512, 1024]), ("final_norm_g", [1024]),
]


def build(S, n_layers=2, dbg=None):
    assert S % EB == 0
    NT = S // TT
    NS = S // 128
    NB = (2 * S) // EB + NEXP
    nc = bass.Bass("TRN2", target_bir_lowering=False)
    x_d = nc.dram_tensor("x", [S, D], F32, kind="ExternalInput").ap()
    W = {}
    for name, shp in PARAMS:
        W[name] = nc.dram_tensor(name, shp, F32, kind="ExternalInput").ap()
    out_d = nc.dram_tensor("out", [S, D], F32, kind="ExternalOutput").ap()
    hmid_d = nc.dram_tensor("hmid_scr", [S, D], F32, kind="Internal").ap()
    xf_d = nc.dram_tensor("xf_scr", [S, D], BF16, kind="Internal").ap()
    xs_d = nc.dram_tensor("xs_scr", [NB * EB, D], BF16, kind="Internal").ap()
    ys_d = nc.dram_tensor("ys_scr", [NB * EB, D], F32, kind="Internal").ap()
    r_hmid, r_xf, r_xs, r_ys = Res(), Res(), Res(), Res()
    dbg_d = {}
    REGS = {}
    if dbg:
        for k, shp in dbg.items():
            dbg_d[k] = nc.dram_tensor("dbg_" + k, shp, F32, kind="ExternalOutput").ap()

    nc_ctx = nc.allow_non_contiguous_dma(reason="small strided parameter loads")
    with ExitStack() as st0:
        st0.enter_context(nc_ctx)
        P = Prog(nc, st0)

        def fsz(ap):
            try:
                return int(ap.free_size())
            except Exception:
                n = 1
                for d in ap.shape[1:]:
                    n *= d
                return n

        def ecost(eng, ap, mult=1.0):
            n = fsz(ap) * mult
            if eng == "act":
                return 0.28 + n / 1200.0
            if eng == "dve":
                return 0.17 + n / 960.0
            return 0.35 + n / 480.0

        TSET = {AF.Gelu_apprx_tanh: 11, AF.Tanh: 11, AF.Exp: 6, AF.Ln: 6, AF.Sin: 9, AF.Silu: 18}

        def act(out, in_, func, reads, writes, **kw):
            P.op("act", lambda e: e.activation(out=out, in_=in_, func=func, **kw), reads, writes, cost=ecost("act", out),
                 tset=TSET.get(func))

        def tt(eng, out, in0, in1, op, reads, writes):
            P.op(eng, lambda e: e.tensor_tensor(out=out, in0=in0, in1=in1, op=op), reads, writes, cost=ecost(eng, out))

        def ts(eng, out, in0, s1, s2, op0, op1, reads, writes, **kw):
            if s2 is None:
                P.op(eng, lambda e: e.tensor_scalar(out=out, in0=in0, scalar1=s1, scalar2=None, op0=op0, **kw), reads, writes, cost=ecost(eng, out))
            else:
                P.op(eng, lambda e: e.tensor_scalar(out=out, in0=in0, scalar1=s1, scalar2=s2, op0=op0, op1=op1, **kw), reads, writes, cost=ecost(eng, out))

        def stt(out, in0, scalar, in1, op0, op1, reads, writes):
            P.op("dve", lambda e: e.scalar_tensor_tensor(out=out, in0=in0, scalar=scalar, in1=in1, op0=op0, op1=op1), reads, writes, cost=ecost("dve", out))

        def scan(out, d0, d1, init, reads, writes):
            P.op("dve", lambda e: e.tensor_tensor_scan(out=out, data0=d0, data1=d1, initial=init, op0=ALU.mult, op1=ALU.add), reads, writes,
                 cost=ecost("dve", out, 2.0))

        def red(out, in_, op, reads, writes):
            P.op("dve", lambda e: e.tensor_reduce(out=out, in_=in_, axis=AX.X, op=op), reads, writes, cost=ecost("dve", in_))

        def cp(eng, out, in_, reads, writes):
            if eng == "act":
                P.op("act", lambda e: e.activation(out=out, in_=in_, func=AF.Copy), reads, writes, cost=ecost("act", out))
            else:
                P.op(eng, lambda e: e.tensor_copy(out=out, in_=in_), reads, writes, cost=ecost(eng, out))

        def mm(out, lhsT, rhs, start, stop, reads, writes):
            n = max(64, fsz(rhs))
            c = n / 2400.0 * (4.0 if rhs.dtype == F32 else 1.0) + 0.03
            P.op("pe", lambda e: e.matmul(out, lhsT=lhsT, rhs=rhs, start=start, stop=stop), reads, writes, cost=c)

        def tr(out, in_, ident, reads, writes):
            P.op("pe", lambda e: e.transpose(out=out, in_=in_, identity=ident), reads, writes, cost=(0.35 if in_.dtype == F32 else 0.12))

        def dma(eng, out, in_, reads, writes):
            try:
                nb = int(out.nbytes())
            except Exception:
                nb = 65536
            P.dma(eng, lambda e: e.dma_start(out=out, in_=in_), reads, writes, cost=2.0 + nb / 250e3)

        def memset(eng, ap, val, writes):
            P.op(eng, lambda e: e.memset(ap, val), (), writes, cost=ecost(eng, ap))

        def recip(out, in_, reads, writes):
            P.op("dve", lambda e: e.reciprocal(out=out, in_=in_), reads, writes, cost=ecost("dve", out, 4.0))

        def rstd_from_ss(rs, ss, n, k=1):
            act(rs[:, 0:k], ss[:, 0:k], AF.Ln, [ss, eps_t], [rs], scale=1.0 / n, bias=eps_t[:, 0:1])
            act(rs[:, 0:k], rs[:, 0:k], AF.Exp, [rs], [rs], scale=-0.5)

        ident_b = P.sb([128, 128], BF16)
        ident_f = P.sb([128, 128], F32)
        ones_b = P.sb([128, 128], BF16)
        triu_b = P.sb([128, 128], BF16)
        iota_f = P.sb([128, 512], F32)
        iota16 = P.sb([128, 16], F32)
        pidx_i = P.sb([128, 1], I32)
        pidx_f = P.sb([128, 1], F32)
        eps_t = P.sb([128, 1], F32)
        onep_t = P.sb([128, 1], F32)
        halfpi_t = P.sb([128, 1], F32)
        rowtwo = P.sb([128, 2], F32)
        rowhalf = P.sb([128, 2], F32)
        rowq4 = P.sb([128, 4], F32)
        colq4 = P.sb([128, 4, 128], F32)
        tmpc = P.sb([128, 128], F32)
        tmpi = P.sb([128, 1], I32)
        ohs = P.sb([128, NS, 2, 16], BF16)
        rks = P.sb([128, NS, 2], F32)
        gts = P.sb([128, NS, 2], F32)
        base = P.sb([128, 16], F32)
        dest_i = P.sb([128, NS, 2], I32)
        blk_e = P.sb([128, NB], I32)
        psT = P.ps([128, 1024], BF16)
        psA = [P.ps([128, 512], F32) for _ in range(2)]
        psZ = [P.ps([128, 512], F32) for _ in range(2)]
        psM = [P.ps([128, 512], F32) for _ in range(3)]

        P.op("pool", lambda e: e.iota(iota_f[:], [[1, 512]], base=0, channel_multiplier=0, allow_small_or_imprecise_dtypes=True), (), [iota_f])
        P.op("pool", lambda e: e.iota(iota16[:], [[1, 16]], base=0, channel_multiplier=0, allow_small_or_imprecise_dtypes=True), (), [iota16])
        P.op("pool", lambda e: e.iota(pidx_i[:], [[1, 1]], base=0, channel_multiplier=1), (), [pidx_i])
        cp("dve", pidx_f[:], pidx_i[:], [pidx_i], [pidx_f])
        P.op("pool", lambda e: e.iota(tmpc[:], [[1, 128]], base=0, channel_multiplier=-1, allow_small_or_imprecise_dtypes=True), (), [tmpc])
        P.op("dve", lambda e: e.tensor_single_scalar(out=ident_f[:], in_=tmpc[:], scalar=0.0, op=ALU.is_equal), [tmpc], [ident_f])
        cp("dve", ident_b[:], ident_f[:], [ident_f], [ident_b])
        P.op("dve", lambda e: e.tensor_single_scalar(out=triu_b[:], in_=tmpc[:], scalar=0.0, op=ALU.is_gt), [tmpc], [triu_b])
        memset("pool", ones_b[:], 1.0, [ones_b])
        memset("pool", eps_t[:], EPS, [eps_t])
        memset("pool", onep_t[:], 1.0 + 1.2e-7, [onep_t])
        memset("pool", halfpi_t[:], float(np.pi / 2), [halfpi_t])
        P.op("dve", lambda e: e.tensor_single_scalar(out=tmpi[:], in_=pidx_i[:], scalar=4, op=ALU.arith_shift_right), [pidx_i], [tmpi])
        P.op("dve", lambda e: e.tensor_single_scalar(out=tmpi[:], in_=tmpi[:], scalar=1, op=ALU.bitwise_and), [tmpi], [tmpi])
        cp("dve", rowtwo[:, 1:2], tmpi[:], [tmpi], [rowtwo])
        ts("dve", rowtwo[:, 0:1], rowtwo[:, 1:2], -1.0, 1.0, ALU.mult, ALU.add, [rowtwo], [rowtwo])
        P.op("dve", lambda e: e.tensor_single_scalar(out=rowhalf[:, 1:2], in_=pidx_f[:], scalar=64.0, op=ALU.is_ge), [pidx_f], [rowhalf])
        ts("dve", rowhalf[:, 0:1], rowhalf[:, 1:2], -1.0, 1.0, ALU.mult, ALU.add, [rowhalf], [rowhalf])
        for q4 in range(4):
            ts("dve", rowq4[:, q4:q4 + 1], pidx_f[:], float(32 * q4), None, ALU.is_ge, None, [pidx_f], [rowq4])
            P.op("dve", lambda e, q4=q4: e.tensor_single_scalar(out=tmpc[:, 0:1], in_=pidx_f[:], scalar=float(32 * q4 + 32), op=ALU.is_lt), [pidx_f], [tmpc])
            tt("dve", rowq4[:, q4:q4 + 1], rowq4[:, q4:q4 + 1], tmpc[:, 0:1], ALU.mult, [rowq4, tmpc], [rowq4])
            memset("pool", colq4[:, q4, :], 0.0, [colq4])
            memset("pool", colq4[:, q4, 32 * q4:32 * q4 + 32], 1.0, [colq4])

        def load_layer_consts(l, C, stS):
            sb = P.sb

            def tmp(shape, dt):
                return P.sb(shape, dt, stack=stS)

            C["win"] = sb([128, 8, 1792], BF16)
            C["wout"] = sb([128, 8, 1024], BF16)
            C["gmix"] = sb([128, 1024], F32)
            C["gffn"] = sb([128, 1024], F32)
            C["gv"] = sb([128, 384], F32)
            C["goa"] = sb([128, 384], F32)
            C["gobc"] = sb([128, 5], F32)
            C["wr"] = sb([128, 8, 20], F32)
            C["rb"] = sb([128, 20], F32)
            C["wmT"] = sb([128, 6, 128], BF16)
            C["bsT"] = sb([128, 6], F32)
            C["cw"] = sb([128, 3, 4], F32)
            C["coef"] = sb([128, 3], F32)
            C["coef2"] = sb([128, 3], F32)
            C["coefh"] = sb([128, 3], F32)
            C["rgbh"] = sb([128, 3], F32)
            C["igbh"] = sb([128, 3], F32)
            C["glubh"] = sb([128, 2], F32)
            C["gobch"] = sb([128, 2], F32)
            C["rr"] = sb([128, 8], F32)
            C["cos"] = sb([128, 8, TT], F32)
            C["sin"] = sb([128, 8, TT], F32)
            C["blhs"] = sb([128, 8, 2, 128], BF16)
            C["clhs"] = sb([128, 8, 2, 128], BF16)
            C["dd"] = sb([128, 2], F32)
            C["glub"] = sb([128, 2], F32)
            C["gluw"] = sb([128, 2, 256], BF16)
            C["cb"] = sb([128, 3], F32)
            C["rgb"] = sb([128, 3], F32)
            C["igb"] = sb([128, 3], F32)
            C["lam"] = sb([128, 3], F32)
            C["bdr"] = sb([128, 3, 128], BF16)
            C["bdi"] = sb([128, 3, 128], BF16)
            dma("pool", C["win"][:], W["w_in"][l].rearrange("(k p) n -> p k n", p=128), (), [C["win"]])
            dma("pool", C["wout"][:], W["w_out"][l].rearrange("(k p) n -> p k n", p=128), (), [C["wout"]])
            dma("sp", C["gmix"][:], W["norm_mix_g"][l].partition_broadcast(128), (), [C["gmix"]])
            dma("sp", C["gffn"][:], W["norm_ffn_g"][l].partition_broadcast(128), (), [C["gffn"]])
            dma("sp", C["gv"][:], W["a_v_norm_g"][l].partition_broadcast(128), (), [C["gv"]])
            dma("sp", C["goa"][:], W["mix_out_norm_g"][l][0:384].partition_broadcast(128), (), [C["goa"]])
            dma("sp", C["gobc"][:], W["mix_out_norm_g"][l][384:1024].rearrange("(c p) -> p c", p=128), (), [C["gobc"]])
            dma("sp", C["wr"][:, :, 0:4], W["router_group_w"][l].rearrange("(k p) n -> p k n", p=128), (), [C["wr"]])
            for g in range(4):
                dma("sp", C["wr"][:, :, 4 + 4 * g:8 + 4 * g], W["router_expert_w"][l, g].rearrange("(k p) n -> p k n", p=128), (), [C["wr"]])
            dma("sp", C["rb"][:, 0:4], W["router_group_b"][l].partition_broadcast(128), (), [C["rb"]])
            dma("sp", C["rb"][:, 4:20], W["router_expert_b"][l].rearrange("g e -> (g e)").partition_broadcast(128), (), [C["rb"]])
            wa_nat = tmp([128, 6, 128], F32)
            dma("sp", wa_nat[:], W["a_spatial_w"][l].rearrange("h i j -> i h j"), (), [wa_nat])
            for h in range(6):
                pz = psM[h % 3]
                tr(pz[:, 0:128], wa_nat[:, h, :], ident_f[:], [wa_nat, ident_f], [pz])
                cp("act" if h % 2 else "dve", C["wmT"][:, h, :], pz[:, 0:128], [pz], [C["wmT"]])
            memset("pool", C["wmT"][64:128, :, 0:64], 0.0, [C["wmT"]])
            dma("sp", C["bsT"][:], W["a_spatial_b"][l].rearrange("h i -> i h"), (), [C["bsT"]])
            for k in range(4):
                dma("sp", C["cw"][:, :, k], W["b_conv_w"][l, k].rearrange("(c p) -> p c", p=128), (), [C["cw"]])
            for nm, key in (("cb", "b_conv_b"), ("rgb", "b_rg_b"), ("igb", "b_ig_b"), ("lam", "b_lambda")):
                dma("sp", C[nm][:], W[key][l].rearrange("(c p) -> p c", p=128), (), [C[nm]])
            for nm, key in (("bdr", "b_rg_w"), ("bdi", "b_ig_w")):
                memset("pool", C[nm][:], 0.0, [C[nm]])
                for c3 in range(3):
                    for two in range(2):
                        dma("pool", C[nm][64 * two:64 * two + 64, c3, 64 * two:64 * two + 64], W[key][l, 2 * c3 + two], (), [C[nm]])
            spt = tmp([128, 3], F32)
            act(spt[:], C["lam"][:], AF.Exp, [C["lam"]], [spt], scale=-1.0)
            act(spt[:], spt[:], AF.Ln, [spt], [spt], bias=1.0)
            ts("dve", C["coef"][:], spt[:], -8.0, None, ALU.mult, None, [spt], [C["coef"]])
            ts("dve", C["coef2"][:], spt[:], -16.0, None, ALU.mult, None, [spt], [C["coef2"]])
            ts("dve", C["coefh"][:], spt[:], -4.0, None, ALU.mult, None, [spt], [C["coefh"]])
            ts("dve", C["rgbh"][:], C["rgb"][:], 0.5, None, ALU.mult, None, [C["rgb"]], [C["rgbh"]])
            ts("dve", C["igbh"][:], C["igb"][:], 0.5, None, ALU.mult, None, [C["igb"]], [C["igbh"]])
            ts("dve", C["gobch"][:], C["gobc"][:, 3:5], 0.5, None, ALU.mult, None, [C["gobc"]], [C["gobch"]])
            are = tmp([128, 8], F32)
            aim = tmp([128, 8], F32)
            ldt = tmp([128, 8], F32)
            dma("sp", are[:], W["c_a_re"][l].rearrange("(q two) p -> (two p) q", two=2), (), [are])
            dma("sp", aim[:], W["c_a_im"][l].rearrange("(q two) p -> (two p) q", two=2), (), [aim])
            ldv = W["c_log_dt"][l].rearrange("(q two) -> two q", two=2)
            dma("sp", ldt[0:64, :], ldv[0].partition_broadcast(64), (), [ldt])
            dma("sp", ldt[64:128, :], ldv[1].partition_broadcast(64), (), [ldt])
            dtt = tmp([128, 8], F32)
            act(dtt[:], ldt[:], AF.Exp, [ldt], [dtt])
            th = tmp([128, 8], F32)
            tt("dve", th[:], aim[:], dtt[:], ALU.mult, [aim, dtt], [th])
            tt("dve", C["rr"][:], are[:], dtt[:], ALU.mult, [are, dtt], [C["rr"]])
            act(C["rr"][:], C["rr"][:], AF.Exp, [C["rr"]], [C["rr"]])
            ang = tmp([128, 8, TT], F32)
            angi = tmp([128, 8, TT], I32)
            thn = tmp([128, 8], F32)
            ts("dve", thn[:], th[:], 1.0 / TWO_PI, None, ALU.mult, None, [th], [thn])
            ts("dve", ang[:, 0, :], iota_f[:, 0:TT], 1.0, None, ALU.add, None, [iota_f], [ang])
            for q in range(1, 8):
                cp("pool", ang[:, q, :], ang[:, 0, :], [ang], [ang])
            tt("dve", ang[:], ang[:], thn[:].unsqueeze(2).to_broadcast([128, 8, TT]), ALU.mult, [ang, thn], [ang])
            cp("dve", angi[:], ang[:], [ang], [angi])
            cp("dve", C["cos"][:], angi[:], [angi], [C["cos"]])
            tt("dve", ang[:], ang[:], C["cos"][:], ALU.subtract, [ang, C["cos"]], [ang])
            act(C["sin"][:], ang[:], AF.Sin, [ang], [C["sin"]], scale=TWO_PI * (1 - 1e-6))
            act(ang[:], ang[:], AF.Abs, [ang], [ang])
            act(C["cos"][:], ang[:], AF.Sin, [ang], [C["cos"]], scale=-TWO_PI * (1 - 1e-6), bias=halfpi_t[:, 0:1])
            nr = tmp([128, 8], F32)
            ni = tmp([128, 8], F32)
            den = tmp([128, 8], F32)
            fr = tmp([128, 8], F32)
            fi = tmp([128, 8], F32)
            t8 = tmp([128, 8], F32)
            tt("dve", nr[:], C["rr"][:], C["cos"][:, :, 0], ALU.mult, [C["rr"], C["cos"]], [nr])
            ts("dve", nr[:], nr[:], -1.0, None, ALU.add, None, [nr], [nr])
            tt("dve", ni[:], C["rr"][:], C["sin"][:, :, 0], ALU.mult, [C["rr"], C["sin"]], [ni])
            tt("dve", den[:], are[:], are[:], ALU.mult, [are], [den])
            tt("dve", t8[:], aim[:], aim[:], ALU.mult, [aim], [t8])
            tt("dve", den[:], den[:], t8[:], ALU.add, [den, t8], [den])
            recip(den[:], den[:], [den], [den])
            tt("dve", fr[:], nr[:], are[:], ALU.mult, [nr, are], [fr])
            tt("dve", t8[:], ni[:], aim[:], ALU.mult, [ni, aim], [t8])
            tt("dve", fr[:], fr[:], t8[:], ALU.add, [fr, t8], [fr])
            tt("dve", fr[:], fr[:], den[:], ALU.mult, [fr, den], [fr])
            tt("dve", fi[:], ni[:], are[:], ALU.mult, [ni, are], [fi])
            tt("dve", t8[:], nr[:], aim[:], ALU.mult, [nr, aim], [t8])
            tt("dve", fi[:], fi[:], t8[:], ALU.subtract, [fi, t8], [fi])
            tt("dve", fi[:], fi[:], den[:], ALU.mult, [fi, den], [fi])
            bre = tmp([128, 8, 16], F32)
            bim = tmp([128, 8, 16], F32)
            dma("sp", bre[:], W["c_b_re"][l].rearrange("(q two) p c -> (two p) q c", two=2), (), [bre])
            dma("sp", bim[:], W["c_b_im"][l].rearrange("(q two) p c -> (two p) q c", two=2), (), [bim])
            bbr = tmp([128, 8, 16], F32)
            bbi = tmp([128, 8, 16], F32)
            t16 = tmp([128, 8, 16], F32)
            frb = fr[:].unsqueeze(2).to_broadcast([128, 8, 16])
            fib = fi[:].unsqueeze(2).to_broadcast([128, 8, 16])
            tt("dve", bbr[:], bre[:], frb, ALU.mult, [bre, fr], [bbr])
            tt("dve", t16[:], bim[:], fib, ALU.mult, [bim, fi], [t16])
            tt("dve", bbr[:], bbr[:], t16[:], ALU.subtract, [bbr, t16], [bbr])
            tt("dve", bbi[:], bim[:], frb, ALU.mult, [bim, fr], [bbi])
            tt("dve", t16[:], bre[:], fib, ALU.mult, [bre, fi], [t16])
            tt("dve", bbi[:], bbi[:], t16[:], ALU.add, [bbi, t16], [bbi])
            bpad = tmp([128, 8, 2, 16], F32)
            for ri, src in enumerate((bbr, bbi)):
                for two in range(2):
                    ts("dve", bpad[:, :, two, :], src[:], rowhalf[:, two:two + 1], None, ALU.mult, None, [src, rowhalf], [bpad])
                for hh in range(2):
                    pz = psM[(ri * 2 + hh) % 3]
                    tr(pz[:, 0:128], bpad[:, 4 * hh:4 * hh + 4, :, :].rearrange("p a b c -> p (a b c)"), ident_f[:], [bpad, ident_f], [pz])
                    for q4 in range(4):
                        ts("dve", C["blhs"][:, 4 * hh + q4, ri, :], pz[:, 0:128], rowq4[:, q4:q4 + 1], None, ALU.mult, None, [pz, rowq4], [C["blhs"]])
            cn = tmp([128, 2, 64], F32)
            cpad = tmp([128, 2, 2, 64], F32)
            for ri, key in enumerate(("c_c_re", "c_c_im")):
                for hh in range(2):
                    dma("sp", cn[:, hh, :], W[key][l, 8 * hh:8 * hh + 8].rearrange("g c p -> (g c) p"), (), [cn])
                for two in range(2):
                    ts("dve", cpad[:, :, two, :], cn[:], rowtwo[:, two:two + 1], None, ALU.mult, None, [cn, rowtwo], [cpad])
                for hh in range(2):
                    pz = psM[(ri * 2 + hh) % 3]
                    tr(pz[:, 0:128], cpad[:, hh, :, :].rearrange("p a b -> p (a b)"), ident_f[:], [cpad, ident_f], [pz])
                    for q4 in range(4):
                        if ri == 0:
                            tt("dve", C["clhs"][:, 4 * hh + q4, 0, :], pz[:, 0:128], colq4[:, q4, :], ALU.mult, [pz, colq4], [C["clhs"]])
                        else:
                            stt(C["clhs"][:, 4 * hh + q4, 1, :], pz[:, 0:128], -1.0, colq4[:, q4, :], ALU.mult, ALU.mult, [pz, colq4], [C["clhs"]])
            dma("sp", C["dd"][:], W["c_d"][l].rearrange("(c p) -> p c", p=128), (), [C["dd"]])
            dma("sp", C["glub"][:], W["c_glu_b"][l].rearrange("(c p) -> p c", p=128), (), [C["glub"]])
            dma("pool", C["gluw"][:], W["c_glu_w"][l].rearrange("(k p) n -> p k n", p=128), (), [C["gluw"]])
            ts("dve", C["glubh"][:], C["glub"][:], 0.5, None, ALU.mult, None, [C["glub"]], [C["glubh"]])

        def alloc_mixer_work(Wk):
            sb = P.sb
            Wk["h"] = [sb([128, NSUB, 1024], F32) for _ in range(2)]
            Wk["sqp"] = sb([128, 1024], BF16)
            Wk["sqa"] = sb([128, 384], BF16)
            Wk["sqe"] = sb([128, 1024], BF16)
            Wk["zero"] = sb([128, 1024], BF16)
            Wk["xn"] = [sb([128, 1024], BF16) for _ in range(2)]
            Wk["xnT"] = [sb([128, 8, TT], BF16) for _ in range(2)]
            Wk["yT"] = [sb([128, 8, TT], BF16) for _ in range(2)]
            for nm in ("ssp", "rsp", "ssa", "rsa", "ssa2", "rsa2", "sse", "rse"):
                Wk[nm] = [sb([128, 2], F32) for _ in range(2)]
            Wk["u"] = sb([128, 384], F32)
            Wk["vg"] = sb([128, 384], F32)
            Wk["v"] = sb([128, 384], BF16)
            Wk["yab"] = sb([128, 384], BF16)
            Wk["xbe"] = [sb([128, 3, TT + 3], F32) for _ in range(2)]
            Wk["xc"] = sb([128, TT], F32)
            Wk["xcb"] = sb([128, TT], BF16)
            Wk["rg"] = sb([128, TT], F32)
            Wk["ig"] = sb([128, TT], F32)
            Wk["aa"] = sb([128, TT], F32)
            Wk["bb"] = sb([128, TT], F32)
            Wk["hs"] = sb([128, TT], F32)
            Wk["yb"] = sb([128, 3, TT], F32)
            Wk["ybs"] = sb([128, 3, TT], BF16)
            Wk["hcar"] = sb([128, 3], F32)
            Wk["nrmB"] = sb([128, TT], F32)
            Wk["nrmC"] = sb([128, TT], F32)
            Wk["xcc"] = [sb([128, 2, TT], F32) for _ in range(2)]
            Wk["xccb"] = [sb([128, 2, TT], BF16) for _ in range(2)]
            for nm in ("ure", "uim", "mre", "mim", "gre", "gim"):
                Wk[nm] = [sb([128, TT], F32) for _ in range(2)]
            Wk["t1"] = [sb([128, TT], F32) for _ in range(2)]
            Wk["t2"] = [sb([128, TT], F32) for _ in range(2)]
            Wk["cst"] = [sb([128, 2], F32) for _ in range(2)]
            Wk["sre"] = [sb([128, TT], BF16) for _ in range(2)]
            Wk["sim"] = [sb([128, TT], BF16) for _ in range(2)]
            Wk["scar"] = sb([128, 8, 2], F32)
            Wk["yc"] = sb([128, 2, TT], F32)
            Wk["ycb"] = sb([128, 2, TT], BF16)
            Wk["ycs"] = sb([128, 2, TT], BF16)
            Wk["sg"] = sb([128, TT], F32)
            Wk["xf"] = [sb([128, 1024], BF16) for _ in range(2)]
            Wk["xf32"] = sb([128, 1024], F32)
            Wk["xfT"] = [sb([128, 4, 128], F32) for _ in range(2)]
            for nm, w_ in (("lg", 20), ("em", 16), ("em2", 16), ("pen", 4), ("gsel", 4), ("rank", 16)):
                Wk[nm] = [sb([128, NSUB, w_], F32) for _ in range(2)]
            Wk["sm"] = [sb([128, 9, NSUB], F32) for _ in range(2)]
            Wk["ohb"] = [sb([128, NSUB, 16], BF16) for _ in range(2)]

        def mixer_tile(l, ti, C, Wk, first_layer):
            hb = Wk["h"][ti % 2]
            t0 = ti * TT
            for s in range(NSUB):
                gs = ti * NSUB + s
                rows = slice(t0 + s * 128, t0 + (s + 1) * 128)
                dma("sp", hb[:, s, :], (x_d if first_layer else hmid_d)[rows, :], (), [hb])
            xnT = Wk["xnT"][ti % 2]
            ss, rs = Wk["ssp"][ti % 2], Wk["rsp"][ti % 2]
            for s in range(NSUB):
                jk = Wk["sqp"]
                act(jk[:], hb[:, s, :], AF.Square, [hb], [ss, jk], accum_out=ss[:, s:s + 1])
            rstd_from_ss(rs, ss, 1024, NSUB)
            for s in range(NSUB):
                xn = Wk["xn"][s % 2]
                stt(xn[:], hb[:, s, :], rs[:, s:s + 1], C["gmix"][:], ALU.mult, ALU.mult, [hb, rs, C["gmix"]], [xn])
                for k in range(8):
                    tr(psT[:, k * 128:(k + 1) * 128], xn[:, k * 128:(k + 1) * 128], ident_b[:], [xn, ident_b], [psT])
                cp("act", xnT[:, :, s * 128:(s + 1) * 128], psT[:].rearrange("p (k t) -> p k t", k=8), [psT], [xnT])
            import os as _os
            _cut = float(_os.environ.get("K_CUT", "9"))
            if _cut <= 1:
                return
            yT = Wk["yT"][ti % 2]
            for s in range(NSUB):
                ssa, rsa = Wk["ssa"][s % 2], Wk["rsa"][s % 2]
                for half in range(2):
                    for k in range(8):
                        mm(psA[half][:, 0:384], xnT[:, k, s * 128:(s + 1) * 128], C["win"][:, k, 384 * half:384 * half + 384],
                           k == 0, k == 7, [xnT, C["win"]], [psA[half]])
                act(Wk["u"][:], psA[0][:, 0:384], AF.Gelu_apprx_tanh, [psA[0]], [Wk["u"]])
                act(Wk["vg"][:], psA[1][:, 0:384], AF.Gelu_apprx_tanh, [psA[1]], [Wk["vg"]])
                jk = Wk["sqa"]
                act(jk[:], Wk["vg"][:], AF.Square, [Wk["vg"]], [ssa, jk], accum_out=ssa[:, 0:1])
                rstd_from_ss(rsa, ssa, 384)
                stt(Wk["v"][:], Wk["vg"][:], rsa[:, 0:1], C["gv"][:], ALU.mult, ALU.mult, [Wk["vg"], rsa, C["gv"]], [Wk["v"]])
                for hh in range(6):
                    mm(psA[1][:, 64 * hh:64 * hh + 64], C["wmT"][:, hh, :], Wk["v"][:, 64 * hh:64 * hh + 64], True, True,
                       [C["wmT"], Wk["v"]], [psA[1]])
                ya = Wk["vg"]
                tt("dve", ya[:].rearrange("p (h d) -> p h d", h=6), psA[1][:, 0:384].rearrange("p (h d) -> p h d", h=6),
                   C["bsT"][:].unsqueeze(2).to_broadcast([128, 6, 64]), ALU.add, [psA[1], C["bsT"]], [ya])
                tt("dve", ya[:], ya[:], Wk["u"][:], ALU.mult, [ya, Wk["u"]], [ya])
                if dbg and "y_a" in dbg_d:
                    dma("sp", dbg_d["y_a"][t0 + s * 128:t0 + (s + 1) * 128, :], ya[:], [ya], ())
                ssa2, rsa2 = Wk["ssa2"][s % 2], Wk["rsa2"][s % 2]
                jk = Wk["sqa"]
                act(jk[:], ya[:], AF.Square, [ya], [ssa2, jk], accum_out=ssa2[:, 0:1])
                rstd_from_ss(rsa2, ssa2, 384)
                stt(Wk["yab"][:], ya[:], rsa2[:, 0:1], C["goa"][:], ALU.mult, ALU.mult, [ya, rsa2, C["goa"]], [Wk["yab"]])
                for k in range(3):
                    tr(psT[:, k * 128:(k + 1) * 128], Wk["yab"][:, k * 128:(k + 1) * 128], ident_b[:], [Wk["yab"], ident_b], [psT])
                cp("act", yT[:, 0:3, s * 128:(s + 1) * 128], psT[:, 0:384].rearrange("p (k t) -> p k t", k=3), [psT], [yT])

            if _cut <= 2:
                return

            def zchunk(col0, pz, c0=0):
                for k in range(8):
                    mm(pz[:, c0:c0 + TT], C["win"][:, k, col0:col0 + 128], xnT[:, k, :], k == 0, k == 7, [C["win"], xnT], [pz])

            psB0, psB1 = psZ[0], psM[0]
            psC0, psC1, psC2 = psZ[1], psM[1], psM[2]
            xbe = Wk["xbe"][ti % 2]
            xbp = Wk["xbe"][(ti + 1) % 2]
            for c3 in range(3):
                zchunk(768 + 128 * c3, psB0)
                cp("act", xbe[:, c3, 3:3 + TT], psB0[:, 0:TT], [psB0], [xbe])
            if ti == 0:
                memset("pool", xbe[:, :, 0:3], 0.0, [xbe])
                memset("pool", Wk["hcar"][:], 0.0, [Wk["hcar"]])
                memset("pool", Wk["scar"][:], 0.0, [Wk["scar"]])
            else:
                cp("pool", xbe[:, :, 0:3], xbp[:, :, TT:TT + 3], [xbp], [xbe])
            for c3 in range(3):
                xc, xcb = Wk["xc"], Wk["xcb"]
                act(xc[:], xbe[:, c3, 3:3 + TT], AF.Identity, [xbe, C["cw"], C["cb"]], [xc], scale=C["cw"][:, c3, 3:4], bias=C["cb"][:, c3:c3 + 1])
                for k in range(3):
                    stt(xc[:], xbe[:, c3, k:k + TT], C["cw"][:, c3, k:k + 1], xc[:], ALU.mult, ALU.add, [xbe, C["cw"], xc], [xc])
                cp("act", xcb[:], xc[:], [xc], [xcb])
                mm(psB1[:, 0:TT], C["bdr"][:, c3, :], xcb[:], True, True, [C["bdr"], xcb], [psB1])
                mm(psB1[:, TT:2 * TT], C["bdi"][:, c3, :], xcb[:], True, True, [C["bdi"], xcb], [psB1])
                act(Wk["rg"][:], psB1[:, 0:TT], AF.Tanh, [psB1, C["rgbh"]], [Wk["rg"]], scale=0.5, bias=C["rgbh"][:, c3:c3 + 1])
                act(Wk["ig"][:], psB1[:, TT:2 * TT], AF.Tanh, [psB1, C["igbh"]], [Wk["ig"]], scale=0.5, bias=C["igbh"][:, c3:c3 + 1])
                act(Wk["aa"][:], Wk["rg"][:], AF.Exp, [Wk["rg"], C["coefh"]], [Wk["aa"]], scale=C["coefh"][:, c3:c3 + 1], bias=C["coefh"][:, c3:c3 + 1])
                act(Wk["bb"][:], Wk["rg"][:], AF.Exp, [Wk["rg"], C["coef"]], [Wk["bb"]], scale=C["coef"][:, c3:c3 + 1], bias=C["coef"][:, c3:c3 + 1])
                act(Wk["bb"][:], Wk["bb"][:], AF.Ln, [Wk["bb"], onep_t], [Wk["bb"]], scale=-1.0, bias=onep_t[:, 0:1])
                act(Wk["bb"][:], Wk["bb"][:], AF.Exp, [Wk["bb"]], [Wk["bb"]], scale=0.5)
                ts("pool", Wk["ig"][:], Wk["ig"][:], 1.0, None, ALU.add, None, [Wk["ig"]], [Wk["ig"]])
                tt("pool", Wk["ig"][:], Wk["ig"][:], xc[:], ALU.mult, [Wk["ig"], xc], [Wk["ig"]])
                stt(Wk["bb"][:], Wk["bb"][:], 0.5, Wk["ig"][:], ALU.mult, ALU.mult, [Wk["bb"], Wk["ig"]], [Wk["bb"]])
                scan(Wk["hs"][:], Wk["aa"][:], Wk["bb"][:], Wk["hcar"][:, c3:c3 + 1], [Wk["aa"], Wk["bb"], Wk["hcar"]], [Wk["hs"]])
                cp("act", Wk["hcar"][:, c3:c3 + 1], Wk["hs"][:, TT - 1:TT], [Wk["hs"]], [Wk["hcar"]])
                zchunk(1152 + 128 * c3, psB0)
                gg = Wk["rg"]
                act(gg[:], psB0[:, 0:TT], AF.Gelu_apprx_tanh, [psB0], [gg])
                tt("dve", Wk["yb"][:, c3, :], Wk["hs"][:], gg[:], ALU.mult, [Wk["hs"], gg], [Wk["yb"]])
                act(Wk["ybs"][:, c3, :], Wk["yb"][:, c3, :], AF.Square, [Wk["yb"]], [Wk["ybs"]])
            if dbg and "y_b" in dbg_d:
                for c3 in range(3):
                    dma("sp", dbg_d["y_b"][c3 * 128:(c3 + 1) * 128, t0:t0 + TT], Wk["yb"][:, c3, :], [Wk["yb"]], ())
            for c3 in range(3):
                mm(psB1[:, 0:TT], ones_b[:], Wk["ybs"][:, c3, :], c3 == 0, c3 == 2, [ones_b, Wk["ybs"]], [psB1])
            nrm = Wk["nrmB"]
            act(nrm[:], psB1[:, 0:TT], AF.Ln, [psB1, eps_t], [nrm], scale=1.0 / 384, bias=eps_t[:, 0:1])
            act(nrm[:], nrm[:], AF.Exp, [nrm], [nrm], scale=-0.5)
            for c3 in range(3):
                stt(yT[:, 3 + c3, :], Wk["yb"][:, c3, :], C["gobc"][:, c3:c3 + 1], nrm[:], ALU.mult, ALU.mult,
                    [Wk["yb"], C["gobc"], nrm], [yT])
            if _cut <= 3:
                return
            xcc, xccb = Wk["xcc"][ti % 2], Wk["xccb"][ti % 2]
            for hh in range(2):
                zchunk(1536 + 128 * hh, psC0)
                cp("act", xcc[:, hh, :], psC0[:, 0:TT], [psC0], [xcc])
                cp("dve", xccb[:, hh, :], psC0[:, 0:TT], [psC0], [xccb])
            for hh in range(2):
                for q4 in range(4):
                    q = 4 * hh + q4
                    j = q % 2
                    ure, uim, mre, mim, gre, gim, t1, t2 = (Wk[nm][j] for nm in ("ure", "uim", "mre", "mim", "gre", "gim", "t1", "t2"))
                    sre, sim = Wk["sre"][j], Wk["sim"][j]
                    cst = Wk["cst"][j]
                    mm(psC1[:, 0:TT], C["blhs"][:, q, 0, :], xccb[:, hh, :], True, True, [C["blhs"], xccb], [psC1])
                    mm(psC1[:, TT:2 * TT], C["blhs"][:, q, 1, :], xccb[:, hh, :], True, True, [C["blhs"], xccb], [psC1])
                    cq, sq_ = C["cos"][:, q, :], C["sin"][:, q, :]
                    L = TT - 1
                    cp("act", ure[:], psC1[:, 0:TT], [psC1], [ure])
                    cp("act", uim[:], psC1[:, TT:2 * TT], [psC1], [uim])
                    tt("pool", mre[:], ure[:], cq, ALU.mult, [ure, C["cos"]], [mre])
                    tt("pool", t1[:], uim[:], sq_, ALU.mult, [uim, C["sin"]], [t1])
                    tt("dve", mre[:], mre[:], t1[:], ALU.add, [mre, t1], [mre])
                    tt("pool", mim[:], uim[:], cq, ALU.mult, [uim, C["cos"]], [mim])
                    tt("pool", t2[:], ure[:], sq_, ALU.mult, [ure, C["sin"]], [t2])
                    tt("dve", mim[:], mim[:], t2[:], ALU.subtract, [mim, t2], [mim])
                    rb_ = C["rr"][:, q:q + 1].to_broadcast([128, TT])
                    scan(gre[:], rb_, mre[:], Wk["scar"][:, q, 0:1], [C["rr"], mre, Wk["scar"]], [gre])
                    scan(gim[:], rb_, mim[:], Wk["scar"][:, q, 1:2], [C["rr"], mim, Wk["scar"]], [gim])
                    tt("dve", cst[:, 0:1], gim[:, L:L + 1], C["sin"][:, q, L:L + 1], ALU.mult, [gim, C["sin"]], [cst])
                    stt(Wk["scar"][:, q, 0:1], gre[:, L:L + 1], C["cos"][:, q, L:L + 1], cst[:, 0:1], ALU.mult, ALU.subtract,
                        [gre, C["cos"], cst], [Wk["scar"]])
                    tt("dve", cst[:, 1:2], gim[:, L:L + 1], C["cos"][:, q, L:L + 1], ALU.mult, [gim, C["cos"]], [cst])
                    stt(Wk["scar"][:, q, 1:2], gre[:, L:L + 1], C["sin"][:, q, L:L + 1], cst[:, 1:2], ALU.mult, ALU.add,
                        [gre, C["sin"], cst], [Wk["scar"]])
                    tt("pool", t1[:], gre[:], cq, ALU.mult, [gre, C["cos"]], [t1])
                    tt("pool", t2[:], gim[:], sq_, ALU.mult, [gim, C["sin"]], [t2])
                    tt("dve", sre[:], t1[:], t2[:], ALU.subtract, [t1, t2], [sre])
                    tt("pool", t1[:], gre[:], sq_, ALU.mult, [gre, C["sin"]], [t1])
                    tt("pool", t2[:], gim[:], cq, ALU.mult, [gim, C["cos"]], [t2])
                    tt("dve", sim[:], t1[:], t2[:], ALU.add, [t1, t2], [sim])
                    mm(psC2[:, 0:TT], C["clhs"][:, q, 0, :], sre[:], q4 == 0, False, [C["clhs"], sre], [psC2])
                    mm(psC2[:, 0:TT], C["clhs"][:, q, 1, :], sim[:], False, q4 == 3, [C["clhs"], sim], [psC2])
                if _cut <= 3.7:
                    continue
                stt(Wk["yc"][:, hh, :], xcc[:, hh, :], C["dd"][:, hh:hh + 1], psC2[:, 0:TT], ALU.mult, ALU.add,
                    [xcc, C["dd"], psC2], [Wk["yc"]])
                act(Wk["yc"][:, hh, :], Wk["yc"][:, hh, :], AF.Gelu_apprx_tanh, [Wk["yc"]], [Wk["yc"]])
                cp("act", Wk["ycb"][:, hh, :], Wk["yc"][:, hh, :], [Wk["yc"]], [Wk["ycb"]])
            if _cut <= 3.8:
                return
            for ho in range(2):
                for hi in range(2):
                    mm(psC0[:, 0:TT], C["gluw"][:, hi, 128 * ho:128 * ho + 128], Wk["ycb"][:, hi, :], hi == 0, hi == 1, [C["gluw"], Wk["ycb"]], [psC0])
                act(Wk["sg"][:], psC0[:, 0:TT], AF.Tanh, [psC0, C["glubh"]], [Wk["sg"]], scale=0.5, bias=C["glubh"][:, ho:ho + 1])
                stt(Wk["yc"][:, ho, :], Wk["sg"][:], 1.0, Wk["yc"][:, ho, :], ALU.add, ALU.mult, [Wk["yc"], Wk["sg"]], [Wk["yc"]])
                act(Wk["ycs"][:, ho, :], Wk["yc"][:, ho, :], AF.Square, [Wk["yc"]], [Wk["ycs"]], scale=0.5)
            if dbg and "y_c" in dbg_d:
                for hh in range(2):
                    dma("sp", dbg_d["y_c"][hh * 128:(hh + 1) * 128, t0:t0 + TT], Wk["yc"][:, hh, :], [Wk["yc"]], ())
            for hh in range(2):
                mm(psC0[:, 0:TT], ones_b[:], Wk["ycs"][:, hh, :], hh == 0, hh == 1, [ones_b, Wk["ycs"]], [psC0])
            nrm = Wk["nrmC"]
            act(nrm[:], psC0[:, 0:TT], AF.Ln, [psC0, eps_t], [nrm], scale=1.0 / 256, bias=eps_t[:, 0:1])
            act(nrm[:], nrm[:], AF.Exp, [nrm], [nrm], scale=-0.5)
            for hh in range(2):
                stt(yT[:, 6 + hh, :], Wk["yc"][:, hh, :], C["gobch"][:, hh:hh + 1], nrm[:], ALU.mult, ALU.mult,
                    [Wk["yc"], C["gobch"], nrm], [yT])
            if _cut <= 4:
                return
            for s in range(NSUB):
                gs = ti * NSUB + s
                rows = slice(t0 + s * 128, t0 + (s + 1) * 128)
                for half in range(2):
                    for k in range(8):
                        mm(psA[half][:], yT[:, k, s * 128:(s + 1) * 128], C["wout"][:, k, 512 * half:512 * half + 512], k == 0, k == 7,
                           [yT, C["wout"]], [psA[half]])
                    tt("dve", hb[:, s, 512 * half:512 * half + 512], hb[:, s, 512 * half:512 * half + 512], psA[half][:], ALU.add,
                       [hb, psA[half]], [hb])
                dma("sp", hmid_d[rows, :], hb[:, s, :], [hb], ())
                sse, rse = Wk["sse"][gs % 2], Wk["rse"][gs % 2]
                jk = Wk["sqe"]
                act(jk[:], hb[:, s, :], AF.Square, [hb], [sse, jk], accum_out=sse[:, 0:1])
                rstd_from_ss(rse, sse, 1024)
                xf = Wk["xf"][gs % 2]
                stt(Wk["xf32"][:], hb[:, s, :], rse[:, 0:1], C["gffn"][:], ALU.mult, ALU.mult, [hb, rse, C["gffn"]], [Wk["xf32"]])
                cp("act", xf[:], Wk["xf32"][:], [Wk["xf32"]], [xf])
                dma("sp", xf_d[rows, :], xf[:], [xf], ())
                route_logits(gs, s, C, Wk, psZ[0])
            route_tile(ti, C, Wk, psZ[0])

        def route_logits(gs, s_, C, Wk, pl):
            xf32 = Wk["xf32"]
            for half in range(2):
                xfT = Wk["xfT"][half]
                for k4 in range(4):
                    k = 4 * half + k4
                    tr(psA[half][:, k4 * 128:(k4 + 1) * 128], xf32[:, k * 128:(k + 1) * 128], ident_f[:], [xf32, ident_f], [psA[half]])
                cp("act", xfT[:], psA[half][:].rearrange("p (k t) -> p k t", k=4), [psA[half]], [xfT])
            for half in range(2):
                xfT = Wk["xfT"][half]
                for k4 in range(4):
                    k = 4 * half + k4
                    mm(pl[:, TT + 32 * s_:TT + 32 * s_ + 20], xfT[:, k4, :], C["wr"][:, k, :], k == 0, k == 7, [xfT, C["wr"]], [pl])

        def route_tile(ti, C, Wk, pl):
            J = NSUB
            g0 = ti * NSUB
            j = ti % 2
            lg, sm, em, em2, pen, gsel, rank, ohb = (Wk[nm][j] for nm in ("lg", "sm", "em", "em2", "pen", "gsel", "rank", "ohb"))
            plv = pl[:, TT:TT + 32 * J].rearrange("p (j c) -> p j c", j=J)
            tt("dve", lg[:], plv[:, :, 0:20], C["rb"][:].unsqueeze(1).to_broadcast([128, J, 20]), ALU.add, [pl, C["rb"]], [lg])
            red(sm[:, 0, :], lg[:, :, 0:4], ALU.max, [lg], [sm])
            tt("dve", em2[:, :, 0:4], lg[:, :, 0:4], sm[:, 0, :].unsqueeze(2).to_broadcast([128, J, 4]), ALU.subtract, [lg, sm], [em2])
            act(em2[:, :, 0:4], em2[:, :, 0:4], AF.Exp, [em2], [em2])
            red(sm[:, 1, :], em2[:, :, 0:4], ALU.add, [em2], [sm])
            recip(sm[:, 2, :], sm[:, 1, :], [sm], [sm])
            tt("dve", gsel[:], lg[:, :, 0:4], sm[:, 0, :].unsqueeze(2).to_broadcast([128, J, 4]), ALU.is_ge, [lg, sm], [gsel])
            ts("dve", pen[:], gsel[:], -1.0, BIG, ALU.add, ALU.mult, [gsel], [pen])
            for jj in range(J):
                tt("dve", em[:, jj, :].rearrange("p (g e) -> p g e", g=4), lg[:, jj, 4:20].rearrange("p (g e) -> p g e", g=4),
                   pen[:, jj, :].unsqueeze(2).to_broadcast([128, 4, 4]), ALU.add, [lg, pen], [em])
            red(sm[:, 3, :], em[:], ALU.max, [em], [sm])
            oh1 = ohs[:, g0:g0 + J, 0, :]
            oh2 = ohs[:, g0:g0 + J, 1, :]
            tt("dve", oh1, em[:], sm[:, 3, :].unsqueeze(2).to_broadcast([128, J, 16]), ALU.is_ge, [em, sm], [ohs])
            stt(em2[:], oh1, -BIG, em[:], ALU.mult, ALU.add, [ohs, em], [em2])
            red(sm[:, 4, :], em2[:], ALU.max, [em2], [sm])
            tt("dve", oh2, em2[:], sm[:, 4, :].unsqueeze(2).to_broadcast([128, J, 16]), ALU.is_ge, [em2, sm], [ohs])
            tt("dve", sm[:, 5, :], sm[:, 4, :], sm[:, 3, :], ALU.subtract, [sm], [sm])
            act(sm[:, 6, :], sm[:, 5, :], AF.Exp, [sm], [sm])
            ts("dve", sm[:, 7, :], sm[:, 6, :], 1.0, None, ALU.add, None, [sm], [sm])
            recip(sm[:, 7, :], sm[:, 7, :], [sm], [sm])
            tt("dve", sm[:, 8, :], sm[:, 6, :], sm[:, 7, :], ALU.mult, [sm], [sm])
            tt("dve", gts[:, g0:g0 + J, 0], sm[:, 7, :], sm[:, 2, :], ALU.mult, [sm], [gts])
            tt("dve", gts[:, g0:g0 + J, 1], sm[:, 8, :], sm[:, 2, :], ALU.mult, [sm], [gts])
            tt("dve", ohb[:], oh1, oh2, ALU.add, [ohs], [ohb])
            o1, o2 = TT + 32 * J, TT + 32 * J + 16 * J
            for jj in range(J):
                mm(pl[:, o1 + 16 * jj:o1 + 16 * jj + 16], triu_b[:], ohb[:, jj, :], True, True, [triu_b, ohb], [pl])
                mm(pl[:, o2 + 16 * jj:o2 + 16 * jj + 16], ones_b[:], ohb[:, jj, :], True, True, [ones_b, ohb], [pl])
            if ti == 0:
                memset("pool", base[:], 0.0, [base])
            prv = pl[:, o1:o1 + 16 * J].rearrange("p (j e) -> p j e", j=J)
            tt("dve", rank[:], prv, base[:].unsqueeze(1).to_broadcast([128, J, 16]), ALU.add, [pl, base], [rank])
            for jj in range(J):
                if jj >= 1:
                    for j0 in range(jj):
                        tt("dve", rank[:, jj, :], rank[:, jj, :], pl[:, o2 + 16 * j0:o2 + 16 * j0 + 16], ALU.add, [rank, pl], [rank])
            for jj in range(J):
                tt("dve", base[:], base[:], pl[:, o2 + 16 * jj:o2 + 16 * jj + 16], ALU.add, [base, pl], [base])
            for k in range(2):
                tt("dve", em2[:], ohs[:, g0:g0 + J, k, :], rank[:], ALU.mult, [ohs, rank], [em2])
                red(rks[:, g0:g0 + J, k], em2[:], ALU.add, [em2], [rks])

        def combine_phase(final):
            sb = P.sb
            hbs = [sb([128, 1024], F32) for _ in range(6)]
            yks = [sb([128, 1024], F32) for _ in range(8)]
            if final:
                gfin = sb([128, 1024], F32)
                dma("sp", gfin[:], W["final_norm_g"].partition_broadcast(128), (), [gfin])
                obs = [sb([128, 1024], F32) for _ in range(2)]
                junk = sb([128, 1024], BF16)
                sss = [sb([128, 1], F32) for _ in range(2)]
                rss = [sb([128, 1], F32) for _ in range(2)]
            for gs in range(NS):
                hb = hbs[gs % 6]
                rows = slice(gs * 128, (gs + 1) * 128)
                dma("sp", hb[:], hmid_d[rows, :], (), [hb])
                for k in range(2):
                    yk = yks[(2 * gs + k) % 8]
                    P.dma("pool", lambda e, k=k, yk=yk, gs=gs: e.indirect_dma_start(out=yk[:], out_offset=None, in_=ys_d,
                                                                             in_offset=bass.IndirectOffsetOnAxis(dest_i[:, gs, k:k + 1], 0)),
                          [dest_i], [yk], cost=6.0)
                    stt(hb[:], yk[:], gts[:, gs, k:k + 1], hb[:], ALU.mult, ALU.add, [yk, gts, hb], [hb])
                if not final:
                    dma("sp", hmid_d[rows, :], hb[:], [hb], ())
                else:
                    ss, rs, ob = sss[gs % 2], rss[gs % 2], obs[gs % 2]
                    act(junk[:], hb[:], AF.Square, [hb], [ss, junk], accum_out=ss[:, 0:1])
                    rstd_from_ss(rs, ss, 1024)
                    stt(ob[:], hb[:], rs[:, 0:1], gfin[:], ALU.mult, ALU.mult, [hb, rs, gfin], [ob])
                    dma("sp", out_d[rows, :], ob[:], [ob], ())

        class TTv:
            def __init__(self, base_t, sl):
                self.b = base_t
                self.sl = sl
                self.r = base_t.r

            def __getitem__(self, k):
                return self.b.t[:, self.sl]

        def dispatch_phase(Wd):
            sb = P.sb
            padded = sb([128, 16], F32)
            pend = sb([128, 16], F32)
            pstart = sb([128, 16], F32)
            ti_ = sb([128, 16], I32)
            ts("dve", padded[:], base[:], float(EB - 1), 1.0 / EB, ALU.add, ALU.mult, [base], [padded])
            ts("dve", padded[:], padded[:], -0.5 + 1.0 / (4 * EB), None, ALU.add, None, [padded], [padded])
            cp("dve", ti_[:], padded[:], [padded], [ti_])
            cp("dve", padded[:], ti_[:], [ti_], [padded])
            ts("dve", padded[:], padded[:], float(EB), None, ALU.mult, None, [padded], [padded])
            onesf = sb([128, 16], F32)
            memset("pool", onesf[:], 1.0, [onesf])
            P.op("dve", lambda e: e.tensor_tensor_scan(out=pend[:], data0=onesf[:], data1=padded[:], initial=0.0, op0=ALU.mult, op1=ALU.add),
                 [onesf, padded], [pend])
            tt("dve", pstart[:], pend[:], padded[:], ALU.subtract, [pend, padded], [pstart])
            big = sb([128, NS * 2, 16], F32)
            dsum = sb([128, NS * 2], F32)
            tt("dve", big[:], ohs[:].rearrange("p s k e -> p (s k) e"), pstart[:].unsqueeze(1).to_broadcast([128, NS * 2, 16]), ALU.mult,
               [ohs, pstart], [big])
            P.op("dve", lambda e: e.tensor_reduce(out=dsum[:], in_=big[:], axis=AX.X, op=ALU.add), [big], [dsum])
            tt("dve", dsum[:], dsum[:], rks[:].rearrange("p s k -> p (s k)"), ALU.add, [dsum, rks], [dsum])
            cp("dve", dest_i[:].rearrange("p s k -> p (s k)"), dsum[:], [dsum], [dest_i])
            bb_ = sb([128, NB, 16], F32)
            bsum = sb([128, NB], F32)
            thr = sb([128, NB], F32)
            ts("dve", thr[:], iota_f[:, 0:NB], float(EB), None, ALU.mult, None, [iota_f], [thr])
            tt("dve", bb_[:], pend[:].unsqueeze(1).to_broadcast([128, NB, 16]), thr[:].unsqueeze(2).to_broadcast([128, NB, 16]), ALU.is_le,
               [pend, thr], [bb_])
            P.op("dve", lambda e: e.tensor_reduce(out=bsum[:], in_=bb_[:], axis=AX.X, op=ALU.add), [bb_], [bsum])
            ts("dve", bsum[:], bsum[:], 15.0, None, ALU.min, None, [bsum], [bsum])
            cp("dve", blk_e[:], bsum[:], [bsum], [blk_e])
            xt = [sb([128, 1024], BF16) for _ in range(8)]
            for gs in range(NS):
                xb_ = xt[gs % 8]
                dma("sp", xb_[:], xf_d[gs * 128:(gs + 1) * 128, :], [r_xf], [xb_])
                for k in range(2):
                    P.dma("pool", lambda e, gs=gs, k=k, xb_=xb_: e.indirect_dma_start(
                        out=xs_d, out_offset=bass.IndirectOffsetOnAxis(dest_i[:, gs, k:k + 1], 0), in_=xb_[:], in_offset=None),
                        [xb_, dest_i], ())

        def expert_phase(l):
            sb = P.sb
            wg = [sb([128, 8, 512], BF16) for _ in range(2)]
            wu = [sb([128, 8, 512], BF16) for _ in range(2)]
            wd = [sb([128, 4, 1024], BF16) for _ in range(2)]
            xsb = [sb([128, 4, 1024], BF16) for _ in range(2)]
            xsTs = [sb([128, 8, EB], BF16) for _ in range(2)]
            sgts = [sb([128, EB], F32) for _ in range(2)]
            hTs = [sb([128, 4, EB], BF16) for _ in range(2)]
            yo = [sb([128, 1024], F32) for _ in range(4)]

            def wload(e, b, dst, src, pat):
                if "r" not in REGS:
                    REGS["r"] = e.alloc_register("ereg")
                rg = REGS["r"]
                id0 = nc.next_id()
                e.reg_load(rg, blk_e[0:1, b:b + 1])
                v = e.snap(rg, min_val=0, max_val=NEXP - 1)
                ins = e.dma_start(out=dst[:], in_=src[l][bass.ds(v, 1), :, :].rearrange(pat, p=128))
                id1 = nc.next_id()
                for i in range(id0, id1 + 1):
                    for nm in ("Pool_tmp_%d" % i, "Pool_Pool_ereg_snap_%d" % i):
                        try:
                            e.free_register(bass.RegisterHandle(nm, rg.engine))
                        except ValueError:
                            pass
                return ins

            for b in range(NB):
                i2 = b % 2
                xsT, hT = xsTs[i2], hTs[i2]
                P.dma("pool", lambda e, b=b, i2=i2: wload(e, b, wg[i2], W["expert_w_gate"], "o (k p) n -> p (o k) n"), [blk_e], [wg[i2]])
                P.dma("pool", lambda e, b=b, i2=i2: wload(e, b, wu[i2], W["expert_w_up"], "o (k p) n -> p (o k) n"), [blk_e], [wu[i2]])
                P.dma("pool", lambda e, b=b, i2=i2: wload(e, b, wd[i2], W["expert_w_down"], "o (k p) n -> p (o k) n"), [blk_e], [wd[i2]])
                dma("sp", xsb[i2][:], xs_d[b * EB:(b + 1) * EB, :].rearrange("(s p) d -> p s d", p=128), [r_xs], [xsb[i2]])
                for s in range(4):
                    if s % 2 == 0:
                        pt_ap, pt_res = psT[:], psT
                    else:
                        pt_ap, pt_res = psM[2][:].bitcast(BF16), psM[2]
                    for k in range(8):
                        tr(pt_ap[:, k * 128:(k + 1) * 128], xsb[i2][:, s, k * 128:(k + 1) * 128], ident_b[:], [xsb[i2], ident_b], [pt_res])
                    cp("act" if s % 2 else "dve", xsT[:, :, s * 128:(s + 1) * 128], pt_ap.rearrange("p (k t) -> p k t", k=8), [pt_res], [xsT])
                for f in range(4):
                    pg, pu = (psZ[0], psZ[1]) if f % 2 == 0 else (psM[0], psM[1])
                    sgt = sgts[f % 2]
                    for k in range(8):
                        mm(pg[:], wg[i2][:, k, 128 * f:128 * f + 128], xsT[:, k, :], k == 0, k == 7, [wg[i2], xsT], [pg])
                    for k in range(8):
                        mm(pu[:], wu[i2][:, k, 128 * f:128 * f + 128], xsT[:, k, :], k == 0, k == 7, [wu[i2], xsT], [pu])
                    act(sgt[:], pg[:], AF.Silu, [pg], [sgt])
                    tt("dve", hT[:, f, :], sgt[:], pu[:], ALU.mult, [sgt, pu], [hT])
                for s in range(4):
                    yb_ = yo[s % 4]
                    for half in range(2):
                        py = psA[half]
                        for f in range(4):
                            mm(py[:], hT[:, f, s * 128:(s + 1) * 128], wd[i2][:, f, 512 * half:512 * half + 512], f == 0, f == 3, [hT, wd[i2]], [py])
                        cp("act" if half else "dve", yb_[:, 512 * half:512 * half + 512], py[:], [py], [yb_])
                    dma("sp", ys_d[b * EB + s * 128:b * EB + (s + 1) * 128, :], yb_[:], [yb_], ())

        P.phase_end()
        for l in range(n_layers):
            with ExitStack() as stL:
                P.stack = stL
                C, Wk = {}, {}
                with ExitStack() as stS:
                    load_layer_consts(l, C, stS)
                    P.phase_end()
                alloc_mixer_work(Wk)
                memset("pool", Wk["zero"][:], 0.0, [Wk["zero"]])
                zrows = list(range(0, NB * EB, 128))
                zper = -(-len(zrows) // NT)
                for ti in range(NT):
                    mixer_tile(l, ti, C, Wk, first_layer=(l == 0))
                    for r0 in zrows[ti * zper:(ti + 1) * zper]:
                        dma("sp", xs_d[r0:r0 + 128, :], Wk["zero"][:], [Wk["zero"]], ())
                P.phase_end()
            import os as _os
            if _os.environ.get("K_STOP") == "mixer":
                break
            with ExitStack() as stD:
                P.stack = stD
                dispatch_phase(None)
                P.phase_end()
            with ExitStack() as stE:
                P.stack = stE
                expert_phase(l)
                P.phase_end()
            with ExitStack() as stC:
                P.stack = stC
                combine_phase(final=(l == n_layers - 1))
                P.phase_end()
        P.stack = st0
    return nc


_CACHE = {}


def kernel(**inputs):
    x = np.ascontiguousarray(inputs["x"], dtype=np.float32)
    B, S, _ = x.shape
    if S not in _CACHE:
        _CACHE[S] = build(S)
    nc = _CACHE[S]
    shared = {name: np.ascontiguousarray(inputs[name], dtype=np.float32) for name, _ in PARAMS}
    in_maps = []
    for b in range(B):
        m = dict(shared)
        m["x"] = x[b]
        in_maps.append(m)
    res = run_bass_kernel_spmd(nc, in_maps, core_ids=list(range(B)))
    return np.stack([np.asarray(r["out"], dtype=np.float32) for r in res.results], axis=0)
```
